# Optimizing a Trainium2 kernel written in Bass

```python
import math
import jax, jax.numpy as jnp
from jax import lax
import numpy as np

D_MODEL = 1024
BATCH = 8
SEQ = 4096
DEPTH = 2

GRID_W = 64
CTX_LEN = 256

BRANCH_W = 512
N_BRANCH = 3

ATT_HEADS = 4
ATT_DH = 64
ATT_DV = 2 * ATT_DH
ATT_QK = ATT_HEADS * 2 * ATT_DH
Q_BLOCK = 128
ROPE_BASE = 10000.0

LRU_BLOCKS = 8
LRU_BS = BRANCH_W // LRU_BLOCKS
LRU_C = 8.0
CONV_W = 4

SSD_HEADDIM = 64
SSD_HEADS = BRANCH_W // SSD_HEADDIM
SSD_GROUPS = 2
SSD_HPG = SSD_HEADS // SSD_GROUPS
SSD_STATE = 128
SSD_CHUNK = 128
SSD_XBC = BRANCH_W + 2 * SSD_GROUPS * SSD_STATE
N_DIR = 2

N_EXPERTS = 64
TOP_K = 8
N_GROUPS = 8
TOPK_GROUPS = 4
EXPERT_F = 256
SHARED_F = 256
ROUTED_SCALE = 2.5
MOE_BLOCK = 128

DN_ALPHA = (2 * DEPTH) ** 0.25
DN_BETA = (8 * DEPTH) ** -0.25
LN_EPS = 1e-5
RMS_EPS = 1e-6

IN_WIDTHS = (ATT_QK, ATT_QK, ATT_HEADS * ATT_DV,
             BRANCH_W, BRANCH_W,
             BRANCH_W, SSD_XBC, N_DIR * SSD_HEADS,
             N_BRANCH * D_MODEL)
IN_SPLITS = tuple(int(s) for s in np.cumsum(IN_WIDTHS)[:-1])
IN_TOTAL = int(sum(IN_WIDTHS))

kernel_name = 'hybrid_dit_diffattn_rglru_ssd_moe'


def layer_norm(x, g, b):
    xf = x.astype(jnp.float32)
    mu = jnp.mean(xf, axis=-1, keepdims=True)
    var = jnp.mean(jnp.square(xf - mu), axis=-1, keepdims=True)
    return ((xf - mu) * lax.rsqrt(var + LN_EPS)).astype(x.dtype) * g + b


def rms_norm(x, g):
    xf = x.astype(jnp.float32)
    return (xf * lax.rsqrt(jnp.mean(xf * xf, axis=-1, keepdims=True) + RMS_EPS)).astype(x.dtype) * g


def axial_rope(n_tokens):
    rows = n_tokens // GRID_W
    row = jnp.repeat(jnp.arange(rows, dtype=jnp.float32), GRID_W)
    col = (jnp.arange(n_tokens) % GRID_W).astype(jnp.float32)
    n_freq = ATT_DH // 4
    inv = ROPE_BASE ** (-jnp.arange(n_freq, dtype=jnp.float32) / n_freq)
    ang = jnp.concatenate([row[:, None] * inv, col[:, None] * inv], axis=-1)
    return jnp.cos(ang), jnp.sin(ang)


def apply_rope(x, cos, sin):
    x1, x2 = jnp.split(x, 2, axis=-1)
    c = cos[:, None, None, :].astype(x.dtype)
    s = sin[:, None, None, :].astype(x.dtype)
    return jnp.concatenate([x1 * c - x2 * s, x2 * c + x1 * s], axis=-1)


def diff_attn_block(q, k, v, lam):
    s = jnp.einsum('bqhmd,bkhmd->bhmqk', q, k).astype(jnp.float32) * (ATT_DH ** -0.5)
    p = jax.nn.softmax(s, axis=-1)
    p = p[:, :, 0] - lam * p[:, :, 1]
    return jnp.einsum('bhqk,bkhe->bqhe', p.astype(v.dtype), v)


def diff_attn_sweep(q, k, v, lam):
    B, L = q.shape[:2]
    qb = q.reshape(B, L // Q_BLOCK, Q_BLOCK, *q.shape[2:]).swapaxes(0, 1)
    ob = lax.map(lambda qi: diff_attn_block(qi, k, v, lam), qb)
    return ob.swapaxes(0, 1).reshape(B, L, *ob.shape[3:])


def diff_attn_head_norm(o, g, lam_init):
    B, L = o.shape[:2]
    return (rms_norm(o, g) * (1.0 - lam_init)).reshape(B, L, -1)


def dwconv_centred(x, w, bias):
    L = x.shape[1]
    left = CONV_W // 2
    xp = jnp.pad(x, ((0, 0), (left, CONV_W - 1 - left), (0, 0)))
    y = xp[:, 0:L] * w[0]
    for j in range(1, CONV_W):
        y = y + xp[:, j:j + L] * w[j]
    return y + bias


def _lin_combine(e1, e2):
    a1, b1 = e1
    a2, b2 = e2
    return a1 * a2, a2 * b1 + b2


def linear_scan(a, b, h0, reverse):
    if reverse:
        b = b.at[:, -1].add(a[:, -1] * h0)
    else:
        b = b.at[:, 0].add(a[:, 0] * h0)
    _, h = lax.associative_scan(_lin_combine, (a, b), reverse=reverse, axis=1)
    return h


def rglru_coeffs(u, w_a, b_a, w_i, b_i, lam):
    B, L, W = u.shape
    ub = u.reshape(B, L, LRU_BLOCKS, LRU_BS)
    r = jax.nn.sigmoid(jnp.einsum('blnk,nkj->blnj', ub, w_a).reshape(B, L, W) + b_a)
    i = jax.nn.sigmoid(jnp.einsum('blnk,nkj->blnj', ub, w_i).reshape(B, L, W) + b_i)
    log_a = -LRU_C * r * jax.nn.softplus(-lam)
    return jnp.exp(log_a), jnp.sqrt(1.0 - jnp.exp(2.0 * log_a)) * (i * u)


def rglru_bidir(u, uc, w_a, b_a, w_i, b_i, lam):
    u = u.astype(jnp.float32)
    uc = uc.astype(jnp.float32)
    y = jnp.zeros_like(u)
    yc = jnp.zeros_like(uc)
    for d in range(N_DIR):
        rev = d == 1
        a_c, b_c = rglru_coeffs(uc, w_a[d], b_a[d], w_i[d], b_i[d], lam[d])
        h_c = linear_scan(a_c, b_c, jnp.zeros_like(b_c[:, 0]), rev)
        h_end = h_c[:, 0] if rev else h_c[:, -1]
        a_l, b_l = rglru_coeffs(u, w_a[d], b_a[d], w_i[d], b_i[d], lam[d])
        y = y + linear_scan(a_l, b_l, h_end, rev)
        yc = yc + h_c
    return y, yc


def segsum(x):
    T = x.shape[-1]
    xe = jnp.broadcast_to(x[..., :, None], x.shape + (T,))
    strict = jnp.tril(jnp.ones((T, T), dtype=bool), -1)
    cs = jnp.cumsum(jnp.where(strict, xe, 0.0), axis=-2)
    return jnp.where(jnp.tril(jnp.ones((T, T), dtype=bool)), cs, -jnp.inf)


def ssd_chunked(xdt, a, bm, cm, h0):
    B, T, G, R, P = xdt.shape
    nc = T // SSD_CHUNK
    xdt = xdt.reshape(B, nc, SSD_CHUNK, G, R, P)
    bm = bm.reshape(B, nc, SSD_CHUNK, G, -1)
    cm = cm.reshape(B, nc, SSD_CHUNK, G, -1)
    a = a.reshape(B, nc, SSD_CHUNK, G, R).transpose(0, 3, 4, 1, 2)
    a_cs = jnp.cumsum(a, axis=-1)
    decay_in = jnp.exp(segsum(a))
    cb = jnp.einsum('bclgn,bcsgn->bcgls', cm, bm)
    y_diag = jnp.einsum('bcgls,bgrcls,bcsgrp->bclgrp', cb, decay_in, xdt)
    decay_to_end = jnp.exp(a_cs[..., -1:] - a_cs)
    states = jnp.einsum('bcsgn,bgrcs,bcsgrp->bcgrpn', bm, decay_to_end, xdt)
    states = jnp.concatenate([h0[:, None], states], axis=1)
    chunk_a = jnp.pad(a_cs[..., -1], ((0, 0), (0, 0), (0, 0), (1, 0)))
    states = jnp.einsum('bgrzc,bcgrpn->bzgrpn', jnp.exp(segsum(chunk_a)), states)
    y_off = jnp.einsum('bclgn,bcgrpn,bgrcl->bclgrp', cm, states[:, :-1], jnp.exp(a_cs))
    return (y_diag + y_off).reshape(B, T, G, R, P), states[:, -1]


def ssd_direction(xs, bm, cm, dt, A, h0, reverse):
    if reverse:
        xs, bm, cm, dt = xs[:, ::-1], bm[:, ::-1], cm[:, ::-1], dt[:, ::-1]
    y, h_last = ssd_chunked(xs * dt[..., None], dt * A, bm, cm, h0)
    return (y[:, ::-1] if reverse else y), h_last


def ssd_prep(xbc, dt_raw, conv_w, conv_b):
    u = jax.nn.silu(dwconv_centred(xbc, conv_w, conv_b)).astype(jnp.float32)
    B, L = u.shape[:2]
    xs, bm, cm = jnp.split(u, [BRANCH_W, BRANCH_W + SSD_GROUPS * SSD_STATE], axis=-1)
    return (xs.reshape(B, L, SSD_GROUPS, SSD_HPG, SSD_HEADDIM),
            bm.reshape(B, L, SSD_GROUPS, SSD_STATE),
            cm.reshape(B, L, SSD_GROUPS, SSD_STATE),
            dt_raw.astype(jnp.float32).reshape(B, L, N_DIR, SSD_GROUPS, SSD_HPG))


def ssd_bidir(xbc, dt_raw, xbcc, dtc_raw, conv_w, conv_b, dt_bias, a_log, d_skip):
    xs, bm, cm, dt = ssd_prep(xbc, dt_raw, conv_w, conv_b)
    xsc, bmc, cmc, dtc = ssd_prep(xbcc, dtc_raw, conv_w, conv_b)
    B, L = xs.shape[:2]
    Lc = xsc.shape[1]
    dtb = dt_bias.astype(jnp.float32).reshape(N_DIR, SSD_GROUPS, SSD_HPG)
    A = -jnp.exp(a_log.astype(jnp.float32)).reshape(N_DIR, SSD_GROUPS, SSD_HPG)
    skip = d_skip.astype(jnp.float32).reshape(SSD_GROUPS, SSD_HPG, 1)
    y = skip * xs
    yc = skip * xsc
    h0 = jnp.zeros((B, SSD_GROUPS, SSD_HPG, SSD_HEADDIM, SSD_STATE), jnp.float32)
    for d in range(N_DIR):
        rev = d == 1
        dt_c = jax.nn.softplus(dtc[:, :, d] + dtb[d])
        dt_l = jax.nn.softplus(dt[:, :, d] + dtb[d])
        y_c, h_ctx = ssd_direction(xsc, bmc, cmc, dt_c, A[d], h0, rev)
        y_l, _ = ssd_direction(xs, bm, cm, dt_l, A[d], h_ctx, rev)
        y = y + y_l
        yc = yc + y_c
    return y.reshape(B, L, BRANCH_W), yc.reshape(B, Lc, BRANCH_W)


def gated_group_rmsnorm(y, z, g):
    B, L = y.shape[:2]
    t = (y * jax.nn.silu(z.astype(jnp.float32))).reshape(B, L, SSD_GROUPS, BRANCH_W // SSD_GROUPS)
    t = t * lax.rsqrt(jnp.mean(t * t, axis=-1, keepdims=True) + RMS_EPS)
    return (t.reshape(B, L, BRANCH_W) * g).astype(z.dtype)


def merge_branches(ya, yb, yc, gates, w_branch, w_out):
    ys = jnp.stack([ya, yb, yc], axis=2)
    proj = jnp.einsum('blnk,nkd->blnd', ys, w_branch)
    g = jax.nn.sigmoid(gates.reshape(*gates.shape[:2], N_BRANCH, D_MODEL).astype(jnp.float32))
    return jnp.einsum('blnd,de->ble', g.astype(proj.dtype) * proj, w_out)


def token_mixers(h, hc, cos, sin, w_in, lam_q, lam_k, lam_init, attn_g,
                 lru_cw, lru_cb, lru_wa, lru_ba, lru_wi, lru_bi, lru_lam,
                 ssd_cw, ssd_cb, ssd_dtb, ssd_alog, ssd_d, ssd_g,
                 w_branch, w_out, need_ctx):
    B, L, _ = h.shape
    Lc = hc.shape[1]
    q, k, v, lx, lg, z, xbc, dt, gates = jnp.split(h @ w_in, IN_SPLITS, axis=-1)
    qc, kc, vc, lxc, lgc, zc, xbcc, dtc, gatesc = jnp.split(hc @ w_in, IN_SPLITS, axis=-1)

    lam = (jnp.exp(jnp.sum(lam_q[0] * lam_k[0])) - jnp.exp(jnp.sum(lam_q[1] * lam_k[1]))).astype(jnp.float32) + lam_init
    q = apply_rope(q.reshape(B, L, ATT_HEADS, 2, ATT_DH), cos, sin)
    k = apply_rope(k.reshape(B, L, ATT_HEADS, 2, ATT_DH), cos, sin)
    kc = kc.reshape(B, Lc, ATT_HEADS, 2, ATT_DH)
    vc = vc.reshape(B, Lc, ATT_HEADS, ATT_DV)
    k_all = jnp.concatenate([k, kc], axis=1)
    v_all = jnp.concatenate([v.reshape(B, L, ATT_HEADS, ATT_DV), vc], axis=1)
    ya = diff_attn_head_norm(diff_attn_sweep(q, k_all, v_all, lam), attn_g, lam_init)

    yb, ybc = rglru_bidir(dwconv_centred(lx, lru_cw, lru_cb), dwconv_centred(lxc, lru_cw, lru_cb),
                          lru_wa, lru_ba, lru_wi, lru_bi, lru_lam)
    yb = yb.astype(h.dtype) * jax.nn.gelu(lg)

    yc, ycc = ssd_bidir(xbc, dt, xbcc, dtc, ssd_cw, ssd_cb, ssd_dtb, ssd_alog, ssd_d)
    yc = gated_group_rmsnorm(yc, z, ssd_g)

    out = merge_branches(ya, yb, yc, gates, w_branch, w_out)
    if not need_ctx:
        return out, None
    qc = qc.reshape(B, Lc, ATT_HEADS, 2, ATT_DH)
    yac = diff_attn_head_norm(diff_attn_block(qc, kc, vc, lam), attn_g, lam_init)
    ybc = ybc.astype(hc.dtype) * jax.nn.gelu(lgc)
    ycc = gated_group_rmsnorm(ycc, zc, ssd_g)
    return out, merge_branches(yac, ybc, ycc, gatesc, w_branch, w_out)


def swiglu(h, w_up, w_down):
    g, u = jnp.split(h @ w_up, 2, axis=-1)
    return (jax.nn.silu(g) * u) @ w_down


def routed_experts(h, eidx, w, w_up, w_down):
    N, D = h.shape
    NK = N * TOP_K
    nb = (NK + N_EXPERTS * (MOE_BLOCK - 1) + MOE_BLOCK - 1) // MOE_BLOCK
    P = nb * MOE_BLOCK
    e_flat = eidx.reshape(-1)
    tok_flat = jnp.repeat(jnp.arange(N, dtype=jnp.int32), TOP_K)
    w_flat = w.reshape(-1)
    counts = jnp.zeros((N_EXPERTS,), jnp.int32).at[e_flat].add(1)
    padded = (counts + MOE_BLOCK - 1) // MOE_BLOCK * MOE_BLOCK
    pend = jnp.cumsum(padded)
    pstart = pend - padded
    start = jnp.cumsum(counts) - counts
    order = jnp.argsort(e_flat)
    se = e_flat[order]
    dest = pstart[se] + jnp.arange(NK, dtype=jnp.int32) - start[se]
    row_tok = jnp.full((P,), N, jnp.int32).at[dest].set(tok_flat[order])
    row_w = jnp.zeros((P,), w.dtype).at[dest].set(w_flat[order])
    blk_e = jnp.minimum(jnp.searchsorted(pend, jnp.arange(nb, dtype=jnp.int32) * MOE_BLOCK, side='right'),
                        N_EXPERTS - 1)
    h_pad = jnp.concatenate([h, jnp.zeros((1, D), h.dtype)], axis=0)

    def expert_block(args):
        e, toks, ws = args
        return swiglu(h_pad[toks], w_up[e], w_down[e]) * ws[:, None].astype(h.dtype)

    ys = lax.map(expert_block, (blk_e, row_tok.reshape(nb, MOE_BLOCK), row_w.reshape(nb, MOE_BLOCK)))
    return jax.ops.segment_sum(ys.reshape(P, D), row_tok, num_segments=N + 1)[:N]


def moe_ffn(h, w_router, router_bias, w_up, w_down, ws_up, ws_down):
    N = h.shape[0]
    scores = jax.nn.sigmoid((h @ w_router).astype(jnp.float32))
    sel = scores + router_bias
    grp_score = lax.top_k(sel.reshape(N, N_GROUPS, N_EXPERTS // N_GROUPS), 2)[0].sum(-1)
    _, gidx = lax.top_k(grp_score, TOPK_GROUPS)
    gmask = jax.nn.one_hot(gidx, N_GROUPS).sum(-2) > 0
    sel = jnp.where(jnp.repeat(gmask, N_EXPERTS // N_GROUPS, axis=-1), sel, -jnp.inf)
    _, eidx = lax.top_k(sel, TOP_K)
    w = jnp.take_along_axis(scores, eidx, axis=-1)
    w = w / jnp.sum(w, axis=-1, keepdims=True) * ROUTED_SCALE
    return swiglu(h, ws_up, ws_down) + routed_experts(h, eidx, w, w_up, w_down)


def setup_inputs(seed: int = 0) -> dict:
    key = jax.random.key(seed)
    kit = iter(jax.random.split(key, 64))

    def nrm(shape, scale):
        return jax.random.normal(next(kit), shape, jnp.float32) * scale

    L, D = DEPTH, D_MODEL
    v0 = 2 * ATT_QK
    col_scale = np.ones((IN_TOTAL,), np.float32)
    col_scale[v0:v0 + ATT_HEADS * ATT_DV] = DN_BETA
    s_lru = jax.random.uniform(next(kit), (L, N_DIR, BRANCH_W), jnp.float32,
                               minval=0.9, maxval=0.999) ** (1.0 / LRU_C)
    dt0 = jnp.exp(jax.random.uniform(next(kit), (L, N_DIR, SSD_HEADS), jnp.float32,
                                     minval=math.log(1e-3), maxval=math.log(1e-1)))
    a0 = jax.random.uniform(next(kit), (L, N_DIR, SSD_HEADS), jnp.float32, minval=1.0, maxval=16.0)
    return {
        'x': nrm((BATCH, SEQ, D), 1.0),
        'c': nrm((BATCH, D), 1.0),
        'ctx': nrm((BATCH, CTX_LEN, D), 1.0),
        'c_ctx': nrm((D,), 1.0),
        'w_mod': nrm((L, D, 6 * D), 0.5 * D ** -0.5),
        'b_mod': nrm((L, 6 * D), 0.01),
        'w_in': nrm((L, D, IN_TOTAL), D ** -0.5) * jnp.asarray(col_scale),
        'lam_q': nrm((L, 2, ATT_DH), 0.1),
        'lam_k': nrm((L, 2, ATT_DH), 0.1),
        'attn_norm_g': 1.0 + nrm((L, ATT_HEADS, ATT_DV), 0.02),
        'lru_conv_w': nrm((L, CONV_W, BRANCH_W), CONV_W ** -0.5),
        'lru_conv_b': nrm((L, BRANCH_W), 0.01),
        'lru_wa': nrm((L, N_DIR, LRU_BLOCKS, LRU_BS, LRU_BS), LRU_BS ** -0.5),
        'lru_ba': nrm((L, N_DIR, BRANCH_W), 0.01),
        'lru_wi': nrm((L, N_DIR, LRU_BLOCKS, LRU_BS, LRU_BS), LRU_BS ** -0.5),
        'lru_bi': nrm((L, N_DIR, BRANCH_W), 0.01),
        'lru_lambda': jnp.log(s_lru) - jnp.log1p(-s_lru),
        'ssd_conv_w': nrm((L, CONV_W, SSD_XBC), CONV_W ** -0.5),
        'ssd_conv_b': nrm((L, SSD_XBC), 0.01),
        'ssd_dt_bias': dt0 + jnp.log(-jnp.expm1(-dt0)),
        'ssd_a_log': jnp.log(a0),
        'ssd_d': 1.0 + nrm((L, SSD_HEADS), 0.02),
        'ssd_norm_g': 1.0 + nrm((L, BRANCH_W), 0.02),
        'w_branch': nrm((L, N_BRANCH, BRANCH_W, D), BRANCH_W ** -0.5 * DN_BETA),
        'w_out': nrm((L, D, D), D ** -0.5 * DN_BETA),
        'ln1_g': 1.0 + nrm((L, D), 0.02),
        'ln1_b': nrm((L, D), 0.01),
        'w_router': nrm((L, D, N_EXPERTS), D ** -0.5),
        'router_bias': nrm((L, N_EXPERTS), 0.01),
        'w_up': nrm((L, N_EXPERTS, D, 2 * EXPERT_F), D ** -0.5),
        'w_down': nrm((L, N_EXPERTS, EXPERT_F, D), EXPERT_F ** -0.5 * DN_BETA),
        'ws_up': nrm((L, D, 2 * SHARED_F), D ** -0.5),
        'ws_down': nrm((L, SHARED_F, D), SHARED_F ** -0.5 * DN_BETA),
        'ln2_g': 1.0 + nrm((L, D), 0.02),
        'ln2_b': nrm((L, D), 0.01),
    }


def reference(x, c, ctx, c_ctx, w_mod, b_mod, w_in, lam_q, lam_k, attn_norm_g,
              lru_conv_w, lru_conv_b, lru_wa, lru_ba, lru_wi, lru_bi, lru_lambda,
              ssd_conv_w, ssd_conv_b, ssd_dt_bias, ssd_a_log, ssd_d, ssd_norm_g,
              w_branch, w_out, ln1_g, ln1_b, w_router, router_bias, w_up, w_down,
              ws_up, ws_down, ln2_g, ln2_b):
    B, L, D = x.shape
    Lc = ctx.shape[1]
    cos, sin = axial_rope(L)
    s_c = jax.nn.silu(c)
    s_cc = jax.nn.silu(c_ctx)
    for l in range(DEPTH):
        last = l == DEPTH - 1
        lam_init = 0.8 - 0.6 * math.exp(-0.3 * l)
        m = jnp.split((s_c @ w_mod[l] + b_mod[l])[:, None, :], 6, axis=-1)
        mc = jnp.split(s_cc @ w_mod[l] + b_mod[l], 6, axis=-1)
        h = x * (1.0 + m[1]) + m[0]
        hc = ctx * (1.0 + mc[1]) + mc[0]
        mix, mix_c = token_mixers(
            h, hc, cos, sin, w_in[l], lam_q[l], lam_k[l], lam_init, attn_norm_g[l],
            lru_conv_w[l], lru_conv_b[l], lru_wa[l], lru_ba[l], lru_wi[l], lru_bi[l], lru_lambda[l],
            ssd_conv_w[l], ssd_conv_b[l], ssd_dt_bias[l], ssd_a_log[l], ssd_d[l], ssd_norm_g[l],
            w_branch[l], w_out[l], not last)
        x = layer_norm(DN_ALPHA * x + m[2] * mix, ln1_g[l], ln1_b[l])
        h2 = (x * (1.0 + m[4]) + m[3]).reshape(B * L, D)
        if last:
            f = moe_ffn(h2, w_router[l], router_bias[l], w_up[l], w_down[l], ws_up[l], ws_down[l])
        else:
            ctx = layer_norm(DN_ALPHA * ctx + mc[2] * mix_c, ln1_g[l], ln1_b[l])
            hc2 = (ctx * (1.0 + mc[4]) + mc[3]).reshape(B * Lc, D)
            f_all = moe_ffn(jnp.concatenate([h2, hc2], axis=0), w_router[l], router_bias[l],
                            w_up[l], w_down[l], ws_up[l], ws_down[l])
            f = f_all[:B * L]
            ctx = layer_norm(DN_ALPHA * ctx + mc[5] * f_all[B * L:].reshape(B, Lc, D), ln2_g[l], ln2_b[l])
        x = layer_norm(DN_ALPHA * x + m[5] * f.reshape(B, L, D), ln2_g[l], ln2_b[l])
    return x
```

```python
import math
from contextlib import ExitStack
import numpy as np
import concourse.bass as bass
import concourse.mybir as mybir
from concourse.bass_utils import run_bass_kernel_spmd

F32 = mybir.dt.float32
U32 = mybir.dt.uint32
BF16 = mybir.dt.bfloat16
AF = mybir.ActivationFunctionType
ALU = mybir.AluOpType
AX = mybir.AxisListType

D = 1024
LAT = 4096
CTX = 256
T = LAT + CTX
NT = T // 128
DEPTH = 2
KC = D // 128
N_MOD = 6 * D
E = 64
EF = 256
DN_ALPHA = (2 * DEPTH) ** 0.25
LN_EPS = 1e-5
RMS_EPS = 1e-6
CHUNKS = [(0, 256)] + [(256 + 512 * i, 512) for i in range(8)]

_W = dict(q=(0, 512), k=(512, 512), v=(1024, 512), lx=(1536, 512), lg=(2048, 512), z=(2560, 512),
          xbc=(3072, 1024), dt=(4096, 16), gates=(4112, 3072))
FM_ROWS = dict(q=0, k=512, lx=1024, lg=1536, xbc=2048, gates=3072)
N_FM = 6144


class TT:
    def __init__(self, ap, name="", multi=False):
        self.ap = ap
        self.name = name
        self.w = None
        self.r = []
        self.multi = multi
        self.wset = {}

    def __getitem__(self, idx):
        return self.ap[idx]


class KB:
    def __init__(self, nc, n_dma_sems=8):
        self.nc = nc
        self.eng = {"pe": nc.tensor, "act": nc.scalar, "dve": nc.vector, "pool": nc.gpsimd, "sp": nc.sync}
        self.sem = {}
        self.cnt = {}
        self.waited = {e: {} for e in self.eng}
        self._ctx = []
        self.allsems = {}
        for e in ("pe", "act", "dve", "pool"):
            cm = nc.semaphore("s_" + e)
            s = cm.__enter__()
            self._ctx.append(cm)
            self.sem[e] = s
            self.cnt[e] = 0
        self.dsem = {}
        for q in ("sp", "act", "pool"):
            lst = []
            for i in range(n_dma_sems):
                cm = nc.semaphore(f"d_{q}{i}")
                s = cm.__enter__()
                self._ctx.append(cm)
                lst.append([s, 0])
            self.dsem[q] = [lst, 0]
        self.n_inst = 0

    def close(self):
        for cm in reversed(self._ctx):
            cm.__exit__(None, None, None)

    def _need(self, e, toks):
        eng = self.eng[e]
        wd = self.waited[e]
        best = {}
        for tok in toks:
            if tok is None:
                continue
            s, v = tok
            key = id(s)
            if wd.get(key, 0) >= v:
                continue
            if key not in best or best[key][1] < v:
                best[key] = (s, v)
        for key, (s, v) in best.items():
            eng.wait_ge(s, v)
            wd[key] = v
            self.n_inst += 1

    def _deps(self, e, reads, writes):
        toks = []
        for t in reads:
            if t.w is None and not t.wset and not getattr(t, "ext", False):
                raise RuntimeError(f"read of {t.name} before any tracked write")
            toks.append(t.w)
            if t.wset:
                toks.extend(t.wset.values())
        for t in writes:
            toks.append(t.w)
            toks.extend(t.r)
            if t.wset and not t.multi:
                toks.extend(t.wset.values())
        if e == "pe":
            own = id(self.sem["pe"])
            toks = [t for t in toks if t is not None and id(t[0]) != own]
        self._need(e, toks)

    def _mark(self, tok, reads, writes):
        for t in reads:
            t.r.append(tok)
            if len(t.r) > 16:
                m = {}
                for s, v in t.r:
                    k = id(s)
                    if k not in m or m[k][1] < v:
                        m[k] = (s, v)
                t.r = list(m.values())
        for t in writes:
            if t.multi:
                kk_ = id(tok[0])
                if kk_ not in t.wset or t.wset[kk_][1] < tok[1]:
                    t.wset[kk_] = tok
            else:
                t.w = tok
                t.wset = {}
            t.r = []

    def op(self, e, fn, reads=(), writes=()):
        self._deps(e, reads, writes)
        ins = fn(self.eng[e])
        self.cnt[e] += 1
        ins.then_inc(self.sem[e], 1)
        tok = (self.sem[e], self.cnt[e])
        self._mark(tok, reads, writes)
        self.n_inst += 1
        return ins

    def dma(self, q, out, in_, reads=(), writes=(), **kw):
        lst = self.dsem[q][0]
        i = self.dsem[q][1]
        self.dsem[q][1] = (i + 1) % len(lst)
        ent = lst[i]
        if ent[1] > 0:
            self._need(q, [(ent[0], ent[1])])
        self._deps(q, reads, writes)
        ins = self.eng[q].dma_start(out=out, in_=in_, **kw)
        ent[1] += 16
        ins.then_inc(ent[0], 16)
        tok = (ent[0], ent[1])
        self._mark(tok, reads, writes)
        self.n_inst += 1
        return ins

    def dma_fn(self, q, fn, reads=(), writes=()):
        lst = self.dsem[q][0]
        i = self.dsem[q][1]
        self.dsem[q][1] = (i + 1) % len(lst)
        ent = lst[i]
        if ent[1] > 0:
            self._need(q, [(ent[0], ent[1])])
        self._deps(q, reads, writes)
        ins = fn(self.eng[q])
        ent[1] += 16
        ins.then_inc(ent[0], 16)
        tok = (ent[0], ent[1])
        self._mark(tok, reads, writes)
        self.n_inst += 1
        return ins

    def barrier(self):
        toks = [(self.sem[e], self.cnt[e]) for e in self.sem if self.cnt[e] > 0]
        for q in self.dsem:
            for s, v in self.dsem[q][0]:
                if v > 0:
                    toks.append((s, v))
        for e in self.eng:
            self._need(e, toks)


class Rot:
    def __init__(self, tiles):
        self.tiles = tiles
        self.i = 0

    def next(self):
        t = self.tiles[self.i]
        self.i = (self.i + 1) % len(self.tiles)
        return t


class Prog:
    def __init__(self, debug=False):
        self.debug = debug
        self.nc = bass.Bass("TRN2", target_bir_lowering=False)
        self.k = None
        self.ins = {}
        self.dbg = {}

    def inp(self, name, shape, dt=F32):
        ap = self.nc.dram_tensor(name, list(shape), dt, kind="ExternalInput").ap()
        self.ins[name] = TT(ap, name)
        self.ins[name].ext = True
        return self.ins[name]

    def scratch(self, name, shape, dt, dump=False, multi=False):
        if dump and self.debug:
            ap = self.nc.dram_tensor(name, list(shape), dt, kind="ExternalOutput").ap()
            self.dbg[name] = True
        else:
            ap = self.nc.dram_tensor(name, list(shape), dt).ap()
        return TT(ap, name, multi=multi)


_UID = [0]


def sbt(nc, es, name, shape, dt):
    _UID[0] += 1
    name = f"{name}_s{_UID[0]}"
    return TT(es.enter_context(nc.sbuf_tensor(name, list(shape), dt)), name)


def pst(nc, es, name, shape, dt=F32):
    _UID[0] += 1
    name = f"{name}_p{_UID[0]}"
    return TT(es.enter_context(nc.psum_tensor(name, list(shape), dt)), name)


def build_program(debug=False, n_layers=DEPTH, stop_after=None, stages=None):
    P = Prog(debug)
    nc = P.nc
    xin = P.inp("xin", [T, D])
    c2 = P.inp("c2", [128, KC, 2])
    ident_in = P.inp("ident", [128, 128])
    w_mod = P.inp("w_mod", [DEPTH, D, N_MOD])
    b_modT = P.inp("b_modT", [DEPTH, 128, 48])
    w_fm = P.inp("w_fm", [DEPTH, 14, 128, KC, 512])
    w_tm = P.inp("w_tm", [DEPTH, 2, 128, KC, 512])
    w_dt = P.inp("w_dt", [DEPTH, 128, KC, 16])
    ropec = P.inp("ropec", [128, T])
    ropes = P.inp("ropes", [128, T])
    lamqk = P.inp("lamqk", [DEPTH, 128, 256])
    attn_gT = P.inp("attn_gT", [DEPTH, 128, 4])
    lru_cw = P.inp("lru_cw", [DEPTH, 128, 4, 4])
    lru_cb = P.inp("lru_cb", [DEPTH, 128, 4])
    lru_wbd = P.inp("lru_wbd", [DEPTH, 128, 16, 128])
    lru_bias = P.inp("lru_bias", [DEPTH, 128, 16])
    lru_lam = P.inp("lru_lam", [DEPTH, 128, 8])
    ssd_cw = P.inp("ssd_cw", [DEPTH, 128, 8, 4])
    ssd_cb = P.inp("ssd_cb", [DEPTH, 128, 8])
    ssd_dtb = P.inp("ssd_dtb", [DEPTH, 128, NT * 16])
    ssd_alog = P.inp("ssd_alog", [DEPTH, 128, NT * 16])
    ssd_dsk = P.inp("ssd_dsk", [DEPTH, 128, 8])
    ssd_gn = P.inp("ssd_gn", [DEPTH, 128, 512])
    tri_in = P.inp("tri", [128, 2, 128])
    maskf_in = P.inp("maskf", [128, 2, 8, 128])
    identb_in = P.inp("identb", [128, 128])
    w_br = P.inp("w_br", [DEPTH, 128, 12, D])
    w_o = P.inp("w_o", [DEPTH, 128, KC, D])
    ln_gb = P.inp("ln_gb", [DEPTH, 4, 128, D])
    w_rt = P.inp("w_rt", [DEPTH, 128, KC, E])
    r_bias = P.inp("r_bias", [DEPTH, 128, E])
    w_upx = P.inp("w_upx", [DEPTH, E + 1, 128, KC, 512])
    w_dnx = P.inp("w_dnx", [DEPTH, E + 1, 128, 2, D])
    iota64 = P.inp("iota64", [128, 64])
    iotap = P.inp("iotap", [128, 1])
    thr_in = P.inp("thr_in", [128, 200])
    ustrict = P.inp("ustrict", [128, 128])
    rowtok_sh = P.inp("rowtok_sh", [128, 34], U32)
    out = TT(nc.dram_tensor("out", [LAT, D], F32, kind="ExternalOutput").ap(), "out")
    xs_full = P.scratch("xs", [T, D], F32, dump=True)
    xs_t = [TT(xs_full.ap[tt * 128:(tt + 1) * 128, :], f"xs{tt}") for tt in range(NT)]
    NBR = 200
    NBS = T // 256
    NBLK = NBR + NBS
    NROWS = NBLK * 256
    H2TM = P.scratch("H2TM", [T, D], BF16, multi=True)
    WUPB = P.scratch("WUPB", [(E + 1) * 128, KC * 512], BF16, multi=True)
    WDNB = P.scratch("WDNB", [(E + 1) * 128, 2 * D], BF16, multi=True)
    ROWTOK = P.scratch("ROWTOK", [NROWS, 1], U32, dump=True)
    OUTB = P.scratch("OUTB", [NROWS, D], BF16, multi=True)
    PF = P.scratch("PF", [N_FM, T], BF16, dump=True, multi=True)
    PV = P.scratch("PVt", [T, 512], BF16, dump=True, multi=True)
    PZ = P.scratch("PZt", [T, 512], BF16, dump=True, multi=True)
    PDT = P.scratch("PDT", [T, 16], F32, dump=True)
    YT = P.scratch("YT", [1536, T], BF16, dump=True, multi=True)

    k = KB(nc)
    P.k = k
    with ExitStack() as es0:
        ident = sbt(nc, es0, "ident_sb", [128, 128], F32)
        mod = sbt(nc, es0, "mod", [128, 48, 2], F32)
        mod1 = sbt(nc, es0, "mod1", [128, 48, 2], F32)
        s2 = sbt(nc, es0, "s2", [128, KC, 2], F32)
        D8 = sbt(nc, es0, "D8", [128, NT, 8], U32)
        W8 = sbt(nc, es0, "W8", [128, NT, 8], F32)
        IDXW = sbt(nc, es0, "IDXW", [128, NBLK], U32)
        idb_g = sbt(nc, es0, "idb0", [128, 128], BF16)
        k.dma("sp", ident[:], ident_in[:], writes=[ident])
        k.op("dve", lambda e: e.tensor_copy(idb_g[:], ident[:]), reads=[ident], writes=[idb_g])
        k.dma("sp", s2[:], c2[:], writes=[s2])
        k.op("act", lambda e: e.activation(s2[:], s2[:], AF.Silu), reads=[s2], writes=[s2])

        for l in range(n_layers):
            with ExitStack() as es:
                wm = Rot([sbt(nc, es, f"wm{i}", [128, N_MOD], F32) for i in range(2)])
                bm = sbt(nc, es, "bm", [128, 48], F32)
                pm = pst(nc, es, "pm", [128, 96])
                k.dma("sp", bm[:], b_modT[l], writes=[bm])
                for kc in range(KC):
                    w = wm.next()
                    k.dma("sp" if kc % 2 == 0 else "pool", w[:], w_mod[l, kc * 128:(kc + 1) * 128, :], writes=[w])
                    for cb in range(48):
                        k.op("pe", lambda e, w=w, cb=cb, kc=kc: e.matmul(
                            pm[:, 2 * cb:2 * cb + 2], w[:, cb * 128:(cb + 1) * 128], s2[:, kc, :],
                            start=(kc == 0 and cb == 0), stop=(kc == KC - 1 and cb == 47), skip_group_check=True),
                            reads=[w, s2], writes=[pm])
                pm3 = pm.ap.rearrange("p (c j) -> p c j", j=2)
                for j in range(2):
                    k.op("dve", lambda e, j=j: e.tensor_tensor(mod[:, :, j], pm3[:, :, j], bm[:], ALU.add),
                         reads=[pm, bm], writes=[mod])
                k.op("dve", lambda e: e.tensor_scalar_add(mod1[:], mod[:], 1.0), reads=[mod], writes=[mod1])
                k.barrier()
            if stop_after == "mod":
                break

            with ExitStack() as es:
                hT = sbt(nc, es, "hT", [128, KC, T], BF16)
                with ExitStack() as es1:
                    xt = Rot([sbt(nc, es1, f"xt{i}", [128, D], F32) for i in range(3)])
                    ptr = Rot([pst(nc, es1, f"ptr{i}", [128, 512]) for i in range(4)])
                    for tt in range(NT):
                        x_t = xt.next()
                        if l == 0:
                            k.dma("sp" if tt % 2 == 0 else "pool", x_t[:], xin[tt * 128:(tt + 1) * 128, :], reads=[xin], writes=[x_t])
                        else:
                            k.dma("sp" if tt % 2 == 0 else "pool", x_t[:], xs_t[tt][:], reads=[xs_t[tt]], writes=[x_t])
                        j = 1 if tt < 2 else 0
                        for half in range(2):
                            p_t = ptr.next()
                            for q4 in range(4):
                                kc = half * 4 + q4
                                k.op("pe", lambda e, p_t=p_t, x_t=x_t, kc=kc, q4=q4: e.transpose(
                                    p_t[:, q4 * 128:(q4 + 1) * 128], x_t[:, kc * 128:(kc + 1) * 128], ident[:]),
                                    reads=[x_t, ident], writes=[p_t])
                            for q4 in range(4):
                                kc = half * 4 + q4
                                eng = "act" if q4 % 2 == 0 else "dve"
                                if eng == "act":
                                    k.op("act", lambda e, p_t=p_t, kc=kc, q4=q4, tt=tt, j=j: e.activation(
                                        hT[:, kc, tt * 128:(tt + 1) * 128], p_t[:, q4 * 128:(q4 + 1) * 128], AF.Identity,
                                        bias=mod[:, kc, j:j + 1], scale=mod1[:, 8 + kc, j:j + 1]),
                                        reads=[p_t, mod, mod1], writes=[hT])
                                else:
                                    k.op("dve", lambda e, p_t=p_t, kc=kc, q4=q4, tt=tt, j=j: e.tensor_scalar(
                                        hT[:, kc, tt * 128:(tt + 1) * 128], p_t[:, q4 * 128:(q4 + 1) * 128],
                                        mod1[:, 8 + kc, j:j + 1], mod[:, kc, j:j + 1], ALU.mult, ALU.add),
                                        reads=[p_t, mod, mod1], writes=[hT])
                    k.barrier()
                with ExitStack() as es2:
                    wf32 = Rot([sbt(nc, es2, f"wf32_{i}", [128, KC, 512], F32) for i in range(2)])
                    wbf = Rot([sbt(nc, es2, f"wbf_{i}", [128, KC, 512], BF16) for i in range(3)])
                    stg = Rot([sbt(nc, es2, f"stg{i}", [128, T], BF16) for i in range(3)])
                    pp = Rot([pst(nc, es2, f"pp{i}", [128, 512]) for i in range(6)])
                    rc = sbt(nc, es2, "rc", [128, T], F32)
                    rs = sbt(nc, es2, "rs", [128, T], F32)
                    tmp = Rot([sbt(nc, es2, f"rtmp{i}", [128, 512], F32) for i in range(4)])
                    k.dma("sp", rc[:], ropec[:], writes=[rc])
                    k.dma("pool", rs[:], ropes[:], writes=[rs])
                    cast_i = [0]

                    def load_w(src_ap, src_t):
                        a = wf32.next()
                        k.dma("sp", a[:], src_ap, reads=[src_t], writes=[a])
                        b = wbf.next()
                        eng = ("pool", "dve")[cast_i[0] % 2]
                        cast_i[0] += 1
                        for h2 in range(2):
                            k.op(eng, lambda e, a=a, b=b, h2=h2: e.tensor_copy(b[:, h2 * 4:(h2 + 1) * 4, :], a[:, h2 * 4:(h2 + 1) * 4, :]),
                                 reads=[a], writes=[b])
                        return b

                    evac_i = [0]

                    def fm_block(wb, row0):
                        for m in range(4):
                            st = stg.next()
                            for (t0, n) in CHUNKS:
                                p_t = pp.next()
                                for kc in range(KC):
                                    k.op("pe", lambda e, p_t=p_t, wb=wb, kc=kc, m=m, t0=t0, n=n: e.matmul(
                                        p_t[:, :n], wb[:, kc, m * 128:(m + 1) * 128], hT[:, kc, t0:t0 + n],
                                        start=(kc == 0), stop=(kc == KC - 1)), reads=[wb, hT], writes=[p_t])
                                eng = ("act", "dve")[evac_i[0] % 2]
                                evac_i[0] += 1
                                if eng == "act":
                                    k.op("act", lambda e, p_t=p_t, st=st, t0=t0, n=n: e.copy(st[:, t0:t0 + n], p_t[:, :n]),
                                         reads=[p_t], writes=[st])
                                else:
                                    k.op("dve", lambda e, p_t=p_t, st=st, t0=t0, n=n: e.tensor_copy(st[:, t0:t0 + n], p_t[:, :n]),
                                         reads=[p_t], writes=[st])
                            k.dma("pool", PF[row0 + m * 128:row0 + (m + 1) * 128, :], st[:], reads=[st], writes=[PF])

                    def rope_block(wb, wbs, row0):
                        for m in range(4):
                            st = stg.next()
                            for (t0, n) in CHUNKS:
                                pa = pp.next()
                                pb = pp.next()
                                for kc in range(KC):
                                    k.op("pe", lambda e, pa=pa, wb=wb, kc=kc, m=m, t0=t0, n=n: e.matmul(
                                        pa[:, :n], wb[:, kc, m * 128:(m + 1) * 128], hT[:, kc, t0:t0 + n],
                                        start=(kc == 0), stop=(kc == KC - 1)), reads=[wb, hT], writes=[pa])
                                for kc in range(KC):
                                    k.op("pe", lambda e, pb=pb, wbs=wbs, kc=kc, m=m, t0=t0, n=n: e.matmul(
                                        pb[:, :n], wbs[:, kc, m * 128:(m + 1) * 128], hT[:, kc, t0:t0 + n],
                                        start=(kc == 0), stop=(kc == KC - 1)), reads=[wbs, hT], writes=[pb])
                                t1 = tmp.next()
                                t2 = tmp.next()
                                k.op("dve", lambda e, pa=pa, t1=t1, t0=t0, n=n: e.tensor_tensor(t1[:, :n], pa[:, :n], rc[:, t0:t0 + n], ALU.mult),
                                     reads=[pa, rc], writes=[t1])
                                k.op("dve", lambda e, pb=pb, t2=t2, t0=t0, n=n: e.tensor_tensor(t2[:, :n], pb[:, :n], rs[:, t0:t0 + n], ALU.mult),
                                     reads=[pb, rs], writes=[t2])
                                k.op("pool", lambda e, t1=t1, t2=t2, st=st, t0=t0, n=n: e.tensor_tensor(st[:, t0:t0 + n], t1[:, :n], t2[:, :n], ALU.add),
                                     reads=[t1, t2], writes=[st])
                            k.dma("pool", PF[row0 + m * 128:row0 + (m + 1) * 128, :], st[:], reads=[st], writes=[PF])

                    for qi, name in enumerate(("q", "k")):
                        wb = load_w(w_fm[l, 2 * qi], w_fm)
                        wbs = load_w(w_fm[l, 2 * qi + 1], w_fm)
                        rope_block(wb, wbs, FM_ROWS[name])
                    for bi in range(4, 14):
                        wb = load_w(w_fm[l, bi], w_fm)
                        fm_block(wb, 1024 + (bi - 4) * 512)
                    stt = Rot([sbt(nc, es2, f"stt{i}", [128, 512], BF16) for i in range(3)])
                    for vi, dst in enumerate((PV, PZ)):
                        wb = load_w(w_tm[l, vi], w_tm)
                        for tt in range(NT):
                            p_t = pp.next()
                            for kc in range(KC):
                                k.op("pe", lambda e, p_t=p_t, wb=wb, kc=kc, tt=tt: e.matmul(
                                    p_t[:], hT[:, kc, tt * 128:(tt + 1) * 128], wb[:, kc, :],
                                    start=(kc == 0), stop=(kc == KC - 1)), reads=[wb, hT], writes=[p_t])
                            st = stt.next()
                            eng = ("act", "dve")[tt % 2]
                            if eng == "act":
                                k.op("act", lambda e, p_t=p_t, st=st: e.copy(st[:], p_t[:]), reads=[p_t], writes=[st])
                            else:
                                k.op("dve", lambda e, p_t=p_t, st=st: e.tensor_copy(st[:], p_t[:]), reads=[p_t], writes=[st])
                            k.dma("pool", dst[tt * 128:(tt + 1) * 128, :], st[:], reads=[st], writes=[dst])
                    wd32 = sbt(nc, es2, "wd32", [128, KC, 16], F32)
                    wdb = sbt(nc, es2, "wdb", [128, KC, 16], BF16)
                    dts = sbt(nc, es2, "dts", [128, NT, 16], F32)
                    k.dma("sp", wd32[:], w_dt[l], writes=[wd32])
                    k.op("dve", lambda e: e.tensor_copy(wdb[:], wd32[:]), reads=[wd32], writes=[wdb])
                    for tt in range(NT):
                        p_t = pp.next()
                        for kc in range(KC):
                            k.op("pe", lambda e, p_t=p_t, kc=kc, tt=tt: e.matmul(
                                p_t[:, :16], hT[:, kc, tt * 128:(tt + 1) * 128], wdb[:, kc, :],
                                start=(kc == 0), stop=(kc == KC - 1)), reads=[wdb, hT], writes=[p_t])
                        k.op("act", lambda e, p_t=p_t, tt=tt: e.copy(dts[:, tt, :], p_t[:, :16]), reads=[p_t], writes=[dts])
                    k.dma("sp", PDT.ap.rearrange("(n p) c -> p n c", p=128), dts[:], reads=[dts], writes=[PDT])
                    k.barrier()
            if stop_after == "proj":
                break


            if stages is None or "attn" in stages:
              with ExitStack() as es:
                lam_init = 0.8 - 0.6 * math.exp(-0.3 * l)
                QT = sbt(nc, es, "QT", [128, 4, T], BF16)
                KT = sbt(nc, es, "KT", [128, 4, T], BF16)
                V = sbt(nc, es, "V", [128, NT, 512], BF16)
                ones_b = sbt(nc, es, "ones_b", [128, 128], BF16)
                ones_f = sbt(nc, es, "ones_f", [128, 128], F32)
                lq = sbt(nc, es, "lq", [128, 256], F32)
                lpr = sbt(nc, es, "lpr", [128, 128], F32)
                lsum = sbt(nc, es, "lsum", [128, 2], F32)
                nlam = sbt(nc, es, "nlam", [128, 1], F32)
                gsc = sbt(nc, es, "gsc", [128, 4], F32)
                for h in range(4):
                    k.dma("sp", QT[:, h, :], PF[h * 128:(h + 1) * 128, :], reads=[PF], writes=[QT])
                    k.dma("pool", KT[:, h, :], PF[512 + h * 128:512 + (h + 1) * 128, :], reads=[PF], writes=[KT])
                pv3 = PV.ap.rearrange("(n p) c -> p n c", p=128)
                for hh in range(2):
                    k.dma("sp", V[:, hh * 17:(hh + 1) * 17, :], pv3[:, hh * 17:(hh + 1) * 17, :], reads=[PV], writes=[V])
                k.op("dve", lambda e: e.memset(ones_b[:], 1.0), writes=[ones_b])
                k.op("dve", lambda e: e.memset(ones_f[:], 1.0), writes=[ones_f])
                k.dma("sp", lq[:], lamqk[l], writes=[lq])
                k.dma("sp", gsc[:], attn_gT[l], writes=[gsc])
                k.op("dve", lambda e: e.tensor_tensor(lpr[:], lq[:, 0:128], lq[:, 128:256], ALU.mult), reads=[lq], writes=[lpr])
                k.op("dve", lambda e: e.reduce_sum(lsum[:], lpr.ap.rearrange("p (a b) -> p a b", a=2), AX.X), reads=[lpr], writes=[lsum])
                k.op("act", lambda e: e.activation(lsum[:], lsum[:], AF.Exp), reads=[lsum], writes=[lsum])
                k.op("dve", lambda e: e.scalar_tensor_tensor(nlam[:], lsum[:, 1:2], -lam_init, lsum[:, 0:1], ALU.add, ALU.subtract),
                     reads=[lsum], writes=[nlam])
                k.op("dve", lambda e: e.tensor_scalar_mul(gsc[:], gsc[:], 1.0 - lam_init), reads=[gsc], writes=[gsc])
                sps = Rot([pst(nc, es, f"sps{i}", [128, 512]) for i in range(4)])
                acc = [pst(nc, es, f"acc{i}", [128, 512]) for i in range(4)]
                pts = Rot([sbt(nc, es, f"pts{i}", [128, 512], BF16) for i in range(4)])
                wk = Rot([sbt(nc, es, f"awk{i}", [128, 512], F32) for i in range(9)])
                yst = Rot([sbt(nc, es, f"yst{i}", [128, 512], BF16) for i in range(2)])
                su = Rot([sbt(nc, es, f"su{i}", [128, KC, 512], F32) for i in range(2)])
                sd = Rot([sbt(nc, es, f"sd{i}", [128, 2, D], F32) for i in range(2)])
                bu = Rot([sbt(nc, es, f"bu{i}", [128, KC * 512], BF16) for i in range(2)])
                bd = Rot([sbt(nc, es, f"bd{i}", [128, 2 * D], BF16) for i in range(2)])

                def precast_gen():
                    for ex in range(E + 1):
                        a_ = su.next(); b_ = sd.next(); c_ = bu.next(); d_ = bd.next()
                        k.dma("sp", a_[:], w_upx[l, ex], writes=[a_])
                        k.dma("sp", b_[:], w_dnx[l, ex], writes=[b_])
                        yield
                        k.op("pool", lambda e, a_=a_, c_=c_: e.tensor_copy(c_[:], a_.ap.rearrange("p a b -> p (a b)")), reads=[a_], writes=[c_])
                        k.op("dve", lambda e, b_=b_, d_=d_: e.tensor_copy(d_[:], b_.ap.rearrange("p a b -> p (a b)")), reads=[b_], writes=[d_])
                        yield
                        k.dma("pool", WUPB[ex * 128:(ex + 1) * 128, :], c_[:], reads=[c_], writes=[WUPB])
                        k.dma("pool", WDNB[ex * 128:(ex + 1) * 128, :], d_[:], reads=[d_], writes=[WDNB])
                        yield

                pcg = precast_gen()
                pc_ctr = [0]

                def precast_tick():
                    pc_ctr[0] += 1
                    if pc_ctr[0] % 5 == 0:
                        next(pcg, None)

                pend_epi = []
                for h in range(4):
                    for (q0, n) in CHUNKS:
                        kts = [0, 1] if q0 == 0 else list(range(NT))

                        def s_mm(kt, h=h, q0=q0, n=n):
                            res = []
                            for m in range(2):
                                sp_ = sps.next()
                                k.op("pe", lambda e, sp_=sp_, m=m, kt=kt: e.matmul(
                                    sp_[:, :n], KT[m * 64:(m + 1) * 64, h, kt * 128:(kt + 1) * 128],
                                    QT[m * 64:(m + 1) * 64, h, q0:q0 + n], start=True, stop=True),
                                    reads=[KT, QT], writes=[sp_])
                                res.append(sp_)
                            return res

                        cur = s_mm(kts[0])
                        for i, kt in enumerate(kts):
                            precast_tick()
                            if (i == 3 or (len(kts) < 4 and i == len(kts) - 1)) and pend_epi:
                                pend_epi.pop(0)()
                            nxt = s_mm(kts[i + 1]) if i + 1 < len(kts) else None
                            for m in range(2):
                                pt = pts.next()
                                sp_ = cur[m]
                                k.op("act", lambda e, pt=pt, sp_=sp_: e.activation(pt[:, :n], sp_[:, :n], AF.Exp, scale=0.125),
                                     reads=[sp_], writes=[pt])
                                k.op("pe", lambda e, pt=pt, m=m, kt=kt, i=i: e.matmul(
                                    acc[2 * m][:, :n], V[:, kt, h * 128:(h + 1) * 128], pt[:, :n],
                                    start=(i == 0), stop=(i == len(kts) - 1)), reads=[V, pt], writes=[acc[2 * m]])
                                k.op("pe", lambda e, pt=pt, m=m, i=i: e.matmul(
                                    acc[2 * m + 1][:, :n], ones_b[:], pt[:, :n],
                                    start=(i == 0), stop=(i == len(kts) - 1)), reads=[ones_b, pt], writes=[acc[2 * m + 1]])
                            cur = nxt
                        rd1, o1s, rd2, o2s, o, sq = [wk.next() for _ in range(6)]
                        k.op("act", lambda e, o1s=o1s: e.copy(o1s[:, :n], acc[0][:, :n]), reads=[acc[0]], writes=[o1s])
                        k.op("dve", lambda e, rd1=rd1: e.reciprocal(rd1[:, :n], acc[1][:, :n]), reads=[acc[1]], writes=[rd1])
                        k.op("act", lambda e, o2s=o2s: e.copy(o2s[:, :n], acc[2][:, :n]), reads=[acc[2]], writes=[o2s])
                        k.op("dve", lambda e, rd2=rd2: e.reciprocal(rd2[:, :n], acc[3][:, :n]), reads=[acc[3]], writes=[rd2])
                        k.op("dve", lambda e, rd1=rd1, o1s=o1s: e.tensor_tensor(o1s[:, :n], o1s[:, :n], rd1[:, :n], ALU.mult),
                             reads=[o1s, rd1], writes=[o1s])
                        k.op("pool", lambda e, rd2=rd2, o2s=o2s: e.tensor_tensor(o2s[:, :n], o2s[:, :n], rd2[:, :n], ALU.mult),
                             reads=[o2s, rd2], writes=[o2s])
                        k.op("dve", lambda e, o=o, o1s=o1s, o2s=o2s: e.scalar_tensor_tensor(
                            o[:, :n], o2s[:, :n], nlam[:, 0:1], o1s[:, :n], ALU.mult, ALU.add), reads=[o1s, o2s, nlam], writes=[o])
                        k.op("pool", lambda e, o=o, sq=sq: e.tensor_tensor(sq[:, :n], o[:, :n], o[:, :n], ALU.mult), reads=[o], writes=[sq])
                        def epi_b(o=o, sq=sq, h=h, q0=q0, n=n):
                            ssp = sps.tiles[sps.i]
                            k.op("pe", lambda e: e.matmul(ssp[:, :n], ones_f[:], sq[:, :n], start=True, stop=True),
                                 reads=[ones_f, sq], writes=[ssp])
                            rs_ = wk.next()
                            k.op("dve", lambda e: e.tensor_scalar(rs_[:, :n], ssp[:, :n], 1.0 / 128, RMS_EPS, ALU.mult, ALU.add),
                                 reads=[ssp], writes=[rs_])
                            k.op("act", lambda e: e.activation(rs_[:, :n], rs_[:, :n], AF.Sqrt), reads=[rs_], writes=[rs_])
                            k.op("dve", lambda e: e.reciprocal(rs_[:, :n], rs_[:, :n]), reads=[rs_], writes=[rs_])
                            ys = yst.next()
                            k.op("dve", lambda e: e.scalar_tensor_tensor(
                                ys[:, :n], o[:, :n], gsc[:, h:h + 1], rs_[:, :n], ALU.mult, ALU.mult), reads=[o, rs_, gsc], writes=[ys])
                            k.dma("sp", YT[h * 128:(h + 1) * 128, q0:q0 + n], ys[:, :n], reads=[ys], writes=[YT])
                        pend_epi.append(epi_b)
                while pend_epi:
                    pend_epi.pop(0)()
                for _ in pcg:
                    pass
                k.barrier()
            if stop_after == "attn":
                break

            if stages is None or "lru" in stages:
              with ExitStack() as es:
                cw = sbt(nc, es, "cw", [128, 4, 4], F32)
                cb = sbt(nc, es, "cb", [128, 4], F32)
                wbd32 = sbt(nc, es, "wbd32", [128, 16, 128], F32)
                wbd = sbt(nc, es, "wbd", [128, 16, 128], BF16)
                lbias = sbt(nc, es, "lbias", [128, 16], F32)
                cch = sbt(nc, es, "cch", [128, 8], F32)
                cch2 = sbt(nc, es, "cch2", [128, 8], F32)
                k.dma("sp", cw[:], lru_cw[l], writes=[cw])
                k.dma("sp", cb[:], lru_cb[l], writes=[cb])
                k.dma("sp", wbd32[:], lru_wbd[l], writes=[wbd32])
                k.dma("sp", lbias[:], lru_bias[l], writes=[lbias])
                k.dma("sp", cch[:], lru_lam[l], writes=[cch])
                k.op("dve", lambda e: e.tensor_copy(wbd[:], wbd32[:]), reads=[wbd32], writes=[wbd])
                k.op("act", lambda e: e.activation(cch[:], cch[:], AF.Exp, scale=-1.0), reads=[cch], writes=[cch])
                k.op("act", lambda e: e.activation(cch[:], cch[:], AF.Ln, bias=1.0), reads=[cch], writes=[cch])
                k.op("dve", lambda e: e.tensor_scalar_mul(cch2[:], cch[:], -16.0), reads=[cch], writes=[cch2])
                k.op("dve", lambda e: e.tensor_scalar_mul(cch[:], cch[:], -8.0), reads=[cch], writes=[cch])
                lxb = sbt(nc, es, "lxb", [128, T], BF16)
                u = sbt(nc, es, "u", [128, T], F32)
                ub = sbt(nc, es, "ub", [128, T], BF16)
                rr2 = [sbt(nc, es, f"rr{d}", [128, T], F32) for d in range(2)]
                ig2 = [sbt(nc, es, f"ig{d}", [128, T], F32) for d in range(2)]
                aa2 = [sbt(nc, es, f"aa{d}", [128, T], F32) for d in range(2)]
                tmp2 = [sbt(nc, es, f"tmpb{d}", [128, T], F32) for d in range(2)]
                yy = sbt(nc, es, "yy", [128, T], F32)
                lgb = ub
                ybo = lxb
                pg = Rot([pst(nc, es, f"pg{i}", [128, 512]) for i in range(6)])
                SEGS = [(0, CTX), (CTX, T)]
                for ct in range(4):
                    k.dma("sp", lxb[:], PF[1024 + ct * 128:1024 + (ct + 1) * 128, :], reads=[PF], writes=[lxb])
                    k.op("dve", lambda e, ct=ct: e.tensor_scalar(u[:], lxb[:], cw[:, ct, 2:3], cb[:, ct:ct + 1], ALU.mult, ALU.add),
                         reads=[lxb, cw, cb], writes=[u])
                    for j in (0, 1, 3):
                        o = j - 2
                        for (s0, s1) in SEGS:
                            a_, b_ = (s0 - o, s1) if o < 0 else (s0, s1 - o)
                            k.op("dve", lambda e, ct=ct, j=j, a_=a_, b_=b_, o=o: e.scalar_tensor_tensor(
                                u[:, a_:b_], lxb[:, a_ + o:b_ + o], cw[:, ct, j:j + 1], u[:, a_:b_], ALU.mult, ALU.add),
                                reads=[lxb, cw, u], writes=[u])
                    k.op("pool", lambda e: e.tensor_copy(ub[:], u[:]), reads=[u], writes=[ub])
                    for d in range(2):
                        rr, ig = rr2[d], ig2[d]
                        ia = (d * 2 + 0) * 4 + ct
                        ii = (d * 2 + 1) * 4 + ct
                        for (t0, n) in CHUNKS:
                            pa = pg.next()
                            pi = pg.next()
                            k.op("pe", lambda e, pa=pa, ia=ia, t0=t0, n=n: e.matmul(pa[:, :n], wbd[:, ia, :], ub[:, t0:t0 + n], start=True, stop=True),
                                 reads=[wbd, ub], writes=[pa])
                            k.op("pe", lambda e, pi=pi, ii=ii, t0=t0, n=n: e.matmul(pi[:, :n], wbd[:, ii, :], ub[:, t0:t0 + n], start=True, stop=True),
                                 reads=[wbd, ub], writes=[pi])
                            k.op("act", lambda e, pa=pa, ia=ia, t0=t0, n=n, rr=rr: e.activation(rr[:, t0:t0 + n], pa[:, :n], AF.Sigmoid, bias=lbias[:, ia:ia + 1]),
                                 reads=[pa, lbias], writes=[rr])
                            k.op("act", lambda e, pi=pi, ii=ii, t0=t0, n=n, ig=ig: e.activation(ig[:, t0:t0 + n], pi[:, :n], AF.Sigmoid, bias=lbias[:, ii:ii + 1]),
                                 reads=[pi, lbias], writes=[ig])
                    k.dma("sp", lgb[:], PF[1536 + ct * 128:1536 + (ct + 1) * 128, :], reads=[PF], writes=[lgb])
                    for d in range(2):
                        rr, ig, aa, tmpb = rr2[d], ig2[d], aa2[d], tmp2[d]
                        dc = d * 4 + ct
                        k.op("act", lambda e, dc=dc, rr=rr, aa=aa: e.activation(aa[:], rr[:], AF.Exp, scale=cch[:, dc:dc + 1]), reads=[rr, cch], writes=[aa])
                        k.op("act", lambda e, dc=dc, rr=rr, tmpb=tmpb: e.activation(tmpb[:], rr[:], AF.Exp, scale=cch2[:, dc:dc + 1]), reads=[rr, cch2], writes=[tmpb])
                    for d in range(2):
                        tmpb = tmp2[d]
                        k.op("act", lambda e, tmpb=tmpb: e.activation(tmpb[:], tmpb[:], AF.Sqrt, bias=1.0, scale=-1.0), reads=[tmpb], writes=[tmpb])
                    for d in range(2):
                        rr, ig, aa, tmpb = rr2[d], ig2[d], aa2[d], tmp2[d]
                        k.op("dve", lambda e, ig=ig: e.tensor_tensor(ig[:], ig[:], u[:], ALU.mult), reads=[ig, u], writes=[ig])
                        k.op("pool", lambda e, ig=ig, tmpb=tmpb: e.tensor_tensor(ig[:], ig[:], tmpb[:], ALU.mult), reads=[ig, tmpb], writes=[ig])
                        dst = yy if d == 0 else rr
                        if d == 0:
                            k.op("dve", lambda e, dst=dst, aa=aa, ig=ig: e.tensor_tensor_scan(dst[:], aa[:], ig[:], 0.0, ALU.mult, ALU.add),
                                 reads=[aa, ig], writes=[dst])
                        else:
                            k.op("dve", lambda e, dst=dst, aa=aa, ig=ig: e.tensor_tensor_scan(dst[:, 0:CTX][:, ::-1], aa[:, 0:CTX][:, ::-1], ig[:, 0:CTX][:, ::-1],
                                                                                             0.0, ALU.mult, ALU.add), reads=[aa, ig], writes=[dst])
                            k.op("dve", lambda e, dst=dst, aa=aa, ig=ig: e.tensor_tensor_scan(dst[:, CTX:T][:, ::-1], aa[:, CTX:T][:, ::-1], ig[:, CTX:T][:, ::-1],
                                                                                             dst[:, 0:1], ALU.mult, ALU.add), reads=[aa, ig, dst], writes=[dst])
                            k.op("pool", lambda e, dst=dst: e.tensor_tensor(yy[:], yy[:], dst[:], ALU.add), reads=[yy, dst], writes=[yy])
                    tmpb = tmp2[0]
                    k.op("act", lambda e: e.activation(tmpb[:], lgb[:], AF.Gelu), reads=[lgb], writes=[tmpb])
                    k.op("dve", lambda e: e.tensor_tensor(ybo[:], yy[:], tmpb[:], ALU.mult), reads=[yy, tmpb], writes=[ybo])
                    k.dma("sp", YT[512 + ct * 128:512 + (ct + 1) * 128, :], ybo[:], reads=[ybo], writes=[YT])
                k.barrier()
            if stop_after == "lru":
                break

            if stages is None or "ssd" in stages:
              with ExitStack() as es:
                tri = sbt(nc, es, "tri", [128, 2, 128], F32)
                ntri = sbt(nc, es, "ntri", [128, 2, 128], F32)
                ones_f = sbt(nc, es, "ones_f2", [128, 128], F32)
                idb32 = sbt(nc, es, "idb32", [128, 128], F32)
                idb = sbt(nc, es, "idb", [128, 128], BF16)
                scw = sbt(nc, es, "scw", [128, 8, 4], F32)
                scb = sbt(nc, es, "scb", [128, 8], F32)
                dtv = sbt(nc, es, "dtv", [128, NT, 16], F32)
                av = sbt(nc, es, "av", [128, NT, 16], F32)
                eal = sbt(nc, es, "eal", [128, NT * 16], F32)
                dsk = sbt(nc, es, "dsk", [128, 8], F32)
                gn = sbt(nc, es, "gn", [128, 512], F32)
                BCT = sbt(nc, es, "BCT", [128, 4, T], BF16)
                Xtm = sbt(nc, es, "Xtm", [128, NT, 512], BF16)
                Btm = sbt(nc, es, "Btm", [128, NT, 256], BF16)
                k.dma("sp", tri[:], tri_in[:], writes=[tri])
                k.dma("sp", idb32[:], identb_in[:], writes=[idb32])
                k.dma("sp", scw[:], ssd_cw[l], writes=[scw])
                k.dma("sp", scb[:], ssd_cb[l], writes=[scb])
                k.dma("sp", dsk[:], ssd_dsk[l], writes=[dsk])
                k.dma("sp", gn[:], ssd_gn[l], writes=[gn])
                k.dma("sp", eal[:], ssd_alog[l], writes=[eal])
                k.dma("sp", av.ap.rearrange("p n c -> p (n c)"), ssd_dtb[l], writes=[av])
                k.dma("sp", dtv[:], PDT.ap.rearrange("(n p) c -> p n c", p=128), reads=[PDT], writes=[dtv])
                k.op("dve", lambda e: e.tensor_scalar_mul(ntri[:], tri[:], -1.0), reads=[tri], writes=[ntri])
                k.op("dve", lambda e: e.memset(ones_f[:], 1.0), writes=[ones_f])
                k.op("dve", lambda e: e.tensor_copy(idb[:], idb32[:]), reads=[idb32], writes=[idb])
                k.op("dve", lambda e: e.tensor_tensor(dtv[:], dtv[:], av[:], ALU.add), reads=[dtv, av], writes=[dtv])
                k.op("act", lambda e: e.activation(dtv[:], dtv[:], AF.Exp), reads=[dtv], writes=[dtv])
                k.op("act", lambda e: e.activation(dtv[:], dtv[:], AF.Ln, bias=1.0), reads=[dtv], writes=[dtv])
                k.op("act", lambda e: e.activation(eal[:], eal[:], AF.Exp), reads=[eal], writes=[eal])
                k.op("dve", lambda e: e.scalar_tensor_tensor(av.ap.rearrange("p n c -> p (n c)"), dtv.ap.rearrange("p n c -> p (n c)"), -1.0,
                                                            eal[:], ALU.mult, ALU.mult), reads=[dtv, eal], writes=[av])
                SEGS = [(0, CTX), (CTX, T)]
                with ExitStack() as es1:
                    XT = sbt(nc, es1, "XT", [128, 4, T], BF16)
                    inb = Rot([sbt(nc, es1, f"cinb{i}", [128, T], BF16) for i in range(2)])
                    uu = Rot([sbt(nc, es1, f"cu{i}", [128, T], F32) for i in range(2)])
                    ptb = Rot([pst(nc, es1, f"ptb{i}", [128, 512], BF16) for i in range(3)])
                    for ct in range(8):
                        ib = inb.next()
                        u = uu.next()
                        k.dma("sp" if ct % 2 == 0 else "pool", ib[:], PF[2048 + ct * 128:2048 + (ct + 1) * 128, :], reads=[PF], writes=[ib])
                        k.op("dve", lambda e, ct=ct, ib=ib, u=u: e.tensor_scalar(u[:], ib[:], scw[:, ct, 2:3], scb[:, ct:ct + 1], ALU.mult, ALU.add),
                             reads=[ib, scw, scb], writes=[u])
                        for j in (0, 1, 3):
                            o = j - 2
                            for (s0, s1) in SEGS:
                                a_, b_ = (s0 - o, s1) if o < 0 else (s0, s1 - o)
                                k.op("dve", lambda e, ct=ct, j=j, a_=a_, b_=b_, o=o, ib=ib, u=u: e.scalar_tensor_tensor(
                                    u[:, a_:b_], ib[:, a_ + o:b_ + o], scw[:, ct, j:j + 1], u[:, a_:b_], ALU.mult, ALU.add),
                                    reads=[ib, scw, u], writes=[u])
                        dst = XT[:, ct, :] if ct < 4 else BCT[:, ct - 4, :]
                        dstt = XT if ct < 4 else BCT
                        k.op("act", lambda e, u=u, dst=dst: e.activation(dst, u[:], AF.Silu), reads=[u], writes=[dstt])
                    for tt in range(NT):
                        p1 = ptb.next()
                        for c4 in range(4):
                            k.op("pe", lambda e, p1=p1, c4=c4, tt=tt: e.transpose(p1[:, c4 * 128:(c4 + 1) * 128], XT[:, c4, tt * 128:(tt + 1) * 128], idb[:]),
                                 reads=[XT, idb], writes=[p1])
                        k.op("dve" if tt % 2 == 0 else "act", (lambda e, p1=p1, tt=tt: e.tensor_copy(Xtm[:, tt, :], p1[:])) if tt % 2 == 0 else
                             (lambda e, p1=p1, tt=tt: e.copy(Xtm[:, tt, :], p1[:])), reads=[p1], writes=[Xtm])
                        p2 = ptb.next()
                        for g in range(2):
                            k.op("pe", lambda e, p2=p2, g=g, tt=tt: e.transpose(p2[:, g * 128:(g + 1) * 128], BCT[:, g, tt * 128:(tt + 1) * 128], idb[:]),
                                 reads=[BCT, idb], writes=[p2])
                        k.op("act" if tt % 2 == 0 else "dve", (lambda e, p2=p2, tt=tt: e.copy(Btm[:, tt, :], p2[:, 0:256])) if tt % 2 == 0 else
                             (lambda e, p2=p2, tt=tt: e.tensor_copy(Btm[:, tt, :], p2[:, 0:256])), reads=[p2], writes=[Btm])
                    k.barrier()
                Yacc = sbt(nc, es, "Yacc", [128, NT, 512], BF16)
                with ExitStack() as es2:
                    S = sbt(nc, es2, "S", [128, 512], F32)
                    Sb = sbt(nc, es2, "Sb", [128, 512], BF16)
                    Rr = Rot([sbt(nc, es2, f"Rr{i}", [128, 8, 128], F32) for i in range(3)])
                    R2 = Rot([sbt(nc, es2, f"R2{i}", [128, 8, 128], F32) for i in range(2)])
                    Lx = Rot([sbt(nc, es2, f"Lx{i}", [128, 8, 128], F32) for i in range(3)])
                    Mt = Rot([sbt(nc, es2, f"Mt{i}", [128, 8, 128], BF16) for i in range(3)])
                    CBm = Rot([sbt(nc, es2, f"CBm{i}", [128, 2, 128], F32) for i in range(3)])
                    xdt = Rot([sbt(nc, es2, f"xdt{i}", [128, 512], BF16) for i in range(3)])
                    wx = Rot([sbt(nc, es2, f"wx{i}", [128, 512], BF16) for i in range(3)])
                    ecs = Rot([sbt(nc, es2, f"ecs{i}", [128, 8], F32) for i in range(4)])
                    decb = Rot([sbt(nc, es2, f"decb{i}", [128, 8], F32) for i in range(4)])
                    yo = Rot([sbt(nc, es2, f"yo{i}", [128, 512], F32) for i in range(2)])
                    PD = pst(nc, es2, "PD", [128, 1024])
                    PCB = pst(nc, es2, "PCB", [128, 512])
                    PYr = Rot([pst(nc, es2, f"PY{i}", [128, 512]) for i in range(2)])
                    PSr = Rot([pst(nc, es2, f"PS{i}", [128, 512]) for i in range(2)])
                    PYo = pst(nc, es2, "PYo", [128, 512])
                    first_y = [True] * NT

                    def ssd_s0(d, tt):
                        lend = 127 if d == 0 else 0
                        a_ap = av[:, tt, d * 8:(d + 1) * 8]
                        tsl = slice(tt * 128, (tt + 1) * 128)
                        r1 = Rr.next(); r2 = R2.next()
                        k.op("dve", lambda e: e.tensor_tensor(r1[:], tri[:, d, :].unsqueeze(1).to_broadcast([128, 8, 128]),
                                                              a_ap.unsqueeze(2).to_broadcast([128, 8, 128]), ALU.mult), reads=[tri, av], writes=[r1])
                        k.op("pool", lambda e: e.tensor_copy(r2[:], a_ap.unsqueeze(2).to_broadcast([128, 8, 128])), reads=[av], writes=[r2])
                        return (d, tt, tsl, lend, a_ap, r1, r2)

                    def ssd_s0m(d, tt, tsl, lend, a_ap, r1, r2):
                        for hf in range(2):
                            k.op("pe", lambda e, hf=hf: e.matmul(PD[:, hf * 512:(hf + 1) * 512], ones_f[:], r1[:, hf * 4:(hf + 1) * 4, :], start=True, stop=False),
                                 reads=[ones_f, r1], writes=[PD])
                            k.op("pe", lambda e, hf=hf: e.matmul(PD[:, hf * 512:(hf + 1) * 512], ntri[:, d, :], r2[:, hf * 4:(hf + 1) * 4, :], start=False, stop=True),
                                 reads=[ntri, r2], writes=[PD])
                        for g in range(2):
                            k.op("pe", lambda e, g=g: e.matmul(PCB[:, g * 128:(g + 1) * 128], BCT[:, g, tsl], BCT[:, 2 + g, tsl], start=True, stop=True),
                                 reads=[BCT], writes=[PCB])
                        k.op("pe", lambda e: e.matmul(PCB[:, 256:264], tri[:, d, :], a_ap, start=True, stop=True), reads=[tri, av], writes=[PCB])
                        k.op("pe", lambda e: e.matmul(PCB[:, 272:280], ones_f[:], a_ap, start=True, stop=True), reads=[ones_f, av], writes=[PCB])
                        return (d, tt, tsl, lend, a_ap)

                    def ssd_a(d, tt, tsl, lend, a_ap):
                        cbm = CBm.next()
                        k.op("dve", lambda e: e.tensor_tensor(cbm[:], PCB.ap[:, 0:256].rearrange("p (g l) -> p g l", g=2),
                                                              tri[:, d, :].unsqueeze(1).to_broadcast([128, 2, 128]), ALU.mult), reads=[PCB, tri], writes=[cbm])
                        lx = Lx.next()
                        k.op("dve", lambda e: e.tensor_tensor(lx[:], PD.ap.rearrange("p (h l) -> p h l", h=8), tri[:, d, :].unsqueeze(1).to_broadcast([128, 8, 128]), ALU.mult),
                             reads=[PD, tri], writes=[lx])
                        k.op("act", lambda e: e.activation(lx[:], lx[:], AF.Exp), reads=[lx], writes=[lx])
                        ec = ecs.next(); db = decb.next()
                        k.op("act", lambda e: e.activation(ec[:], PCB[:, 256:264], AF.Exp), reads=[PCB], writes=[ec])
                        k.op("act", lambda e: e.activation(db[:], PCB[:, 272:280], AF.Exp), reads=[PCB], writes=[db])
                        mt = Mt.next()
                        for g in range(2):
                            k.op("dve", lambda e, g=g: e.tensor_tensor(
                                mt[:, g * 4:(g + 1) * 4, :], lx[:, g * 4:(g + 1) * 4, :], cbm[:, g, :].unsqueeze(1).to_broadcast([128, 4, 128]), ALU.mult),
                                reads=[lx, cbm], writes=[mt])
                        xd = xdt.next()
                        k.op("dve", lambda e: e.tensor_tensor(
                            xd.ap.rearrange("p (h q) -> p h q", h=8), Xtm[:, tt, :].rearrange("p (h q) -> p h q", h=8),
                            dtv[:, tt, d * 8:(d + 1) * 8].unsqueeze(2).to_broadcast([128, 8, 64]), ALU.mult), reads=[Xtm, dtv], writes=[xd])
                        w_ = wx.next()
                        k.op("dve", lambda e: e.tensor_tensor(
                            w_.ap.rearrange("p (h q) -> p h q", h=8), xd.ap.rearrange("p (h q) -> p h q", h=8),
                            lx[:, :, lend:lend + 1].to_broadcast([128, 8, 64]), ALU.mult), reads=[xd, lx], writes=[w_])
                        return (d, tt, tsl, ec, db, mt, xd, w_)

                    def ssd_a2(d, tt, tsl, ec, db, mt, xd, w_):
                        PY = PYr.next(); PS = PSr.next()
                        for h in range(8):
                            k.op("pe", lambda e, h=h: e.matmul(PY[:, h * 64:(h + 1) * 64], mt[:, h, :], xd[:, h * 64:(h + 1) * 64], start=True, stop=True),
                                 reads=[mt, xd], writes=[PY])
                        for g in range(2):
                            k.op("pe", lambda e, g=g: e.matmul(PS[:, g * 256:(g + 1) * 256], Btm[:, tt, g * 128:(g + 1) * 128],
                                                               w_[:, g * 256:(g + 1) * 256], start=True, stop=True), reads=[Btm, w_], writes=[PS])
                        return (d, tt, tsl, ec, db, PY, PS)

                    def ssd_b(d, tt, tsl, ec, db, PY, PS):
                        for g in range(2):
                            k.op("pe", lambda e, g=g: e.matmul(PYo[:, g * 256:(g + 1) * 256], BCT[:, 2 + g, tsl], Sb[:, g * 256:(g + 1) * 256],
                                                               start=True, stop=True), reads=[BCT, Sb], writes=[PYo])
                        y_ = yo.next()
                        k.op("dve", lambda e: e.tensor_tensor(y_.ap.rearrange("p (h q) -> p h q", h=8), PYo.ap.rearrange("p (h q) -> p h q", h=8),
                                                              ec[:].unsqueeze(2).to_broadcast([128, 8, 64]), ALU.mult), reads=[PYo, ec], writes=[y_])
                        if first_y[tt]:
                            first_y[tt] = False
                            k.op("dve", lambda e: e.tensor_tensor(Yacc[:, tt, :], PY[:], y_[:], ALU.add), reads=[PY, y_], writes=[Yacc])
                        else:
                            k.op("pool", lambda e: e.tensor_tensor(Yacc[:, tt, :], Yacc[:, tt, :], y_[:], ALU.add), reads=[Yacc, y_], writes=[Yacc])
                            k.op("dve", lambda e: e.tensor_tensor(Yacc[:, tt, :], Yacc[:, tt, :], PY[:], ALU.add), reads=[Yacc, PY], writes=[Yacc])
                        k.op("dve", lambda e: e.tensor_tensor(S.ap.rearrange("p (h q) -> p h q", h=8), S.ap.rearrange("p (h q) -> p h q", h=8),
                                                              db[:].unsqueeze(2).to_broadcast([128, 8, 64]), ALU.mult), reads=[S, db], writes=[S])
                        k.op("dve", lambda e: e.tensor_tensor(S[:], S[:], PS[:], ALU.add), reads=[S, PS], writes=[S])
                        k.op("pool", lambda e: e.tensor_copy(Sb[:], S[:]), reads=[S], writes=[Sb])

                    for d in range(2):
                        order = list(range(NT)) if d == 0 else [1, 0] + list(range(NT - 1, 1, -1))
                        k.op("dve", lambda e: e.memset(S[:], 0.0), writes=[S])
                        k.op("pool", lambda e: e.memset(Sb[:], 0.0), writes=[Sb])
                        n_o = len(order)
                        q0 = []; qa = []; qb = []
                        for step in range(n_o + 3):
                            s0e = ssd_s0(d, order[step]) if step < n_o else None
                            if 1 <= step <= n_o:
                                qa.append(ssd_a(*q0.pop(0)))
                            if s0e is not None:
                                q0.append(ssd_s0m(*s0e))
                            if 2 <= step <= n_o + 1:
                                qb.append(ssd_a2(*qa.pop(0)))
                            if 3 <= step <= n_o + 2:
                                ssd_b(*qb.pop(0))
                    k.barrier()
                with ExitStack() as es2:
                    zt = Rot([sbt(nc, es2, f"zt{i}", [128, 512], BF16) for i in range(2)])
                    sz = Rot([sbt(nc, es2, f"sz{i}", [128, 512], F32) for i in range(2)])
                    tq = Rot([sbt(nc, es2, f"tq{i}", [128, 512], F32) for i in range(2)])
                    ssq = Rot([sbt(nc, es2, f"ssq{i}", [128, 2], F32) for i in range(2)])
                    ynb = Rot([sbt(nc, es2, f"ynb{i}", [128, 512], BF16) for i in range(2)])
                    yct = Rot([sbt(nc, es2, f"yct{i}", [128, 512], BF16) for i in range(3)])
                    ptc = Rot([pst(nc, es2, f"ptc{i}", [128, 512], BF16) for i in range(2)])
                    for tt in range(NT):
                        z_ = zt.next(); s_ = sz.next(); t_ = tq.next(); q_ = ssq.next(); yn = ynb.next()
                        k.dma("sp", z_[:], PZ[tt * 128:(tt + 1) * 128, :], reads=[PZ], writes=[z_])
                        k.op("act", lambda e, z_=z_, s_=s_: e.activation(s_[:], z_[:], AF.Silu), reads=[z_], writes=[s_])
                        k.op("dve", lambda e, t_=t_, tt=tt: e.tensor_tensor(t_.ap.rearrange("p (h q) -> p h q", h=8), Xtm[:, tt, :].rearrange("p (h q) -> p h q", h=8),
                                                                            dsk[:].unsqueeze(2).to_broadcast([128, 8, 64]), ALU.mult), reads=[Xtm, dsk], writes=[t_])
                        k.op("pool", lambda e, t_=t_, tt=tt: e.tensor_tensor(t_[:], t_[:], Yacc[:, tt, :], ALU.add), reads=[t_, Yacc], writes=[t_])
                        k.op("dve", lambda e, t_=t_, s_=s_: e.tensor_tensor(t_[:], t_[:], s_[:], ALU.mult), reads=[t_, s_], writes=[t_])
                        for g in range(2):
                            k.op("act", lambda e, t_=t_, s_=s_, q_=q_, g=g: e.activation(s_[:, g * 256:(g + 1) * 256], t_[:, g * 256:(g + 1) * 256], AF.Square,
                                                                                         accum_out=q_[:, g:g + 1]), reads=[t_], writes=[s_, q_])
                        k.op("dve", lambda e, q_=q_: e.tensor_scalar(q_[:], q_[:], 1.0 / 256, RMS_EPS, ALU.mult, ALU.add), reads=[q_], writes=[q_])
                        k.op("act", lambda e, q_=q_: e.activation(q_[:], q_[:], AF.Sqrt), reads=[q_], writes=[q_])
                        k.op("dve", lambda e, q_=q_: e.reciprocal(q_[:], q_[:]), reads=[q_], writes=[q_])
                        for g in range(2):
                            k.op("dve", lambda e, t_=t_, q_=q_, yn=yn, g=g: e.scalar_tensor_tensor(
                                yn[:, g * 256:(g + 1) * 256], t_[:, g * 256:(g + 1) * 256], q_[:, g:g + 1], gn[:, g * 256:(g + 1) * 256], ALU.mult, ALU.mult),
                                reads=[t_, q_, gn], writes=[yn])
                        pc = ptc.next()
                        for c4 in range(4):
                            k.op("pe", lambda e, pc=pc, yn=yn, c4=c4: e.transpose(pc[:, c4 * 128:(c4 + 1) * 128], yn[:, c4 * 128:(c4 + 1) * 128], idb[:]),
                                 reads=[yn, idb], writes=[pc])
                        yc_ = yct.next()
                        k.op("act", lambda e, pc=pc, yc_=yc_: e.copy(yc_[:], pc[:]), reads=[pc], writes=[yc_])
                        k.dma("pool", YT.ap[1024:1536, tt * 128:(tt + 1) * 128].rearrange("(c p) t -> p c t", p=128),
                              yc_.ap.rearrange("p (c t) -> p c t", c=4), reads=[yc_], writes=[YT])
                    k.barrier()
            if stop_after == "ssd":
                break

            def rowbc(es_, dst, c0, j, ones_f_, pbc_):
                dg = sbt(nc, es_, "dg", [128, 4, 128], F32)
                for hf in range(2):
                    for c in range(4):
                        k.op("dve", lambda e, c=c, hf=hf: e.tensor_scalar_mul(dg[:, c, :], ident[:], mod[:, c0 + hf * 4 + c, j:j + 1]),
                             reads=[ident, mod], writes=[dg])
                    k.op("pe", lambda e: e.matmul(pbc_[:], ones_f_[:], dg.ap.rearrange("p c q -> p (c q)"), start=True, stop=True),
                         reads=[ones_f_, dg], writes=[pbc_])
                    k.op("act", lambda e, hf=hf: e.copy(dst[:, hf * 512:(hf + 1) * 512], pbc_[:]), reads=[pbc_], writes=[dst])

            def ln_tile(v, junk, st4, gbc, bbc):
                k.op("dve", lambda e: e.reduce_sum(st4[:, 0:1], v[:], AX.X), reads=[v], writes=[st4])
                k.op("dve", lambda e: e.tensor_scalar_mul(st4[:, 1:2], st4[:, 0:1], -1.0 / D), reads=[st4], writes=[st4])
                k.op("act", lambda e: e.activation(junk[:], v[:], AF.Square, bias=st4[:, 1:2], accum_out=st4[:, 2:3]), reads=[v, st4], writes=[junk, st4])
                k.op("dve", lambda e: e.tensor_scalar(st4[:, 3:4], st4[:, 2:3], 1.0 / D, LN_EPS, ALU.mult, ALU.add), reads=[st4], writes=[st4])
                k.op("act", lambda e: e.activation(st4[:, 3:4], st4[:, 3:4], AF.Sqrt), reads=[st4], writes=[st4])
                k.op("dve", lambda e: e.reciprocal(st4[:, 3:4], st4[:, 3:4]), reads=[st4], writes=[st4])
                k.op("dve", lambda e: e.tensor_scalar(v[:], v[:], st4[:, 1:2], st4[:, 3:4], ALU.add, ALU.mult), reads=[v, st4], writes=[v])
                k.op("pool", lambda e: e.tensor_tensor(v[:], v[:], gbc[:], ALU.mult), reads=[v, gbc], writes=[v])
                k.op("pool", lambda e: e.tensor_tensor(v[:], v[:], bbc[:], ALU.add), reads=[v, bbc], writes=[v])

            last = (l == DEPTH - 1)
            if stages is None or "merge" in stages:
              with ExitStack() as es:
                ones_f = sbt(nc, es, "ones_f3", [128, 128], F32)
                k.op("dve", lambda e: e.memset(ones_f[:], 1.0), writes=[ones_f])
                wbr = sbt(nc, es, "wbr", [128, 12, D], BF16)
                wo = sbt(nc, es, "wo", [128, KC, D], BF16)
                g1bc = [sbt(nc, es, f"g1bc{j}", [128, D], F32) for j in range(2)]
                lng = sbt(nc, es, "lng", [128, D], F32)
                lnb = sbt(nc, es, "lnb", [128, D], F32)
                k.dma("sp", lng[:], ln_gb[l, 0], writes=[lng])
                k.dma("sp", lnb[:], ln_gb[l, 1], writes=[lnb])
                with ExitStack() as es1:
                    stg32 = Rot([sbt(nc, es1, f"wstg{i}", [128, 4, D], F32) for i in range(2)])
                    pbc = pst(nc, es1, "pbc", [128, 512])
                    for i in range(3):
                        a = stg32.next()
                        k.dma("sp", a[:], w_br[l, :, i * 4:(i + 1) * 4, :], writes=[a])
                        k.op("dve" if i % 2 == 0 else "pool", lambda e, a=a, i=i: e.tensor_copy(wbr[:, i * 4:(i + 1) * 4, :], a[:]), reads=[a], writes=[wbr])
                    for i in range(2):
                        a = stg32.next()
                        k.dma("sp", a[:], w_o[l, :, i * 4:(i + 1) * 4, :], writes=[a])
                        k.op("pool" if i % 2 == 0 else "dve", lambda e, a=a, i=i: e.tensor_copy(wo[:, i * 4:(i + 1) * 4, :], a[:]), reads=[a], writes=[wo])
                    for j in range(2):
                        rowbc(es1, g1bc[j], 16, j, ones_f, pbc)
                    k.barrier()
                ysb = Rot([sbt(nc, es, f"ysb{i}", [128, 12, 512], BF16) for i in range(2)])
                gsb = Rot([sbt(nc, es, f"gsb{i}", [128, 24, 512], BF16) for i in range(2)])
                mrg = Rot([sbt(nc, es, f"mrg{i}", [128, KC, 512], BF16) for i in range(2)])
                mm = Rot([sbt(nc, es, f"mm{i}", [128, 512], F32) for i in range(6)])
                xv = Rot([sbt(nc, es, f"xv{i}", [128, D], F32) for i in range(3)])
                tv = Rot([sbt(nc, es, f"tv{i}", [128, D], F32) for i in range(3)])
                junk = sbt(nc, es, "junk", [128, D], F32)
                st4 = Rot([sbt(nc, es, f"st4{i}", [128, 8], F32) for i in range(3)])
                pbr = Rot([pst(nc, es, f"pbr{i}", [128, 512]) for i in range(6)])
                pmx = Rot([pst(nc, es, f"pmx{i}", [128, 512]) for i in range(2)])
                def mg_a(t0, n):
                    y_ = ysb.next(); g_ = gsb.next(); mg = mrg.next()
                    k.dma("sp", y_[:, :, :n], YT.ap[:, t0:t0 + n].rearrange("(c p) t -> p c t", p=128), reads=[YT], writes=[y_])
                    k.dma("sp", g_[:, :, :n], PF.ap[3072:6144, t0:t0 + n].rearrange("(c p) t -> p c t", p=128), reads=[PF], writes=[g_])
                    k.op("act", lambda e: e.activation(g_[:, :, :n], g_[:, :, :n], AF.Sigmoid), reads=[g_], writes=[g_])
                    for dt_ in range(8):
                        ps3 = []
                        for br in range(3):
                            pb = pbr.next()
                            for kc in range(4):
                                k.op("pe", lambda e, pb=pb, br=br, kc=kc, dt_=dt_: e.matmul(
                                    pb[:, :n], wbr[:, br * 4 + kc, dt_ * 128:(dt_ + 1) * 128], y_[:, br * 4 + kc, :n],
                                    start=(kc == 0), stop=(kc == 3)), reads=[wbr, y_], writes=[pb])
                            ps3.append(pb)
                        m3 = [mm.next() for _ in range(3)]
                        for br in range(3):
                            k.op("dve", lambda e, br=br, m3=m3, ps3=ps3, dt_=dt_: e.tensor_tensor(
                                m3[br][:, :n], ps3[br][:, :n], g_[:, br * 8 + dt_, :n], ALU.mult), reads=[ps3[br], g_], writes=[m3[br]])
                        k.op("pool", lambda e, m3=m3: e.tensor_tensor(m3[0][:, :n], m3[0][:, :n], m3[1][:, :n], ALU.add), reads=[m3[0], m3[1]], writes=[m3[0]])
                        k.op("dve", lambda e, m3=m3, dt_=dt_: e.tensor_tensor(mg[:, dt_, :n], m3[0][:, :n], m3[2][:, :n], ALU.add),
                             reads=[m3[0], m3[2]], writes=[mg])
                    return (t0, n, mg)

                def mg_b(t0, n, mg):
                    j = 1 if t0 == 0 else 0
                    for tl in range(n // 128):
                        tt = t0 // 128 + tl
                        x_ = xv.next(); v_ = tv.next(); s4 = st4.next()
                        if l == 0:
                            k.dma("sp", x_[:], xin[tt * 128:(tt + 1) * 128, :], reads=[xin], writes=[x_])
                        else:
                            k.dma("sp", x_[:], xs_t[tt][:], reads=[xs_t[tt]], writes=[x_])
                        for eh in range(2):
                            pm_ = pmx.next()
                            for kc in range(KC):
                                k.op("pe", lambda e, pm_=pm_, kc=kc, eh=eh: e.matmul(
                                    pm_[:], mg[:, kc, tl * 128:(tl + 1) * 128], wo[:, kc, eh * 512:(eh + 1) * 512],
                                    start=(kc == 0), stop=(kc == KC - 1)), reads=[mg, wo], writes=[pm_])
                            k.op("dve", lambda e, pm_=pm_, eh=eh: e.tensor_tensor(v_[:, eh * 512:(eh + 1) * 512], pm_[:], g1bc[j][:, eh * 512:(eh + 1) * 512], ALU.mult),
                                 reads=[pm_, g1bc[j]], writes=[v_])
                        k.op("dve", lambda e: e.scalar_tensor_tensor(v_[:], x_[:], DN_ALPHA, v_[:], ALU.mult, ALU.add), reads=[x_, v_], writes=[v_])
                        k.op("dve", lambda e: e.reduce_sum(s4[:, 0:1], v_[:], AX.X), reads=[v_], writes=[s4])
                        k.op("dve", lambda e: e.tensor_scalar_mul(s4[:, 1:2], s4[:, 0:1], -1.0 / D), reads=[s4], writes=[s4])
                        k.op("act", lambda e: e.activation(junk[:], v_[:], AF.Square, bias=s4[:, 1:2], accum_out=s4[:, 2:3]), reads=[v_, s4], writes=[junk, s4])
                        k.op("dve", lambda e: e.tensor_scalar(s4[:, 3:4], s4[:, 2:3], 1.0 / D, LN_EPS, ALU.mult, ALU.add), reads=[s4], writes=[s4])
                        k.op("act", lambda e: e.activation(s4[:, 3:4], s4[:, 3:4], AF.Sqrt), reads=[s4], writes=[s4])
                        k.op("dve", lambda e: e.reciprocal(s4[:, 3:4], s4[:, 3:4]), reads=[s4], writes=[s4])
                        k.op("dve", lambda e: e.tensor_tensor(s4[:, 4:5], s4[:, 1:2], s4[:, 3:4], ALU.mult), reads=[s4], writes=[s4])
                        k.op("act", lambda e: e.activation(v_[:], v_[:], AF.Identity, bias=s4[:, 4:5], scale=s4[:, 3:4]), reads=[v_, s4], writes=[v_])
                        k.op("dve", lambda e: e.tensor_tensor(v_[:], v_[:], lng[:], ALU.mult), reads=[v_, lng], writes=[v_])
                        k.op("pool", lambda e: e.tensor_tensor(v_[:], v_[:], lnb[:], ALU.add), reads=[v_, lnb], writes=[v_])
                        k.dma("pool", xs_t[tt][:], v_[:], reads=[v_], writes=[xs_t[tt]])

                mchs = [c for c in CHUNKS if not (last and c[0] == 0)]
                pendm = mg_a(*mchs[0])
                for im in range(len(mchs)):
                    nxtm = mg_a(*mchs[im + 1]) if im + 1 < len(mchs) else None
                    mg_b(*pendm)
                    pendm = nxtm
                k.barrier()
            if stop_after == "merge":
                break

            if stages is None or "moe" in stages:
              tts = list(range(NT))
              with ExitStack() as es:
                ones_f = sbt(nc, es, "ones_f4", [128, 128], F32)
                k.op("dve", lambda e: e.memset(ones_f[:], 1.0), writes=[ones_f])
                wr = sbt(nc, es, "wr", [128, KC, E], F32)
                rb = sbt(nc, es, "rb", [128, E], F32)
                iot = sbt(nc, es, "iot", [128, 64], F32)
                iop = sbt(nc, es, "iop", [128, 1], F32)
                thr = sbt(nc, es, "thr", [128, NBR], F32)
                ust = sbt(nc, es, "ust", [128, 128], F32)
                EM = sbt(nc, es, "EM", [128, NT, 64], F32)
                WW = sbt(nc, es, "WW", [128, NT, 64], F32)
                IX = sbt(nc, es, "IX", [128, NT, 8], F32)
                sc2 = [sbt(nc, es, f"sc2bc{j}", [128, D], F32) for j in range(2)]
                sh2 = [sbt(nc, es, f"sh2bc{j}", [128, D], F32) for j in range(2)]
                k.dma("sp", wr[:], w_rt[l], writes=[wr])
                k.dma("sp", rb[:], r_bias[l], writes=[rb])
                k.dma("sp", iot[:], iota64[:], writes=[iot])
                k.dma("sp", iop[:], iotap[:], writes=[iop])
                k.dma("sp", thr[:], thr_in[:], writes=[thr])
                k.dma("sp", ust[:], ustrict[:], writes=[ust])
                pbc = pst(nc, es, "pbc3", [128, 512])
                with ExitStack() as es1:
                    for j in range(2):
                        rowbc(es1, sc2[j], 32, j, ones_f, pbc)
                        rowbc(es1, sh2[j], 24, j, ones_f, pbc)
                    for j in range(2):
                        k.op("dve", lambda e, j=j: e.tensor_scalar_add(sc2[j][:], sc2[j][:], 1.0), reads=[sc2[j]], writes=[sc2[j]])
                    k.barrier()
                zt_ = sbt(nc, es, "zt_", [128, NROWS // 128], U32)
                k.op("dve", lambda e: e.memset(zt_[:], 0), writes=[zt_])
                ROWTOK.multi = False
                k.dma("sp", ROWTOK.ap[0:NROWS, :].rearrange("(p a) o -> p (a o)", p=128), zt_[:], reads=[zt_], writes=[ROWTOK])
                rsh = sbt(nc, es, "rsh", [128, NT], U32)
                k.dma("sp", rsh[:], rowtok_sh[:], writes=[rsh])
                k.dma("sp", ROWTOK.ap[NBR * 256:NROWS, :].rearrange("(p a) o -> p (a o)", p=128), rsh[:], reads=[rsh], writes=[ROWTOK])
                k._need("pool", [ROWTOK.w])
                ROWTOK.multi = True
                ROWTOK.wset = {id(ROWTOK.w[0]): ROWTOK.w}
                ROWTOK.w = None
                xt = Rot([sbt(nc, es, f"ext{i}", [128, D], F32) for i in range(4)])
                h2f = Rot([sbt(nc, es, f"h2f{i}", [128, KC, 128], F32) for i in range(3)])
                h2t = Rot([sbt(nc, es, f"h2t{i}", [128, D], F32) for i in range(2)])
                h2b = Rot([sbt(nc, es, f"h2b{i}", [128, D], BF16) for i in range(2)])
                ptr = Rot([pst(nc, es, f"eptr{i}", [128, 512]) for i in range(3)])
                prt = Rot([pst(nc, es, f"prt{i}", [128, 512]) for i in range(2)])
                pcnt = pst(nc, es, "pcnt", [128, 512])
                rw = Rot([sbt(nc, es, f"rw{i}", [128, 64], F32) for i in range(10)])
                r8 = Rot([sbt(nc, es, f"r8{i}", [128, 8], F32) for i in range(9)])
                i8 = Rot([sbt(nc, es, f"i8{i}", [128, 8], U32) for i in range(2)])
                v3 = lambda t_: t_.ap.rearrange("p (g i) -> p g i", g=8)
                for tt in tts:
                    j = 1 if tt < 2 else 0
                    x_ = xt.next(); hf_ = h2f.next(); ht_ = h2t.next(); hb_ = h2b.next()
                    k.dma("sp", x_[:], xs_t[tt][:], reads=[xs_t[tt]], writes=[x_])
                    k.op("pool", lambda e, x_=x_, ht_=ht_, j=j: e.tensor_tensor(ht_[:], x_[:], sc2[j][:], ALU.mult), reads=[x_, sc2[j]], writes=[ht_])
                    k.op("pool", lambda e, hb_=hb_, ht_=ht_, j=j: e.tensor_tensor(hb_[:], ht_[:], sh2[j][:], ALU.add), reads=[ht_, sh2[j]], writes=[hb_])
                    k.dma("pool", H2TM[tt * 128:(tt + 1) * 128, :], hb_[:], reads=[hb_], writes=[H2TM])
                    for half in range(2):
                        p_t = ptr.next()
                        for q4 in range(4):
                            kc = half * 4 + q4
                            k.op("pe", lambda e, p_t=p_t, x_=x_, kc=kc, q4=q4: e.transpose(
                                p_t[:, q4 * 128:(q4 + 1) * 128], x_[:, kc * 128:(kc + 1) * 128], ident[:]), reads=[x_, ident], writes=[p_t])
                        for q4 in range(4):
                            kc = half * 4 + q4
                            if q4 % 2 == 0:
                                k.op("act", lambda e, p_t=p_t, kc=kc, q4=q4, j=j, hf_=hf_: e.activation(
                                    hf_[:, kc, :], p_t[:, q4 * 128:(q4 + 1) * 128], AF.Identity,
                                    bias=mod[:, 24 + kc, j:j + 1], scale=mod1[:, 32 + kc, j:j + 1]), reads=[p_t, mod, mod1], writes=[hf_])
                            else:
                                k.op("dve", lambda e, p_t=p_t, kc=kc, q4=q4, j=j, hf_=hf_: e.tensor_scalar(
                                    hf_[:, kc, :], p_t[:, q4 * 128:(q4 + 1) * 128], mod1[:, 32 + kc, j:j + 1], mod[:, 24 + kc, j:j + 1],
                                    ALU.mult, ALU.add), reads=[p_t, mod, mod1], writes=[hf_])
                    pr = prt.next()
                    for kc in range(KC):
                        k.op("pe", lambda e, pr=pr, hf_=hf_, kc=kc: e.matmul(pr[:, :E], hf_[:, kc, :], wr[:, kc, :], start=(kc == 0), stop=(kc == KC - 1)),
                             reads=[hf_, wr], writes=[pr])
                    sc = rw.next(); sel = rw.next(); eq = rw.next(); sel2 = rw.next(); selm = rw.next()
                    mx1 = r8.next(); mx2 = r8.next(); gs = r8.next(); srt = r8.next(); gm = r8.next(); t8 = r8.next(); sm = r8.next()
                    ix = i8.next()
                    k.op("act", lambda e, sc=sc, pr=pr: e.activation(sc[:], pr[:, :E], AF.Sigmoid), reads=[pr], writes=[sc])
                    k.op("dve", lambda e, sel=sel, sc=sc: e.tensor_tensor(sel[:], sc[:], rb[:], ALU.add), reads=[sc, rb], writes=[sel])
                    k.op("dve", lambda e, mx1=mx1, sel=sel: e.reduce_max(mx1[:], v3(sel), AX.X), reads=[sel], writes=[mx1])
                    k.op("dve", lambda e, eq=eq, sel=sel, mx1=mx1: e.tensor_tensor(v3(eq), v3(sel), mx1[:].unsqueeze(2).to_broadcast([128, 8, 8]), ALU.is_equal),
                         reads=[sel, mx1], writes=[eq])
                    k.op("dve", lambda e, sel2=sel2, eq=eq, sel=sel: e.scalar_tensor_tensor(sel2[:], eq[:], -1e9, sel[:], ALU.mult, ALU.add),
                         reads=[eq, sel], writes=[sel2])
                    k.op("dve", lambda e, mx2=mx2, sel2=sel2: e.reduce_max(mx2[:], v3(sel2), AX.X), reads=[sel2], writes=[mx2])
                    k.op("dve", lambda e, gs=gs, mx1=mx1, mx2=mx2: e.tensor_tensor(gs[:], mx1[:], mx2[:], ALU.add), reads=[mx1, mx2], writes=[gs])
                    k.op("dve", lambda e, srt=srt, gs=gs: e.max(srt[:], gs[:]), reads=[gs], writes=[srt])
                    k.op("dve", lambda e, gm=gm, gs=gs, srt=srt: e.tensor_scalar(gm[:], gs[:], srt[:, 3:4], None, ALU.is_ge), reads=[gs, srt], writes=[gm])
                    k.op("dve", lambda e, selm=selm, sel=sel, gm=gm: e.scalar_tensor_tensor(v3(selm), v3(sel), 10.0, gm[:].unsqueeze(2).to_broadcast([128, 8, 8]),
                                                                                           ALU.add, ALU.mult), reads=[sel, gm], writes=[selm])
                    k.op("dve", lambda e, t8=t8, selm=selm: e.max(t8[:], selm[:]), reads=[selm], writes=[t8])
                    k.op("dve", lambda e, ix=ix, t8=t8, selm=selm: e.max_index(ix[:], t8[:], selm[:]), reads=[selm, t8], writes=[ix])
                    k.op("dve", lambda e, ix=ix, tt=tt: e.tensor_copy(IX[:, tt, :], ix[:]), reads=[ix], writes=[IX])
                    k.op("dve", lambda e, selm=selm, t8=t8, tt=tt: e.tensor_scalar(EM[:, tt, :], selm[:], t8[:, 7:8], None, ALU.is_ge), reads=[selm, t8], writes=[EM])
                    k.op("dve", lambda e, sc=sc, tt=tt: e.tensor_tensor(WW[:, tt, :], sc[:], EM[:, tt, :], ALU.mult), reads=[sc, EM], writes=[WW])
                    k.op("dve", lambda e, sm=sm, tt=tt: e.reduce_sum(sm[:, 0:1], WW[:, tt, :], AX.X), reads=[WW], writes=[sm])
                    k.op("dve", lambda e, sm=sm: e.reciprocal(sm[:, 1:2], sm[:, 0:1]), reads=[sm], writes=[sm])
                    k.op("dve", lambda e, sm=sm, tt=tt: e.tensor_scalar(WW[:, tt, :], WW[:, tt, :], sm[:, 1:2], 2.5, ALU.mult, ALU.mult), reads=[WW, sm], writes=[WW])
                    k.op("pe", lambda e, tt=tt: e.matmul(pcnt[:, 0:64], ones_f[:], EM[:, tt, :], start=(tt == tts[0]), stop=(tt == tts[-1])),
                         reads=[ones_f, EM], writes=[pcnt])
                cnt = sbt(nc, es, "cnt", [128, 64], F32)
                pad = sbt(nc, es, "pad", [128, 64], F32)
                pend = sbt(nc, es, "pend", [128, 64], F32)
                carry = sbt(nc, es, "carry", [128, 64], F32)
                one64 = sbt(nc, es, "one64", [128, 64], F32)
                k.op("dve", lambda e: e.memset(one64[:], 1.0), writes=[one64])
                cmp2 = sbt(nc, es, "cmp2", [128, 64, 18], F32)
                k.op("dve", lambda e: e.tensor_copy(cnt[:], pcnt[:, 0:64]), reads=[pcnt], writes=[cnt])
                k.op("dve", lambda e: e.tensor_tensor(cmp2[:], cnt[:].unsqueeze(2).to_broadcast([128, 64, 18]),
                                                      thr[:, 0:18].unsqueeze(1).to_broadcast([128, 64, 18]), ALU.is_gt), reads=[cnt, thr], writes=[cmp2])
                k.op("dve", lambda e: e.reduce_sum(pad[:], cmp2[:], AX.X), reads=[cmp2], writes=[pad])
                k.op("dve", lambda e: e.tensor_scalar_mul(pad[:], pad[:], 256.0), reads=[pad], writes=[pad])
                k.op("dve", lambda e: e.tensor_tensor_scan(pend[:], one64[:], pad[:], 0.0, ALU.mult, ALU.add), reads=[one64, pad], writes=[pend])
                k.op("dve", lambda e: e.tensor_tensor(carry[:], pend[:], pad[:], ALU.subtract), reads=[pend, pad], writes=[carry])
                BCH = 50
                cmp_ = sbt(nc, es, "cmp_", [128, BCH, 64], F32)
                bke = sbt(nc, es, "bke", [128, NBLK], F32)
                for c in range(NBR // BCH):
                    k.op("dve", lambda e, c=c: e.tensor_tensor(cmp_[:], pend[:].unsqueeze(1).to_broadcast([128, BCH, 64]),
                                                               thr[:, c * BCH:(c + 1) * BCH].unsqueeze(2).to_broadcast([128, BCH, 64]), ALU.is_le),
                         reads=[pend, thr], writes=[cmp_])
                    k.op("dve", lambda e, c=c: e.reduce_sum(bke[:, c * BCH:(c + 1) * BCH], cmp_[:], AX.X), reads=[cmp_], writes=[bke])
                k.op("dve", lambda e: e.memset(bke[:, NBR:NBLK], 64.0), writes=[bke])
                k.op("dve", lambda e: e.tensor_scalar(bke[:, 0:NBR], bke[:, 0:NBR], 63.0, None, ALU.min), reads=[bke], writes=[bke])
                k.op("dve", lambda e: e.tensor_scalar(bke[:], bke[:], 128.0, iop[:, 0:1], ALU.mult, ALU.add), reads=[bke, iop], writes=[bke])
                k.op("dve", lambda e: e.tensor_copy(IDXW[:], bke[:]), reads=[bke], writes=[IDXW])
                tokid = sbt(nc, es, "tokid", [128, 1], U32)
                tokf = sbt(nc, es, "tokf", [128, 1], F32)
                dfu = Rot([sbt(nc, es, f"dfu{i}", [128, 64], F32) for i in range(2)])
                junk64 = sbt(nc, es, "junk64", [128, 64], F32)
                d8f = Rot([sbt(nc, es, f"d8f{i}", [128, 8], F32) for i in range(2)])
                ppf = prt
                for tt in tts:
                    pp_ = ppf.next(); df = dfu.next(); d8 = d8f.next()
                    k.op("pe", lambda e, pp_=pp_, tt=tt: e.matmul(pp_[:, 0:64], ust[:], EM[:, tt, :], start=True, stop=True), reads=[ust, EM], writes=[pp_])
                    k.op("pe", lambda e, pp_=pp_, tt=tt: e.matmul(pp_[:, 64:128], ones_f[:], EM[:, tt, :], start=True, stop=True), reads=[ones_f, EM], writes=[pp_])
                    k.op("dve", lambda e, pp_=pp_, df=df: e.tensor_tensor(df[:], pp_[:, 0:64], carry[:], ALU.add), reads=[pp_, carry], writes=[df])
                    k.op("dve", lambda e, pp_=pp_: e.tensor_tensor(carry[:], carry[:], pp_[:, 64:128], ALU.add), reads=[pp_, carry], writes=[carry])
                    k.op("dve", lambda e, d8=d8: e.memset(d8[:], 0.0), writes=[d8])
                    k.op("dve", lambda e, tt=tt: e.memset(W8[:, tt, :], 0.0), writes=[W8])
                    for kk in range(8):
                        k.op("dve", lambda e, df=df, d8=d8, tt=tt, kk=kk: e.scalar_tensor_tensor(
                            junk64[:], iot[:], IX[:, tt, kk:kk + 1], df[:], ALU.is_equal, ALU.mult, accum_out=d8[:, kk:kk + 1]),
                            reads=[iot, IX, df], writes=[junk64, d8])
                        k.op("dve", lambda e, tt=tt, kk=kk: e.scalar_tensor_tensor(
                            junk64[:], iot[:], IX[:, tt, kk:kk + 1], WW[:, tt, :], ALU.is_equal, ALU.mult, accum_out=W8[:, tt, kk:kk + 1]),
                            reads=[iot, IX, WW], writes=[junk64, W8])
                    k.op("dve", lambda e, d8=d8, tt=tt: e.tensor_copy(D8[:, tt, :], d8[:]), reads=[d8], writes=[D8])
                    k.op("dve", lambda e, tt=tt: e.tensor_scalar_add(tokf[:], iop[:], float(tt * 128)), reads=[iop], writes=[tokf])
                    k.op("dve", lambda e: e.tensor_copy(tokid[:], tokf[:]), reads=[tokf], writes=[tokid])
                    for kk in range(8):
                        k.dma_fn("pool", lambda e, tt=tt, kk=kk: e.indirect_dma_start(
                            out=ROWTOK[:], out_offset=bass.IndirectOffsetOnAxis(ap=D8[:, tt, kk:kk + 1], axis=0), in_=tokid[:], in_offset=None),
                            reads=[tokid, D8], writes=[ROWTOK])
                k.barrier()
              if stop_after == "route":
                break
              with ExitStack() as es:
                rtok = sbt(nc, es, "rtok", [128, 2 * NBLK], U32)
                rtc = [TT(rtok.ap[:, rj:rj + 1], f"rtc{rj}") for rj in range(2 * NBLK)]
                for rj in range(2 * NBLK):
                    k.dma("sp", rtc[rj][:], ROWTOK[rj * 128:(rj + 1) * 128, :], reads=[ROWTOK], writes=[rtc[rj]])
                wub = Rot([sbt(nc, es, f"wub{i}", [128, KC, 512], BF16) for i in range(4)])
                wdb = Rot([sbt(nc, es, f"wdb{i}", [128, 2, D], BF16) for i in range(4)])
                hg = Rot([sbt(nc, es, f"hg{i}", [128, D], BF16) for i in range(4)])
                hgT = Rot([sbt(nc, es, f"hgT{i}", [128, KC, 128], BF16) for i in range(3)])
                sg = Rot([sbt(nc, es, f"sg{i}", [128, 256], F32) for i in range(2)])
                hid = Rot([sbt(nc, es, f"hid{i}", [128, 256], BF16) for i in range(4)])
                hidT = Rot([sbt(nc, es, f"hidT{i}", [128, 2, 128], BF16) for i in range(3)])
                osb = Rot([sbt(nc, es, f"osb{i}", [128, D], BF16) for i in range(3)])
                ptg = Rot([pst(nc, es, f"ptg{i}", [128, 1024], BF16) for i in range(2)])
                pup = Rot([pst(nc, es, f"pup{i}", [128, 512]) for i in range(2)])
                pht = pst(nc, es, "pht", [128, 1024], BF16)
                pdn = [pst(nc, es, f"pdn{i}", [128, 512]) for i in range(2)]
                def ph_t8(rj):
                    g_ = hg.next()
                    k.dma_fn("pool", lambda e: e.indirect_dma_start(
                        out=g_[:], out_offset=None, in_=H2TM[:], in_offset=bass.IndirectOffsetOnAxis(ap=rtok[:, rj:rj + 1], axis=0)),
                        reads=[H2TM, rtc[rj]], writes=[g_])
                    pt_ = ptg.next(); gT = hgT.next()
                    for kc in range(KC):
                        k.op("pe", lambda e, kc=kc: e.transpose(pt_[:, kc * 128:(kc + 1) * 128], g_[:, kc * 128:(kc + 1) * 128], idb_g[:]),
                             reads=[g_, idb_g], writes=[pt_])
                    k.op("act", lambda e: e.copy(gT[:, 0:4, :], pt_.ap[:, 0:512].rearrange("p (a c) -> p a c", a=4)), reads=[pt_], writes=[gT])
                    k.op("dve", lambda e: e.tensor_copy(gT[:, 4:8, :], pt_.ap[:, 512:1024].rearrange("p (a c) -> p a c", a=4)), reads=[pt_], writes=[gT])
                    return gT

                def ph_up(gT, wu_):
                    pu_ = pup.next()
                    for kc in range(KC):
                        k.op("pe", lambda e, kc=kc: e.matmul(pu_[:], gT[:, kc, :], wu_[:, kc, :], start=(kc == 0), stop=(kc == KC - 1)),
                             reads=[gT, wu_], writes=[pu_])
                    s_ = sg.next(); h_ = hid.next()
                    k.op("act", lambda e: e.activation(s_[:], pu_[:, 0:256], AF.Silu), reads=[pu_], writes=[s_])
                    k.op("dve", lambda e: e.tensor_tensor(h_[:], s_[:], pu_[:, 256:512], ALU.mult), reads=[s_, pu_], writes=[h_])
                    return h_

                def ph_t2(h_):
                    hT = hidT.next()
                    for fh in range(2):
                        k.op("pe", lambda e, fh=fh: e.transpose(pht[:, fh * 128:(fh + 1) * 128], h_[:, fh * 128:(fh + 1) * 128], idb_g[:]),
                             reads=[h_, idb_g], writes=[pht])
                    k.op("act", lambda e: e.copy(hT[:], pht.ap[:, 0:256].rearrange("p (a c) -> p a c", a=2)), reads=[pht], writes=[hT])
                    return hT

                def ph_dn(rj, hT, wd_):
                    for dh in range(2):
                        for fh in range(2):
                            k.op("pe", lambda e, dh=dh, fh=fh: e.matmul(pdn[dh][:], hT[:, fh, :], wd_[:, fh, dh * 512:(dh + 1) * 512],
                                                                        start=(fh == 0), stop=(fh == 1)), reads=[hT, wd_], writes=[pdn[dh]])
                    o_ = osb.next()
                    k.op("act", lambda e: e.copy(o_[:, 0:512], pdn[0][:]), reads=[pdn[0]], writes=[o_])
                    k.op("dve", lambda e: e.tensor_copy(o_[:, 512:1024], pdn[1][:]), reads=[pdn[1]], writes=[o_])
                    k.dma("sp", OUTB[rj * 128:(rj + 1) * 128, :], o_[:], reads=[o_], writes=[OUTB])

                NSUB = 2 * NBLK
                wts = {}
                st_gT = {}; st_h = {}; st_hT = {}
                for step in range(NSUB + 3):
                    j = step
                    if j < NSUB:
                        if j % 2 == 0:
                            b = j // 2
                            wu_ = wub.next(); wd_ = wdb.next()
                            k.dma_fn("pool", lambda e, wu_=wu_, b=b: e.indirect_dma_start(
                                out=wu_.ap.rearrange("p a c -> p (a c)"), out_offset=None, in_=WUPB[:],
                                in_offset=bass.IndirectOffsetOnAxis(ap=IDXW[:, b:b + 1], axis=0)), reads=[WUPB, IDXW], writes=[wu_])
                            k.dma_fn("pool", lambda e, wd_=wd_, b=b: e.indirect_dma_start(
                                out=wd_.ap.rearrange("p a c -> p (a c)"), out_offset=None, in_=WDNB[:],
                                in_offset=bass.IndirectOffsetOnAxis(ap=IDXW[:, b:b + 1], axis=0)), reads=[WDNB, IDXW], writes=[wd_])
                            wts[b] = (wu_, wd_)
                        st_gT[j] = ph_t8(j)
                    j1 = step - 1
                    if 0 <= j1 < NSUB:
                        st_h[j1] = ph_up(st_gT.pop(j1), wts[j1 // 2][0])
                    j2 = step - 2
                    if 0 <= j2 < NSUB:
                        st_hT[j2] = ph_t2(st_h.pop(j2))
                    j3 = step - 3
                    if 0 <= j3 < NSUB:
                        ph_dn(j3, st_hT.pop(j3), wts[j3 // 2][1])
                        if j3 % 2 == 1:
                            wts.pop(j3 // 2)
                k.barrier()
              last_ = last
              with ExitStack() as es:
                ones_f = sbt(nc, es, "ones_f5", [128, 128], F32)
                k.op("dve", lambda e: e.memset(ones_f[:], 1.0), writes=[ones_f])
                g2bc = [sbt(nc, es, f"g2bc{j}", [128, D], F32) for j in range(2)]
                lng = sbt(nc, es, "lng2", [128, D], F32)
                lnb = sbt(nc, es, "lnb2", [128, D], F32)
                k.dma("sp", lng[:], ln_gb[l, 2], writes=[lng])
                k.dma("sp", lnb[:], ln_gb[l, 3], writes=[lnb])
                pbc = pst(nc, es, "pbc2", [128, 512])
                with ExitStack() as es1:
                    for j in range(2):
                        rowbc(es1, g2bc[j], 40, j, ones_f, pbc)
                    k.barrier()
                fp_ = Rot([sbt(nc, es, f"fp{i}", [128, D], BF16) for i in range(36)])
                fa = Rot([sbt(nc, es, f"fa{i}", [128, D], F32) for i in range(4)])
                xv = Rot([sbt(nc, es, f"xv2{i}", [128, D], F32) for i in range(4)])
                junk = sbt(nc, es, "junk2", [128, D], F32)
                st4 = Rot([sbt(nc, es, f"st42{i}", [128, 4], F32) for i in range(2)])
                def ln_tile_nopool(v, s4):
                    k.op("dve", lambda e: e.reduce_sum(s4[:, 0:1], v[:], AX.X), reads=[v], writes=[s4])
                    k.op("dve", lambda e: e.tensor_scalar_mul(s4[:, 1:2], s4[:, 0:1], -1.0 / D), reads=[s4], writes=[s4])
                    k.op("act", lambda e: e.activation(junk[:], v[:], AF.Square, bias=s4[:, 1:2], accum_out=s4[:, 2:3]), reads=[v, s4], writes=[junk, s4])
                    k.op("dve", lambda e: e.tensor_scalar(s4[:, 3:4], s4[:, 2:3], 1.0 / D, LN_EPS, ALU.mult, ALU.add), reads=[s4], writes=[s4])
                    k.op("act", lambda e: e.activation(s4[:, 3:4], s4[:, 3:4], AF.Sqrt), reads=[s4], writes=[s4])
                    k.op("dve", lambda e: e.reciprocal(s4[:, 3:4], s4[:, 3:4]), reads=[s4], writes=[s4])
                    k.op("dve", lambda e: e.tensor_tensor(s4[:, 4:5], s4[:, 1:2], s4[:, 3:4], ALU.mult), reads=[s4], writes=[s4])
                    k.op("act", lambda e: e.activation(v[:], v[:], AF.Identity, bias=s4[:, 4:5], scale=s4[:, 3:4]), reads=[v, s4], writes=[v])
                    k.op("dve", lambda e: e.tensor_tensor(v[:], v[:], lng[:], ALU.mult), reads=[v, lng], writes=[v])
                    k.op("dve", lambda e: e.tensor_tensor(v[:], v[:], lnb[:], ALU.add), reads=[v, lnb], writes=[v])

                st5 = Rot([sbt(nc, es, f"st5{i}", [128, 8], F32) for i in range(3)])
                pacc = Rot([pst(nc, es, f"pacc{i}", [128, 512]) for i in range(4)])
                dgs = Rot([sbt(nc, es, f"dgs{i}", [128, 128], BF16) for i in range(6)])

                def e3_a0(tt):
                    acc_ = fa.next(); x_ = xv.next()
                    k.dma("sp", x_[:], xs_t[tt][:], reads=[xs_t[tt]], writes=[x_])
                    psh = fp_.next()
                    k.dma("sp", psh[:], OUTB[NBR * 256 + tt * 128:NBR * 256 + (tt + 1) * 128, :], reads=[OUTB], writes=[psh])
                    ps_ = []
                    for kk in range(8):
                        p_ = fp_.next()
                        k.dma_fn("pool", lambda e, p_=p_, kk=kk: e.indirect_dma_start(
                            out=p_[:], out_offset=None, in_=OUTB[:], in_offset=bass.IndirectOffsetOnAxis(ap=D8[:, tt, kk:kk + 1], axis=0)),
                            reads=[OUTB, D8], writes=[p_])
                        ps_.append(p_)
                    return (tt, acc_, x_, psh, ps_)

                def e3_a1(tt, acc_, x_, psh, ps_):
                    j = 1 if tt < 2 else 0
                    pa = [pacc.next(), pacc.next()]
                    for kk in range(8):
                        dg_ = dgs.next()
                        p_ = ps_[kk]
                        k.op("dve", lambda e, dg_=dg_, kk=kk: e.tensor_scalar_mul(dg_[:], idb_g[:], W8[:, tt, kk:kk + 1]), reads=[idb_g, W8], writes=[dg_])
                        for hf in range(2):
                            k.op("pe", lambda e, dg_=dg_, p_=p_, hf=hf, kk=kk: e.matmul(pa[hf][:], dg_[:], p_[:, hf * 512:(hf + 1) * 512],
                                                                                       start=(kk == 0), stop=False), reads=[dg_, p_], writes=[pa[hf]])
                    for hf in range(2):
                        k.op("pe", lambda e, hf=hf: e.matmul(pa[hf][:], idb_g[:], psh[:, hf * 512:(hf + 1) * 512], start=False, stop=True),
                             reads=[idb_g, psh], writes=[pa[hf]])
                        k.op("dve", lambda e, hf=hf: e.tensor_tensor(acc_[:, hf * 512:(hf + 1) * 512], pa[hf][:], g2bc[j][:, hf * 512:(hf + 1) * 512], ALU.mult),
                             reads=[pa[hf], g2bc[j]], writes=[acc_])
                    k.op("dve", lambda e: e.scalar_tensor_tensor(acc_[:], x_[:], DN_ALPHA, acc_[:], ALU.mult, ALU.add), reads=[x_, acc_], writes=[acc_])
                    return (tt, acc_)

                def e3_b(tt, acc_):
                    s4 = st5.next()
                    ln_tile_nopool(acc_, s4)
                    if last_:
                        k.dma("sp", out[(tt - 2) * 128:(tt - 1) * 128, :], acc_[:], reads=[acc_], writes=[out])
                    else:
                        k.dma("sp", xs_t[tt][:], acc_[:], reads=[acc_], writes=[xs_t[tt]])

                e3_tiles = list(range(2, NT)) if last_ else tts
                n3 = len(e3_tiles)
                q0_ = [e3_a0(e3_tiles[0]), e3_a0(e3_tiles[1])]
                q1_ = [e3_a1(*q0_.pop(0))]
                for i3 in range(n3):
                    if i3 + 2 < n3:
                        q0_.append(e3_a0(e3_tiles[i3 + 2]))
                    if q0_:
                        q1_.append(e3_a1(*q0_.pop(0)))
                    e3_b(*q1_.pop(0))
                k.barrier()
            if stop_after == "moe":
                break
        k.barrier()
        k.close()
    return P


def _rope_tables():
    rows = LAT // 64
    row = np.repeat(np.arange(rows, dtype=np.float32), 64)
    col = (np.arange(LAT) % 64).astype(np.float32)
    inv = (10000.0 ** (-np.arange(16, dtype=np.float32) / 16)).astype(np.float32)
    ang = np.concatenate([row[:, None] * inv, col[:, None] * inv], axis=-1)
    cos = np.cos(ang).astype(np.float32)
    sin = np.sin(ang).astype(np.float32)
    C = np.ones((128, T), np.float32)
    S = np.zeros((128, T), np.float32)
    for r in range(128):
        j = r % 64
        C[r, CTX:] = cos[:, j % 32]
        S[r, CTX:] = -sin[:, j] if j < 32 else sin[:, j - 32]
    return C, S


def _swap_cols(w):
    idx = np.arange(512)
    blk = idx // 64
    j = idx % 64
    return w[:, blk * 64 + (j + 32) % 64]


def _blk(w):
    return np.ascontiguousarray(w.reshape(KC, 128, w.shape[1]).transpose(1, 0, 2))


def prep_shared(inputs):
    w_in = inputs["w_in"]
    sh = {}
    fm = np.empty((DEPTH, 14, 128, KC, 512), np.float32)
    tm = np.empty((DEPTH, 2, 128, KC, 512), np.float32)
    wdt = np.empty((DEPTH, 128, KC, 16), np.float32)
    for l in range(DEPTH):
        w = w_in[l]
        sl = lambda nm: w[:, _W[nm][0]:_W[nm][0] + _W[nm][1]]
        blocks = [sl("q"), _swap_cols(sl("q")), sl("k"), _swap_cols(sl("k")), sl("lx"), sl("lg"),
                  sl("xbc")[:, :512], sl("xbc")[:, 512:]] + [sl("gates")[:, i * 512:(i + 1) * 512] for i in range(6)]
        for i, b in enumerate(blocks):
            fm[l, i] = _blk(b)
        tm[l, 0] = _blk(sl("v"))
        tm[l, 1] = _blk(sl("z"))
        wdt[l] = _blk(sl("dt"))
    sh["w_fm"] = fm
    sh["w_tm"] = tm
    sh["w_dt"] = wdt
    sh["w_mod"] = np.ascontiguousarray(inputs["w_mod"])
    sh["b_modT"] = np.ascontiguousarray(inputs["b_mod"].reshape(DEPTH, 48, 128).transpose(0, 2, 1))
    lqk = np.concatenate([inputs["lam_q"].reshape(DEPTH, 1, 128), inputs["lam_k"].reshape(DEPTH, 1, 128)], axis=2)
    sh["lamqk"] = np.ascontiguousarray(np.broadcast_to(lqk, (DEPTH, 128, 256))).astype(np.float32)
    sh["attn_gT"] = np.ascontiguousarray(inputs["attn_norm_g"].transpose(0, 2, 1))
    L_ = DEPTH
    sh["lru_cw"] = np.ascontiguousarray(inputs["lru_conv_w"].reshape(L_, 4, 4, 128).transpose(0, 3, 2, 1))
    sh["lru_cb"] = np.ascontiguousarray(inputs["lru_conv_b"].reshape(L_, 4, 128).transpose(0, 2, 1))
    wbd = np.zeros((L_, 128, 16, 128), np.float32)
    for l in range(L_):
        for d in range(2):
            for ai, nm in enumerate(("lru_wa", "lru_wi")):
                for ct in range(4):
                    idx = (d * 2 + ai) * 4 + ct
                    for bb in range(2):
                        wbd[l, bb * 64:(bb + 1) * 64, idx, bb * 64:(bb + 1) * 64] = inputs[nm][l, d, 2 * ct + bb]
    sh["lru_wbd"] = wbd
    lb = np.stack([inputs["lru_ba"], inputs["lru_bi"]], axis=2)
    sh["lru_bias"] = np.ascontiguousarray(lb.reshape(L_, 2, 2, 4, 128).transpose(0, 4, 1, 2, 3).reshape(L_, 128, 16))
    sh["lru_lam"] = np.ascontiguousarray(inputs["lru_lambda"].reshape(L_, 2, 4, 128).transpose(0, 3, 1, 2).reshape(L_, 128, 8))
    sh["ssd_cw"] = np.ascontiguousarray(inputs["ssd_conv_w"].reshape(L_, 4, 8, 128).transpose(0, 3, 2, 1))
    sh["ssd_cb"] = np.ascontiguousarray(inputs["ssd_conv_b"].reshape(L_, 8, 128).transpose(0, 2, 1))
    sh["ssd_dtb"] = np.ascontiguousarray(np.broadcast_to(inputs["ssd_dt_bias"].reshape(L_, 1, 1, 16), (L_, 128, NT, 16)).reshape(L_, 128, NT * 16))
    sh["ssd_alog"] = np.ascontiguousarray(np.broadcast_to(inputs["ssd_a_log"].reshape(L_, 1, 1, 16), (L_, 128, NT, 16)).reshape(L_, 128, NT * 16))
    sh["ssd_dsk"] = np.ascontiguousarray(np.broadcast_to(inputs["ssd_d"].reshape(L_, 1, 8), (L_, 128, 8)))
    sh["ssd_gn"] = np.ascontiguousarray(np.broadcast_to(inputs["ssd_norm_g"].reshape(L_, 1, 512), (L_, 128, 512)))
    kk_, ll_ = np.meshgrid(np.arange(128), np.arange(128), indexing="ij")
    tri = np.stack([(kk_ <= ll_), (kk_ >= ll_)], axis=1).astype(np.float32)
    sh["tri"] = np.ascontiguousarray(tri)
    sh["maskf"] = np.ascontiguousarray(np.broadcast_to(tri[:, :, None, :], (128, 2, 8, 128)))
    sh["identb"] = np.eye(128, dtype=np.float32)
    sh["w_br"] = np.ascontiguousarray(inputs["w_branch"].reshape(L_, 12, 128, D).transpose(0, 2, 1, 3))
    sh["w_o"] = np.ascontiguousarray(inputs["w_out"].reshape(L_, KC, 128, D).transpose(0, 2, 1, 3))
    lngb = np.stack([inputs["ln1_g"], inputs["ln1_b"], inputs["ln2_g"], inputs["ln2_b"]], axis=1)
    sh["ln_gb"] = np.ascontiguousarray(np.broadcast_to(lngb[:, :, None, :], (L_, 4, 128, D)))
    sh["w_rt"] = np.ascontiguousarray(inputs["w_router"].reshape(L_, KC, 128, E).transpose(0, 2, 1, 3))
    sh["r_bias"] = np.ascontiguousarray(np.broadcast_to(inputs["router_bias"][:, None, :], (L_, 128, E)))
    wu = np.concatenate([inputs["w_up"], inputs["ws_up"][:, None]], axis=1)
    sh["w_upx"] = np.ascontiguousarray(wu.reshape(L_, E + 1, KC, 128, 512).transpose(0, 1, 3, 2, 4))
    wd = np.concatenate([inputs["w_down"], inputs["ws_down"][:, None]], axis=1)
    sh["w_dnx"] = np.ascontiguousarray(wd.reshape(L_, E + 1, 2, 128, D).transpose(0, 1, 3, 2, 4))
    sh["iota64"] = np.ascontiguousarray(np.broadcast_to(np.arange(64, dtype=np.float32)[None, :], (128, 64)))
    sh["iotap"] = np.arange(128, dtype=np.float32).reshape(128, 1)
    sh["thr_in"] = np.ascontiguousarray(np.broadcast_to((256.0 * np.arange(200, dtype=np.float32))[None, :], (128, 200)))
    kk2, tt2 = np.meshgrid(np.arange(128), np.arange(128), indexing="ij")
    sh["ustrict"] = (kk2 < tt2).astype(np.float32)
    sh["rowtok_sh"] = np.ascontiguousarray((np.arange(34, dtype=np.uint32)[None, :] + 34 * np.arange(128, dtype=np.uint32)[:, None]).astype(np.uint32))
    C, S = _rope_tables()
    sh["ropec"] = C
    sh["ropes"] = S
    sh["ident"] = np.eye(128, dtype=np.float32)
    return sh


def prep_core(inputs, b):
    d = {}
    d["xin"] = np.ascontiguousarray(np.concatenate([inputs["ctx"][b], inputs["x"][b]], axis=0))
    c2 = np.stack([inputs["c"][b].reshape(KC, 128).T, inputs["c_ctx"].reshape(KC, 128).T], axis=-1)
    d["c2"] = np.ascontiguousarray(c2.astype(np.float32))
    return d


_CACHE = {}


def kernel(**inputs):
    inputs = {k_: np.asarray(v) for k_, v in inputs.items()}
    if "prog" not in _CACHE:
        _CACHE["prog"] = build_program()
    P = _CACHE["prog"]
    sh = prep_shared(inputs)
    in_maps = []
    for b in range(8):
        m = dict(sh)
        m.update(prep_core(inputs, b))
        in_maps.append(m)
    res = run_bass_kernel_spmd(P.nc, in_maps, core_ids=list(range(8)))
    return np.stack([r["out"] for r in res.results], axis=0)
```

```python
import math
from contextlib import ExitStack
import numpy as np
import concourse.bass as bass
import concourse.mybir as mybir
from concourse.bass_utils import run_bass_kernel_spmd

F32 = mybir.dt.float32
U32 = mybir.dt.uint32
BF16 = mybir.dt.bfloat16
AF = mybir.ActivationFunctionType
ALU = mybir.AluOpType
AX = mybir.AxisListType

D = 1024
LAT = 4096
CTX = 256
T = LAT + CTX
NT = T // 128
DEPTH = 2
KC = D // 128
N_MOD = 6 * D
E = 64
EF = 256
DN_ALPHA = (2 * DEPTH) ** 0.25
LN_EPS = 1e-5
RMS_EPS = 1e-6
CHUNKS = [(0, 256)] + [(256 + 512 * i, 512) for i in range(8)]

_W = dict(q=(0, 512), k=(512, 512), v=(1024, 512), lx=(1536, 512), lg=(2048, 512), z=(2560, 512),
          xbc=(3072, 1024), dt=(4096, 16), gates=(4112, 3072))
FM_ROWS = dict(q=0, k=512, lx=1024, lg=1536, xbc=2048, gates=3072)
N_FM = 6144


class TT:
    def __init__(self, ap, name="", multi=False):
        self.ap = ap
        self.name = name
        self.w = None
        self.r = []
        self.multi = multi
        self.wset = {}

    def __getitem__(self, idx):
        return self.ap[idx]


class KB:
    def __init__(self, nc, n_dma_sems=8):
        self.nc = nc
        self.eng = {"pe": nc.tensor, "act": nc.scalar, "dve": nc.vector, "pool": nc.gpsimd, "sp": nc.sync}
        self.sem = {}
        self.cnt = {}
        self.waited = {e: {} for e in self.eng}
        self._ctx = []
        self.allsems = {}
        for e in ("pe", "act", "dve", "pool"):
            cm = nc.semaphore("s_" + e)
            s = cm.__enter__()
            self._ctx.append(cm)
            self.sem[e] = s
            self.cnt[e] = 0
        self.dsem = {}
        for q in ("sp", "act", "pool"):
            lst = []
            for i in range(n_dma_sems):
                cm = nc.semaphore(f"d_{q}{i}")
                s = cm.__enter__()
                self._ctx.append(cm)
                lst.append([s, 0])
            self.dsem[q] = [lst, 0]
        self.n_inst = 0

    def close(self):
        for cm in reversed(self._ctx):
            cm.__exit__(None, None, None)

    def _need(self, e, toks):
        eng = self.eng[e]
        wd = self.waited[e]
        best = {}
        for tok in toks:
            if tok is None:
                continue
            s, v = tok
            key = id(s)
            if wd.get(key, 0) >= v:
                continue
            if key not in best or best[key][1] < v:
                best[key] = (s, v)
        for key, (s, v) in best.items():
            eng.wait_ge(s, v)
            wd[key] = v
            self.n_inst += 1

    def _deps(self, e, reads, writes):
        toks = []
        for t in reads:
            if t.w is None and not t.wset and not getattr(t, "ext", False):
                raise RuntimeError(f"read of {t.name} before any tracked write")
            toks.append(t.w)
            if t.wset:
                toks.extend(t.wset.values())
        for t in writes:
            toks.append(t.w)
            toks.extend(t.r)
            if t.wset and not t.multi:
                toks.extend(t.wset.values())
        if e == "pe":
            own = id(self.sem["pe"])
            toks = [t for t in toks if t is not None and id(t[0]) != own]
        self._need(e, toks)

    def _mark(self, tok, reads, writes):
        for t in reads:
            t.r.append(tok)
            if len(t.r) > 16:
                m = {}
                for s, v in t.r:
                    k = id(s)
                    if k not in m or m[k][1] < v:
                        m[k] = (s, v)
                t.r = list(m.values())
        for t in writes:
            if t.multi:
                kk_ = id(tok[0])
                if kk_ not in t.wset or t.wset[kk_][1] < tok[1]:
                    t.wset[kk_] = tok
            else:
                t.w = tok
                t.wset = {}
            t.r = []

    def op(self, e, fn, reads=(), writes=()):
        self._deps(e, reads, writes)
        ins = fn(self.eng[e])
        self.cnt[e] += 1
        ins.then_inc(self.sem[e], 1)
        tok = (self.sem[e], self.cnt[e])
        self._mark(tok, reads, writes)
        self.n_inst += 1
        return ins

    def dma(self, q, out, in_, reads=(), writes=(), **kw):
        lst = self.dsem[q][0]
        i = self.dsem[q][1]
        self.dsem[q][1] = (i + 1) % len(lst)
        ent = lst[i]
        if ent[1] > 0:
            self._need(q, [(ent[0], ent[1])])
        self._deps(q, reads, writes)
        ins = self.eng[q].dma_start(out=out, in_=in_, **kw)
        ent[1] += 16
        ins.then_inc(ent[0], 16)
        tok = (ent[0], ent[1])
        self._mark(tok, reads, writes)
        self.n_inst += 1
        return ins

    def dma_fn(self, q, fn, reads=(), writes=()):
        lst = self.dsem[q][0]
        i = self.dsem[q][1]
        self.dsem[q][1] = (i + 1) % len(lst)
        ent = lst[i]
        if ent[1] > 0:
            self._need(q, [(ent[0], ent[1])])
        self._deps(q, reads, writes)
        ins = fn(self.eng[q])
        ent[1] += 16
        ins.then_inc(ent[0], 16)
        tok = (ent[0], ent[1])
        self._mark(tok, reads, writes)
        self.n_inst += 1
        return ins

    def barrier(self):
        toks = [(self.sem[e], self.cnt[e]) for e in self.sem if self.cnt[e] > 0]
        for q in self.dsem:
            for s, v in self.dsem[q][0]:
                if v > 0:
                    toks.append((s, v))
        for e in self.eng:
            self._need(e, toks)


class Rot:
    def __init__(self, tiles):
        self.tiles = tiles
        self.i = 0

    def next(self):
        t = self.tiles[self.i]
        self.i = (self.i + 1) % len(self.tiles)
        return t


class Prog:
    def __init__(self, debug=False):
        self.debug = debug
        self.nc = bass.Bass("TRN2", target_bir_lowering=False)
        self.k = None
        self.ins = {}
        self.dbg = {}

    def inp(self, name, shape, dt=F32):
        ap = self.nc.dram_tensor(name, list(shape), dt, kind="ExternalInput").ap()
        self.ins[name] = TT(ap, name)
        self.ins[name].ext = True
        return self.ins[name]

    def scratch(self, name, shape, dt, dump=False, multi=False):
        if dump and self.debug:
            ap = self.nc.dram_tensor(name, list(shape), dt, kind="ExternalOutput").ap()
            self.dbg[name] = True
        else:
            ap = self.nc.dram_tensor(name, list(shape), dt).ap()
        return TT(ap, name, multi=multi)


_UID = [0]


def sbt(nc, es, name, shape, dt):
    _UID[0] += 1
    name = f"{name}_s{_UID[0]}"
    return TT(es.enter_context(nc.sbuf_tensor(name, list(shape), dt)), name)


def pst(nc, es, name, shape, dt=F32):
    _UID[0] += 1
    name = f"{name}_p{_UID[0]}"
    return TT(es.enter_context(nc.psum_tensor(name, list(shape), dt)), name)


def build_program(debug=False, n_layers=DEPTH, stop_after=None, stages=None):
    P = Prog(debug)
    nc = P.nc
    xin = P.inp("xin", [T, D])
    c2 = P.inp("c2", [128, KC, 2])
    ident_in = P.inp("ident", [128, 128])
    w_mod = P.inp("w_mod", [DEPTH, D, N_MOD])
    b_modT = P.inp("b_modT", [DEPTH, 128, 48])
    w_fm = P.inp("w_fm", [DEPTH, 14, 128, KC, 512])
    w_tm = P.inp("w_tm", [DEPTH, 2, 128, KC, 512])
    w_dt = P.inp("w_dt", [DEPTH, 128, KC, 16])
    ropec = P.inp("ropec", [128, T])
    ropes = P.inp("ropes", [128, T])
    lamqk = P.inp("lamqk", [DEPTH, 128, 256])
    attn_gT = P.inp("attn_gT", [DEPTH, 128, 4])
    lru_cw = P.inp("lru_cw", [DEPTH, 128, 4, 4])
    lru_cb = P.inp("lru_cb", [DEPTH, 128, 4])
    lru_wbd = P.inp("lru_wbd", [DEPTH, 128, 16, 128])
    lru_bias = P.inp("lru_bias", [DEPTH, 128, 16])
    lru_lam = P.inp("lru_lam", [DEPTH, 128, 8])
    ssd_cw = P.inp("ssd_cw", [DEPTH, 128, 8, 4])
    ssd_cb = P.inp("ssd_cb", [DEPTH, 128, 8])
    ssd_dtb = P.inp("ssd_dtb", [DEPTH, 128, NT * 16])
    ssd_alog = P.inp("ssd_alog", [DEPTH, 128, NT * 16])
    ssd_dsk = P.inp("ssd_dsk", [DEPTH, 128, 8])
    ssd_gn = P.inp("ssd_gn", [DEPTH, 128, 512])
    tri_in = P.inp("tri", [128, 2, 128])
    maskf_in = P.inp("maskf", [128, 2, 8, 128])
    identb_in = P.inp("identb", [128, 128])
    w_br = P.inp("w_br", [DEPTH, 128, 12, D])
    w_o = P.inp("w_o", [DEPTH, 128, KC, D])
    ln_gb = P.inp("ln_gb", [DEPTH, 4, 128, D])
    w_rt = P.inp("w_rt", [DEPTH, 128, KC, E])
    r_bias = P.inp("r_bias", [DEPTH, 128, E])
    w_upx = P.inp("w_upx", [DEPTH, E + 1, 128, KC, 512])
    w_dnx = P.inp("w_dnx", [DEPTH, E + 1, 128, 2, D])
    iota64 = P.inp("iota64", [128, 64])
    iotap = P.inp("iotap", [128, 1])
    thr_in = P.inp("thr_in", [128, 200])
    ustrict = P.inp("ustrict", [128, 128])
    rowtok_sh = P.inp("rowtok_sh", [128, 34], U32)
    out = TT(nc.dram_tensor("out", [LAT, D], F32, kind="ExternalOutput").ap(), "out")
    xs_full = P.scratch("xs", [T, D], F32, dump=True)
    xs_t = [TT(xs_full.ap[tt * 128:(tt + 1) * 128, :], f"xs{tt}") for tt in range(NT)]
    NBR = 200
    NBS = T // 256
    NBLK = NBR + NBS
    NROWS = NBLK * 256
    H2TM = P.scratch("H2TM", [T, D], BF16, multi=True)
    WUPB = P.scratch("WUPB", [(E + 1) * 128, KC * 512], BF16, multi=True)
    WDNB = P.scratch("WDNB", [(E + 1) * 128, 2 * D], BF16, multi=True)
    ROWTOK = P.scratch("ROWTOK", [NROWS, 1], U32, dump=True)
    OUTB = P.scratch("OUTB", [NROWS, D], BF16, multi=True)
    PF = P.scratch("PF", [N_FM, T], BF16, dump=True, multi=True)
    PV = P.scratch("PVt", [T, 512], BF16, dump=True, multi=True)
    PZ = P.scratch("PZt", [T, 512], BF16, dump=True, multi=True)
    PDT = P.scratch("PDT", [T, 16], F32, dump=True)
    YT = P.scratch("YT", [1536, T], BF16, dump=True, multi=True)

    k = KB(nc)
    P.k = k
    with ExitStack() as es0:
        ident = sbt(nc, es0, "ident_sb", [128, 128], F32)
        mod = sbt(nc, es0, "mod", [128, 48, 2], F32)
        mod1 = sbt(nc, es0, "mod1", [128, 48, 2], F32)
        s2 = sbt(nc, es0, "s2", [128, KC, 2], F32)
        D8 = sbt(nc, es0, "D8", [128, NT, 8], U32)
        W8 = sbt(nc, es0, "W8", [128, NT, 8], F32)
        IDXW = sbt(nc, es0, "IDXW", [128, NBLK], U32)
        idb_g = sbt(nc, es0, "idb0", [128, 128], BF16)
        k.dma("sp", ident[:], ident_in[:], writes=[ident])
        k.op("dve", lambda e: e.tensor_copy(idb_g[:], ident[:]), reads=[ident], writes=[idb_g])
        k.dma("sp", s2[:], c2[:], writes=[s2])
        k.op("act", lambda e: e.activation(s2[:], s2[:], AF.Silu), reads=[s2], writes=[s2])

        for l in range(n_layers):
            with ExitStack() as es:
                wm = Rot([sbt(nc, es, f"wm{i}", [128, N_MOD], F32) for i in range(2)])
                bm = sbt(nc, es, "bm", [128, 48], F32)
                pm = pst(nc, es, "pm", [128, 96])
                k.dma("sp", bm[:], b_modT[l], writes=[bm])
                for kc in range(KC):
                    w = wm.next()
                    k.dma("sp" if kc % 2 == 0 else "pool", w[:], w_mod[l, kc * 128:(kc + 1) * 128, :], writes=[w])
                    for cb in range(48):
                        k.op("pe", lambda e, w=w, cb=cb, kc=kc: e.matmul(
                            pm[:, 2 * cb:2 * cb + 2], w[:, cb * 128:(cb + 1) * 128], s2[:, kc, :],
                            start=(kc == 0 and cb == 0), stop=(kc == KC - 1 and cb == 47), skip_group_check=True),
                            reads=[w, s2], writes=[pm])
                pm3 = pm.ap.rearrange("p (c j) -> p c j", j=2)
                for j in range(2):
                    k.op("dve", lambda e, j=j: e.tensor_tensor(mod[:, :, j], pm3[:, :, j], bm[:], ALU.add),
                         reads=[pm, bm], writes=[mod])
                k.op("dve", lambda e: e.tensor_scalar_add(mod1[:], mod[:], 1.0), reads=[mod], writes=[mod1])
                k.barrier()
            if stop_after == "mod":
                break

            with ExitStack() as es:
                hT = sbt(nc, es, "hT", [128, KC, T], BF16)
                with ExitStack() as es1:
                    xt = Rot([sbt(nc, es1, f"xt{i}", [128, D], F32) for i in range(3)])
                    ptr = Rot([pst(nc, es1, f"ptr{i}", [128, 512]) for i in range(4)])
                    for tt in range(NT):
                        x_t = xt.next()
                        if l == 0:
                            k.dma("sp" if tt % 2 == 0 else "pool", x_t[:], xin[tt * 128:(tt + 1) * 128, :], reads=[xin], writes=[x_t])
                        else:
                            k.dma("sp" if tt % 2 == 0 else "pool", x_t[:], xs_t[tt][:], reads=[xs_t[tt]], writes=[x_t])
                        j = 1 if tt < 2 else 0
                        for half in range(2):
                            p_t = ptr.next()
                            for q4 in range(4):
                                kc = half * 4 + q4
                                k.op("pe", lambda e, p_t=p_t, x_t=x_t, kc=kc, q4=q4: e.transpose(
                                    p_t[:, q4 * 128:(q4 + 1) * 128], x_t[:, kc * 128:(kc + 1) * 128], ident[:]),
                                    reads=[x_t, ident], writes=[p_t])
                            for q4 in range(4):
                                kc = half * 4 + q4
                                eng = "act" if q4 % 2 == 0 else "dve"
                                if eng == "act":
                                    k.op("act", lambda e, p_t=p_t, kc=kc, q4=q4, tt=tt, j=j: e.activation(
                                        hT[:, kc, tt * 128:(tt + 1) * 128], p_t[:, q4 * 128:(q4 + 1) * 128], AF.Identity,
                                        bias=mod[:, kc, j:j + 1], scale=mod1[:, 8 + kc, j:j + 1]),
                                        reads=[p_t, mod, mod1], writes=[hT])
                                else:
                                    k.op("dve", lambda e, p_t=p_t, kc=kc, q4=q4, tt=tt, j=j: e.tensor_scalar(
                                        hT[:, kc, tt * 128:(tt + 1) * 128], p_t[:, q4 * 128:(q4 + 1) * 128],
                                        mod1[:, 8 + kc, j:j + 1], mod[:, kc, j:j + 1], ALU.mult, ALU.add),
                                        reads=[p_t, mod, mod1], writes=[hT])
                    k.barrier()
                with ExitStack() as es2:
                    wf32 = Rot([sbt(nc, es2, f"wf32_{i}", [128, KC, 512], F32) for i in range(2)])
                    wbf = Rot([sbt(nc, es2, f"wbf_{i}", [128, KC, 512], BF16) for i in range(3)])
                    stg = Rot([sbt(nc, es2, f"stg{i}", [128, T], BF16) for i in range(3)])
                    pp = Rot([pst(nc, es2, f"pp{i}", [128, 512]) for i in range(6)])
                    rc = sbt(nc, es2, "rc", [128, T], F32)
                    rs = sbt(nc, es2, "rs", [128, T], F32)
                    tmp = Rot([sbt(nc, es2, f"rtmp{i}", [128, 512], F32) for i in range(4)])
                    k.dma("sp", rc[:], ropec[:], writes=[rc])
                    k.dma("pool", rs[:], ropes[:], writes=[rs])
                    cast_i = [0]

                    def load_w(src_ap, src_t):
                        a = wf32.next()
                        k.dma("sp", a[:], src_ap, reads=[src_t], writes=[a])
                        b = wbf.next()
                        eng = ("pool", "dve")[cast_i[0] % 2]
                        cast_i[0] += 1
                        for h2 in range(2):
                            k.op(eng, lambda e, a=a, b=b, h2=h2: e.tensor_copy(b[:, h2 * 4:(h2 + 1) * 4, :], a[:, h2 * 4:(h2 + 1) * 4, :]),
                                 reads=[a], writes=[b])
                        return b

                    evac_i = [0]

                    def fm_block(wb, row0):
                        for m in range(4):
                            st = stg.next()
                            for (t0, n) in CHUNKS:
                                p_t = pp.next()
                                for kc in range(KC):
                                    k.op("pe", lambda e, p_t=p_t, wb=wb, kc=kc, m=m, t0=t0, n=n: e.matmul(
                                        p_t[:, :n], wb[:, kc, m * 128:(m + 1) * 128], hT[:, kc, t0:t0 + n],
                                        start=(kc == 0), stop=(kc == KC - 1)), reads=[wb, hT], writes=[p_t])
                                eng = ("act", "dve")[evac_i[0] % 2]
                                evac_i[0] += 1
                                if eng == "act":
                                    k.op("act", lambda e, p_t=p_t, st=st, t0=t0, n=n: e.copy(st[:, t0:t0 + n], p_t[:, :n]),
                                         reads=[p_t], writes=[st])
                                else:
                                    k.op("dve", lambda e, p_t=p_t, st=st, t0=t0, n=n: e.tensor_copy(st[:, t0:t0 + n], p_t[:, :n]),
                                         reads=[p_t], writes=[st])
                            k.dma("pool", PF[row0 + m * 128:row0 + (m + 1) * 128, :], st[:], reads=[st], writes=[PF])

                    def rope_block(wb, wbs, row0):
                        for m in range(4):
                            st = stg.next()
                            for (t0, n) in CHUNKS:
                                pa = pp.next()
                                pb = pp.next()
                                for kc in range(KC):
                                    k.op("pe", lambda e, pa=pa, wb=wb, kc=kc, m=m, t0=t0, n=n: e.matmul(
                                        pa[:, :n], wb[:, kc, m * 128:(m + 1) * 128], hT[:, kc, t0:t0 + n],
                                        start=(kc == 0), stop=(kc == KC - 1)), reads=[wb, hT], writes=[pa])
                                for kc in range(KC):
                                    k.op("pe", lambda e, pb=pb, wbs=wbs, kc=kc, m=m, t0=t0, n=n: e.matmul(
                                        pb[:, :n], wbs[:, kc, m * 128:(m + 1) * 128], hT[:, kc, t0:t0 + n],
                                        start=(kc == 0), stop=(kc == KC - 1)), reads=[wbs, hT], writes=[pb])
                                t1 = tmp.next()
                                t2 = tmp.next()
                                k.op("dve", lambda e, pa=pa, t1=t1, t0=t0, n=n: e.tensor_tensor(t1[:, :n], pa[:, :n], rc[:, t0:t0 + n], ALU.mult),
                                     reads=[pa, rc], writes=[t1])
                                k.op("dve", lambda e, pb=pb, t2=t2, t0=t0, n=n: e.tensor_tensor(t2[:, :n], pb[:, :n], rs[:, t0:t0 + n], ALU.mult),
                                     reads=[pb, rs], writes=[t2])
                                k.op("pool", lambda e, t1=t1, t2=t2, st=st, t0=t0, n=n: e.tensor_tensor(st[:, t0:t0 + n], t1[:, :n], t2[:, :n], ALU.add),
                                     reads=[t1, t2], writes=[st])
                            k.dma("pool", PF[row0 + m * 128:row0 + (m + 1) * 128, :], st[:], reads=[st], writes=[PF])

                    for qi, name in enumerate(("q", "k")):
                        wb = load_w(w_fm[l, 2 * qi], w_fm)
                        wbs = load_w(w_fm[l, 2 * qi + 1], w_fm)
                        rope_block(wb, wbs, FM_ROWS[name])
                    for bi in range(4, 14):
                        wb = load_w(w_fm[l, bi], w_fm)
                        fm_block(wb, 1024 + (bi - 4) * 512)
                    stt = Rot([sbt(nc, es2, f"stt{i}", [128, 512], BF16) for i in range(3)])
                    for vi, dst in enumerate((PV, PZ)):
                        wb = load_w(w_tm[l, vi], w_tm)
                        for tt in range(NT):
                            p_t = pp.next()
                            for kc in range(KC):
                                k.op("pe", lambda e, p_t=p_t, wb=wb, kc=kc, tt=tt: e.matmul(
                                    p_t[:], hT[:, kc, tt * 128:(tt + 1) * 128], wb[:, kc, :],
                                    start=(kc == 0), stop=(kc == KC - 1)), reads=[wb, hT], writes=[p_t])
                            st = stt.next()
                            eng = ("act", "dve")[tt % 2]
                            if eng == "act":
                                k.op("act", lambda e, p_t=p_t, st=st: e.copy(st[:], p_t[:]), reads=[p_t], writes=[st])
                            else:
                                k.op("dve", lambda e, p_t=p_t, st=st: e.tensor_copy(st[:], p_t[:]), reads=[p_t], writes=[st])
                            k.dma("pool", dst[tt * 128:(tt + 1) * 128, :], st[:], reads=[st], writes=[dst])
                    wd32 = sbt(nc, es2, "wd32", [128, KC, 16], F32)
                    wdb = sbt(nc, es2, "wdb", [128, KC, 16], BF16)
                    dts = sbt(nc, es2, "dts", [128, NT, 16], F32)
                    k.dma("sp", wd32[:], w_dt[l], writes=[wd32])
                    k.op("dve", lambda e: e.tensor_copy(wdb[:], wd32[:]), reads=[wd32], writes=[wdb])
                    for tt in range(NT):
                        p_t = pp.next()
                        for kc in range(KC):
                            k.op("pe", lambda e, p_t=p_t, kc=kc, tt=tt: e.matmul(
                                p_t[:, :16], hT[:, kc, tt * 128:(tt + 1) * 128], wdb[:, kc, :],
                                start=(kc == 0), stop=(kc == KC - 1)), reads=[wdb, hT], writes=[p_t])
                        k.op("act", lambda e, p_t=p_t, tt=tt: e.copy(dts[:, tt, :], p_t[:, :16]), reads=[p_t], writes=[dts])
                    k.dma("sp", PDT.ap.rearrange("(n p) c -> p n c", p=128), dts[:], reads=[dts], writes=[PDT])
                    k.barrier()
            if stop_after == "proj":
                break


            if stages is None or "attn" in stages:
              with ExitStack() as es:
                lam_init = 0.8 - 0.6 * math.exp(-0.3 * l)
                QT = sbt(nc, es, "QT", [128, 4, T], BF16)
                KT = sbt(nc, es, "KT", [128, 4, T], BF16)
                V = sbt(nc, es, "V", [128, NT, 512], BF16)
                ones_b = sbt(nc, es, "ones_b", [128, 128], BF16)
                ones_f = sbt(nc, es, "ones_f", [128, 128], F32)
                lq = sbt(nc, es, "lq", [128, 256], F32)
                lpr = sbt(nc, es, "lpr", [128, 128], F32)
                lsum = sbt(nc, es, "lsum", [128, 2], F32)
                nlam = sbt(nc, es, "nlam", [128, 1], F32)
                gsc = sbt(nc, es, "gsc", [128, 4], F32)
                for h in range(4):
                    k.dma("sp", QT[:, h, :], PF[h * 128:(h + 1) * 128, :], reads=[PF], writes=[QT])
                    k.dma("pool", KT[:, h, :], PF[512 + h * 128:512 + (h + 1) * 128, :], reads=[PF], writes=[KT])
                pv3 = PV.ap.rearrange("(n p) c -> p n c", p=128)
                for hh in range(2):
                    k.dma("sp", V[:, hh * 17:(hh + 1) * 17, :], pv3[:, hh * 17:(hh + 1) * 17, :], reads=[PV], writes=[V])
                k.op("dve", lambda e: e.memset(ones_b[:], 1.0), writes=[ones_b])
                k.op("dve", lambda e: e.memset(ones_f[:], 1.0), writes=[ones_f])
                k.dma("sp", lq[:], lamqk[l], writes=[lq])
                k.dma("sp", gsc[:], attn_gT[l], writes=[gsc])
                k.op("dve", lambda e: e.tensor_tensor(lpr[:], lq[:, 0:128], lq[:, 128:256], ALU.mult), reads=[lq], writes=[lpr])
                k.op("dve", lambda e: e.reduce_sum(lsum[:], lpr.ap.rearrange("p (a b) -> p a b", a=2), AX.X), reads=[lpr], writes=[lsum])
                k.op("act", lambda e: e.activation(lsum[:], lsum[:], AF.Exp), reads=[lsum], writes=[lsum])
                k.op("dve", lambda e: e.scalar_tensor_tensor(nlam[:], lsum[:, 1:2], -lam_init, lsum[:, 0:1], ALU.add, ALU.subtract),
                     reads=[lsum], writes=[nlam])
                k.op("dve", lambda e: e.tensor_scalar_mul(gsc[:], gsc[:], 1.0 - lam_init), reads=[gsc], writes=[gsc])
                sps = Rot([pst(nc, es, f"sps{i}", [128, 512]) for i in range(4)])
                acc = [pst(nc, es, f"acc{i}", [128, 512]) for i in range(4)]
                pts = Rot([sbt(nc, es, f"pts{i}", [128, 512], BF16) for i in range(4)])
                wk = Rot([sbt(nc, es, f"awk{i}", [128, 512], F32) for i in range(9)])
                yst = Rot([sbt(nc, es, f"yst{i}", [128, 512], BF16) for i in range(2)])
                su = Rot([sbt(nc, es, f"su{i}", [128, KC, 512], F32) for i in range(2)])
                sd = Rot([sbt(nc, es, f"sd{i}", [128, 2, D], F32) for i in range(2)])
                bu = Rot([sbt(nc, es, f"bu{i}", [128, KC * 512], BF16) for i in range(2)])
                bd = Rot([sbt(nc, es, f"bd{i}", [128, 2 * D], BF16) for i in range(2)])

                def precast_gen():
                    for ex in range(E + 1):
                        a_ = su.next(); b_ = sd.next(); c_ = bu.next(); d_ = bd.next()
                        k.dma("sp", a_[:], w_upx[l, ex], writes=[a_])
                        k.dma("sp", b_[:], w_dnx[l, ex], writes=[b_])
                        yield
                        k.op("pool", lambda e, a_=a_, c_=c_: e.tensor_copy(c_[:], a_.ap.rearrange("p a b -> p (a b)")), reads=[a_], writes=[c_])
                        k.op("dve", lambda e, b_=b_, d_=d_: e.tensor_copy(d_[:], b_.ap.rearrange("p a b -> p (a b)")), reads=[b_], writes=[d_])
                        yield
                        k.dma("pool", WUPB[ex * 128:(ex + 1) * 128, :], c_[:], reads=[c_], writes=[WUPB])
                        k.dma("pool", WDNB[ex * 128:(ex + 1) * 128, :], d_[:], reads=[d_], writes=[WDNB])
                        yield

                pcg = precast_gen()
                pc_ctr = [0]

                def precast_tick():
                    pc_ctr[0] += 1
                    if pc_ctr[0] % 5 == 0:
                        next(pcg, None)

                pend_epi = []
                for h in range(4):
                    for (q0, n) in CHUNKS:
                        kts = [0, 1] if q0 == 0 else list(range(NT))

                        def s_mm(kt, h=h, q0=q0, n=n):
                            res = []
                            for m in range(2):
                                sp_ = sps.next()
                                k.op("pe", lambda e, sp_=sp_, m=m, kt=kt: e.matmul(
                                    sp_[:, :n], KT[m * 64:(m + 1) * 64, h, kt * 128:(kt + 1) * 128],
                                    QT[m * 64:(m + 1) * 64, h, q0:q0 + n], start=True, stop=True),
                                    reads=[KT, QT], writes=[sp_])
                                res.append(sp_)
                            return res

                        cur = s_mm(kts[0])
                        for i, kt in enumerate(kts):
                            precast_tick()
                            if (i == 3 or (len(kts) < 4 and i == len(kts) - 1)) and pend_epi:
                                pend_epi.pop(0)()
                            nxt = s_mm(kts[i + 1]) if i + 1 < len(kts) else None
                            for m in range(2):
                                pt = pts.next()
                                sp_ = cur[m]
                                k.op("act", lambda e, pt=pt, sp_=sp_: e.activation(pt[:, :n], sp_[:, :n], AF.Exp, scale=0.125),
                                     reads=[sp_], writes=[pt])
                                k.op("pe", lambda e, pt=pt, m=m, kt=kt, i=i: e.matmul(
                                    acc[2 * m][:, :n], V[:, kt, h * 128:(h + 1) * 128], pt[:, :n],
                                    start=(i == 0), stop=(i == len(kts) - 1)), reads=[V, pt], writes=[acc[2 * m]])
                                k.op("pe", lambda e, pt=pt, m=m, i=i: e.matmul(
                                    acc[2 * m + 1][:, :n], ones_b[:], pt[:, :n],
                                    start=(i == 0), stop=(i == len(kts) - 1)), reads=[ones_b, pt], writes=[acc[2 * m + 1]])
                            cur = nxt
                        rd1, o1s, rd2, o2s, o, sq = [wk.next() for _ in range(6)]
                        k.op("act", lambda e, o1s=o1s: e.copy(o1s[:, :n], acc[0][:, :n]), reads=[acc[0]], writes=[o1s])
                        k.op("dve", lambda e, rd1=rd1: e.reciprocal(rd1[:, :n], acc[1][:, :n]), reads=[acc[1]], writes=[rd1])
                        k.op("act", lambda e, o2s=o2s: e.copy(o2s[:, :n], acc[2][:, :n]), reads=[acc[2]], writes=[o2s])
                        k.op("dve", lambda e, rd2=rd2: e.reciprocal(rd2[:, :n], acc[3][:, :n]), reads=[acc[3]], writes=[rd2])
                        k.op("dve", lambda e, rd1=rd1, o1s=o1s: e.tensor_tensor(o1s[:, :n], o1s[:, :n], rd1[:, :n], ALU.mult),
                             reads=[o1s, rd1], writes=[o1s])
                        k.op("pool", lambda e, rd2=rd2, o2s=o2s: e.tensor_tensor(o2s[:, :n], o2s[:, :n], rd2[:, :n], ALU.mult),
                             reads=[o2s, rd2], writes=[o2s])
                        k.op("dve", lambda e, o=o, o1s=o1s, o2s=o2s: e.scalar_tensor_tensor(
                            o[:, :n], o2s[:, :n], nlam[:, 0:1], o1s[:, :n], ALU.mult, ALU.add), reads=[o1s, o2s, nlam], writes=[o])
                        k.op("pool", lambda e, o=o, sq=sq: e.tensor_tensor(sq[:, :n], o[:, :n], o[:, :n], ALU.mult), reads=[o], writes=[sq])
                        def epi_b(o=o, sq=sq, h=h, q0=q0, n=n):
                            ssp = sps.tiles[sps.i]
                            k.op("pe", lambda e: e.matmul(ssp[:, :n], ones_f[:], sq[:, :n], start=True, stop=True),
                                 reads=[ones_f, sq], writes=[ssp])
                            rs_ = wk.next()
                            k.op("dve", lambda e: e.tensor_scalar(rs_[:, :n], ssp[:, :n], 1.0 / 128, RMS_EPS, ALU.mult, ALU.add),
                                 reads=[ssp], writes=[rs_])
                            k.op("act", lambda e: e.activation(rs_[:, :n], rs_[:, :n], AF.Sqrt), reads=[rs_], writes=[rs_])
                            k.op("dve", lambda e: e.reciprocal(rs_[:, :n], rs_[:, :n]), reads=[rs_], writes=[rs_])
                            ys = yst.next()
                            k.op("dve", lambda e: e.scalar_tensor_tensor(
                                ys[:, :n], o[:, :n], gsc[:, h:h + 1], rs_[:, :n], ALU.mult, ALU.mult), reads=[o, rs_, gsc], writes=[ys])
                            k.dma("sp", YT[h * 128:(h + 1) * 128, q0:q0 + n], ys[:, :n], reads=[ys], writes=[YT])
                        pend_epi.append(epi_b)
                while pend_epi:
                    pend_epi.pop(0)()
                for _ in pcg:
                    pass
                k.barrier()
            if stop_after == "attn":
                break

            if stages is None or "lru" in stages:
              with ExitStack() as es:
                cw = sbt(nc, es, "cw", [128, 4, 4], F32)
                cb = sbt(nc, es, "cb", [128, 4], F32)
                wbd32 = sbt(nc, es, "wbd32", [128, 16, 128], F32)
                wbd = sbt(nc, es, "wbd", [128, 16, 128], BF16)
                lbias = sbt(nc, es, "lbias", [128, 16], F32)
                cch = sbt(nc, es, "cch", [128, 8], F32)
                cch2 = sbt(nc, es, "cch2", [128, 8], F32)
                k.dma("sp", cw[:], lru_cw[l], writes=[cw])
                k.dma("sp", cb[:], lru_cb[l], writes=[cb])
                k.dma("sp", wbd32[:], lru_wbd[l], writes=[wbd32])
                k.dma("sp", lbias[:], lru_bias[l], writes=[lbias])
                k.dma("sp", cch[:], lru_lam[l], writes=[cch])
                k.op("dve", lambda e: e.tensor_copy(wbd[:], wbd32[:]), reads=[wbd32], writes=[wbd])
                k.op("act", lambda e: e.activation(cch[:], cch[:], AF.Exp, scale=-1.0), reads=[cch], writes=[cch])
                k.op("act", lambda e: e.activation(cch[:], cch[:], AF.Ln, bias=1.0), reads=[cch], writes=[cch])
                k.op("dve", lambda e: e.tensor_scalar_mul(cch2[:], cch[:], -16.0), reads=[cch], writes=[cch2])
                k.op("dve", lambda e: e.tensor_scalar_mul(cch[:], cch[:], -8.0), reads=[cch], writes=[cch])
                lxb = sbt(nc, es, "lxb", [128, T], BF16)
                u = sbt(nc, es, "u", [128, T], F32)
                ub = sbt(nc, es, "ub", [128, T], BF16)
                rr2 = [sbt(nc, es, f"rr{d}", [128, T], F32) for d in range(2)]
                ig2 = [sbt(nc, es, f"ig{d}", [128, T], F32) for d in range(2)]
                aa2 = [sbt(nc, es, f"aa{d}", [128, T], F32) for d in range(2)]
                tmp2 = [sbt(nc, es, f"tmpb{d}", [128, T], F32) for d in range(2)]
                yy = sbt(nc, es, "yy", [128, T], F32)
                lgb = ub
                ybo = lxb
                pg = Rot([pst(nc, es, f"pg{i}", [128, 512]) for i in range(6)])
                SEGS = [(0, CTX), (CTX, T)]
                for ct in range(4):
                    k.dma("sp", lxb[:], PF[1024 + ct * 128:1024 + (ct + 1) * 128, :], reads=[PF], writes=[lxb])
                    k.op("dve", lambda e, ct=ct: e.tensor_scalar(u[:], lxb[:], cw[:, ct, 2:3], cb[:, ct:ct + 1], ALU.mult, ALU.add),
                         reads=[lxb, cw, cb], writes=[u])
                    for j in (0, 1, 3):
                        o = j - 2
                        for (s0, s1) in SEGS:
                            a_, b_ = (s0 - o, s1) if o < 0 else (s0, s1 - o)
                            k.op("dve", lambda e, ct=ct, j=j, a_=a_, b_=b_, o=o: e.scalar_tensor_tensor(
                                u[:, a_:b_], lxb[:, a_ + o:b_ + o], cw[:, ct, j:j + 1], u[:, a_:b_], ALU.mult, ALU.add),
                                reads=[lxb, cw, u], writes=[u])
                    k.op("pool", lambda e: e.tensor_copy(ub[:], u[:]), reads=[u], writes=[ub])
                    for d in range(2):
                        rr, ig = rr2[d], ig2[d]
                        ia = (d * 2 + 0) * 4 + ct
                        ii = (d * 2 + 1) * 4 + ct
                        for (t0, n) in CHUNKS:
                            pa = pg.next()
                            pi = pg.next()
                            k.op("pe", lambda e, pa=pa, ia=ia, t0=t0, n=n: e.matmul(pa[:, :n], wbd[:, ia, :], ub[:, t0:t0 + n], start=True, stop=True),
                                 reads=[wbd, ub], writes=[pa])
                            k.op("pe", lambda e, pi=pi, ii=ii, t0=t0, n=n: e.matmul(pi[:, :n], wbd[:, ii, :], ub[:, t0:t0 + n], start=True, stop=True),
                                 reads=[wbd, ub], writes=[pi])
                            k.op("act", lambda e, pa=pa, ia=ia, t0=t0, n=n, rr=rr: e.activation(rr[:, t0:t0 + n], pa[:, :n], AF.Sigmoid, bias=lbias[:, ia:ia + 1]),
                                 reads=[pa, lbias], writes=[rr])
                            k.op("act", lambda e, pi=pi, ii=ii, t0=t0, n=n, ig=ig: e.activation(ig[:, t0:t0 + n], pi[:, :n], AF.Sigmoid, bias=lbias[:, ii:ii + 1]),
                                 reads=[pi, lbias], writes=[ig])
                    k.dma("sp", lgb[:], PF[1536 + ct * 128:1536 + (ct + 1) * 128, :], reads=[PF], writes=[lgb])
                    for d in range(2):
                        rr, ig, aa, tmpb = rr2[d], ig2[d], aa2[d], tmp2[d]
                        dc = d * 4 + ct
                        k.op("act", lambda e, dc=dc, rr=rr, aa=aa: e.activation(aa[:], rr[:], AF.Exp, scale=cch[:, dc:dc + 1]), reads=[rr, cch], writes=[aa])
                        k.op("act", lambda e, dc=dc, rr=rr, tmpb=tmpb: e.activation(tmpb[:], rr[:], AF.Exp, scale=cch2[:, dc:dc + 1]), reads=[rr, cch2], writes=[tmpb])
                    for d in range(2):
                        tmpb = tmp2[d]
                        k.op("act", lambda e, tmpb=tmpb: e.activation(tmpb[:], tmpb[:], AF.Sqrt, bias=1.0, scale=-1.0), reads=[tmpb], writes=[tmpb])
                    for d in range(2):
                        rr, ig, aa, tmpb = rr2[d], ig2[d], aa2[d], tmp2[d]
                        k.op("dve", lambda e, ig=ig: e.tensor_tensor(ig[:], ig[:], u[:], ALU.mult), reads=[ig, u], writes=[ig])
                        k.op("pool", lambda e, ig=ig, tmpb=tmpb: e.tensor_tensor(ig[:], ig[:], tmpb[:], ALU.mult), reads=[ig, tmpb], writes=[ig])
                        dst = yy if d == 0 else rr
                        if d == 0:
                            k.op("dve", lambda e, dst=dst, aa=aa, ig=ig: e.tensor_tensor_scan(dst[:], aa[:], ig[:], 0.0, ALU.mult, ALU.add),
                                 reads=[aa, ig], writes=[dst])
                        else:
                            k.op("dve", lambda e, dst=dst, aa=aa, ig=ig: e.tensor_tensor_scan(dst[:, 0:CTX][:, ::-1], aa[:, 0:CTX][:, ::-1], ig[:, 0:CTX][:, ::-1],
                                                                                             0.0, ALU.mult, ALU.add), reads=[aa, ig], writes=[dst])
                            k.op("dve", lambda e, dst=dst, aa=aa, ig=ig: e.tensor_tensor_scan(dst[:, CTX:T][:, ::-1], aa[:, CTX:T][:, ::-1], ig[:, CTX:T][:, ::-1],
                                                                                             dst[:, 0:1], ALU.mult, ALU.add), reads=[aa, ig, dst], writes=[dst])
                            k.op("pool", lambda e, dst=dst: e.tensor_tensor(yy[:], yy[:], dst[:], ALU.add), reads=[yy, dst], writes=[yy])
                    tmpb = tmp2[0]
                    k.op("act", lambda e: e.activation(tmpb[:], lgb[:], AF.Gelu), reads=[lgb], writes=[tmpb])
                    k.op("dve", lambda e: e.tensor_tensor(ybo[:], yy[:], tmpb[:], ALU.mult), reads=[yy, tmpb], writes=[ybo])
                    k.dma("sp", YT[512 + ct * 128:512 + (ct + 1) * 128, :], ybo[:], reads=[ybo], writes=[YT])
                k.barrier()
            if stop_after == "lru":
                break

            if stages is None or "ssd" in stages:
              with ExitStack() as es:
                tri = sbt(nc, es, "tri", [128, 2, 128], F32)
                ntri = sbt(nc, es, "ntri", [128, 2, 128], F32)
                ones_f = sbt(nc, es, "ones_f2", [128, 128], F32)
                idb32 = sbt(nc, es, "idb32", [128, 128], F32)
                idb = sbt(nc, es, "idb", [128, 128], BF16)
                scw = sbt(nc, es, "scw", [128, 8, 4], F32)
                scb = sbt(nc, es, "scb", [128, 8], F32)
                dtv = sbt(nc, es, "dtv", [128, NT, 16], F32)
                av = sbt(nc, es, "av", [128, NT, 16], F32)
                eal = sbt(nc, es, "eal", [128, NT * 16], F32)
                dsk = sbt(nc, es, "dsk", [128, 8], F32)
                gn = sbt(nc, es, "gn", [128, 512], F32)
                BCT = sbt(nc, es, "BCT", [128, 4, T], BF16)
                Xtm = sbt(nc, es, "Xtm", [128, NT, 512], BF16)
                Btm = sbt(nc, es, "Btm", [128, NT, 256], BF16)
                k.dma("sp", tri[:], tri_in[:], writes=[tri])
                k.dma("sp", idb32[:], identb_in[:], writes=[idb32])
                k.dma("sp", scw[:], ssd_cw[l], writes=[scw])
                k.dma("sp", scb[:], ssd_cb[l], writes=[scb])
                k.dma("sp", dsk[:], ssd_dsk[l], writes=[dsk])
                k.dma("sp", gn[:], ssd_gn[l], writes=[gn])
                k.dma("sp", eal[:], ssd_alog[l], writes=[eal])
                k.dma("sp", av.ap.rearrange("p n c -> p (n c)"), ssd_dtb[l], writes=[av])
                k.dma("sp", dtv[:], PDT.ap.rearrange("(n p) c -> p n c", p=128), reads=[PDT], writes=[dtv])
                k.op("dve", lambda e: e.tensor_scalar_mul(ntri[:], tri[:], -1.0), reads=[tri], writes=[ntri])
                k.op("dve", lambda e: e.memset(ones_f[:], 1.0), writes=[ones_f])
                k.op("dve", lambda e: e.tensor_copy(idb[:], idb32[:]), reads=[idb32], writes=[idb])
                k.op("dve", lambda e: e.tensor_tensor(dtv[:], dtv[:], av[:], ALU.add), reads=[dtv, av], writes=[dtv])
                k.op("act", lambda e: e.activation(dtv[:], dtv[:], AF.Exp), reads=[dtv], writes=[dtv])
                k.op("act", lambda e: e.activation(dtv[:], dtv[:], AF.Ln, bias=1.0), reads=[dtv], writes=[dtv])
                k.op("act", lambda e: e.activation(eal[:], eal[:], AF.Exp), reads=[eal], writes=[eal])
                k.op("dve", lambda e: e.scalar_tensor_tensor(av.ap.rearrange("p n c -> p (n c)"), dtv.ap.rearrange("p n c -> p (n c)"), -1.0,
                                                            eal[:], ALU.mult, ALU.mult), reads=[dtv, eal], writes=[av])
                SEGS = [(0, CTX), (CTX, T)]
                with ExitStack() as es1:
                    XT = sbt(nc, es1, "XT", [128, 4, T], BF16)
                    inb = Rot([sbt(nc, es1, f"cinb{i}", [128, T], BF16) for i in range(2)])
                    uu = Rot([sbt(nc, es1, f"cu{i}", [128, T], F32) for i in range(2)])
                    ptb = Rot([pst(nc, es1, f"ptb{i}", [128, 512], BF16) for i in range(3)])
                    for ct in range(8):
                        ib = inb.next()
                        u = uu.next()
                        k.dma("sp" if ct % 2 == 0 else "pool", ib[:], PF[2048 + ct * 128:2048 + (ct + 1) * 128, :], reads=[PF], writes=[ib])
                        k.op("dve", lambda e, ct=ct, ib=ib, u=u: e.tensor_scalar(u[:], ib[:], scw[:, ct, 2:3], scb[:, ct:ct + 1], ALU.mult, ALU.add),
                             reads=[ib, scw, scb], writes=[u])
                        for j in (0, 1, 3):
                            o = j - 2
                            for (s0, s1) in SEGS:
                                a_, b_ = (s0 - o, s1) if o < 0 else (s0, s1 - o)
                                k.op("dve", lambda e, ct=ct, j=j, a_=a_, b_=b_, o=o, ib=ib, u=u: e.scalar_tensor_tensor(
                                    u[:, a_:b_], ib[:, a_ + o:b_ + o], scw[:, ct, j:j + 1], u[:, a_:b_], ALU.mult, ALU.add),
                                    reads=[ib, scw, u], writes=[u])
                        dst = XT[:, ct, :] if ct < 4 else BCT[:, ct - 4, :]
                        dstt = XT if ct < 4 else BCT
                        k.op("act", lambda e, u=u, dst=dst: e.activation(dst, u[:], AF.Silu), reads=[u], writes=[dstt])
                    for tt in range(NT):
                        p1 = ptb.next()
                        for c4 in range(4):
                            k.op("pe", lambda e, p1=p1, c4=c4, tt=tt: e.transpose(p1[:, c4 * 128:(c4 + 1) * 128], XT[:, c4, tt * 128:(tt + 1) * 128], idb[:]),
                                 reads=[XT, idb], writes=[p1])
                        k.op("dve" if tt % 2 == 0 else "act", (lambda e, p1=p1, tt=tt: e.tensor_copy(Xtm[:, tt, :], p1[:])) if tt % 2 == 0 else
                             (lambda e, p1=p1, tt=tt: e.copy(Xtm[:, tt, :], p1[:])), reads=[p1], writes=[Xtm])
                        p2 = ptb.next()
                        for g in range(2):
                            k.op("pe", lambda e, p2=p2, g=g, tt=tt: e.transpose(p2[:, g * 128:(g + 1) * 128], BCT[:, g, tt * 128:(tt + 1) * 128], idb[:]),
                                 reads=[BCT, idb], writes=[p2])
                        k.op("act" if tt % 2 == 0 else "dve", (lambda e, p2=p2, tt=tt: e.copy(Btm[:, tt, :], p2[:, 0:256])) if tt % 2 == 0 else
                             (lambda e, p2=p2, tt=tt: e.tensor_copy(Btm[:, tt, :], p2[:, 0:256])), reads=[p2], writes=[Btm])
                    k.barrier()
                Yacc = sbt(nc, es, "Yacc", [128, NT, 512], BF16)
                with ExitStack() as es2:
                    S = sbt(nc, es2, "S", [128, 512], F32)
                    Sb = sbt(nc, es2, "Sb", [128, 512], BF16)
                    Rr = Rot([sbt(nc, es2, f"Rr{i}", [128, 8, 128], F32) for i in range(3)])
                    R2 = Rot([sbt(nc, es2, f"R2{i}", [128, 8, 128], F32) for i in range(2)])
                    Lx = Rot([sbt(nc, es2, f"Lx{i}", [128, 8, 128], F32) for i in range(3)])
                    Mt = Rot([sbt(nc, es2, f"Mt{i}", [128, 8, 128], BF16) for i in range(3)])
                    CBm = Rot([sbt(nc, es2, f"CBm{i}", [128, 2, 128], F32) for i in range(3)])
                    xdt = Rot([sbt(nc, es2, f"xdt{i}", [128, 512], BF16) for i in range(3)])
                    wx = Rot([sbt(nc, es2, f"wx{i}", [128, 512], BF16) for i in range(3)])
                    ecs = Rot([sbt(nc, es2, f"ecs{i}", [128, 8], F32) for i in range(4)])
                    decb = Rot([sbt(nc, es2, f"decb{i}", [128, 8], F32) for i in range(4)])
                    yo = Rot([sbt(nc, es2, f"yo{i}", [128, 512], F32) for i in range(2)])
                    PD = pst(nc, es2, "PD", [128, 1024])
                    PCB = pst(nc, es2, "PCB", [128, 512])
                    PYr = Rot([pst(nc, es2, f"PY{i}", [128, 512]) for i in range(2)])
                    PSr = Rot([pst(nc, es2, f"PS{i}", [128, 512]) for i in range(2)])
                    PYo = pst(nc, es2, "PYo", [128, 512])
                    first_y = [True] * NT

                    def ssd_s0(d, tt):
                        lend = 127 if d == 0 else 0
                        a_ap = av[:, tt, d * 8:(d + 1) * 8]
                        tsl = slice(tt * 128, (tt + 1) * 128)
                        r1 = Rr.next(); r2 = R2.next()
                        k.op("dve", lambda e: e.tensor_tensor(r1[:], tri[:, d, :].unsqueeze(1).to_broadcast([128, 8, 128]),
                                                              a_ap.unsqueeze(2).to_broadcast([128, 8, 128]), ALU.mult), reads=[tri, av], writes=[r1])
                        k.op("pool", lambda e: e.tensor_copy(r2[:], a_ap.unsqueeze(2).to_broadcast([128, 8, 128])), reads=[av], writes=[r2])
                        return (d, tt, tsl, lend, a_ap, r1, r2)

                    def ssd_s0m(d, tt, tsl, lend, a_ap, r1, r2):
                        for hf in range(2):
                            k.op("pe", lambda e, hf=hf: e.matmul(PD[:, hf * 512:(hf + 1) * 512], ones_f[:], r1[:, hf * 4:(hf + 1) * 4, :], start=True, stop=False),
                                 reads=[ones_f, r1], writes=[PD])
                            k.op("pe", lambda e, hf=hf: e.matmul(PD[:, hf * 512:(hf + 1) * 512], ntri[:, d, :], r2[:, hf * 4:(hf + 1) * 4, :], start=False, stop=True),
                                 reads=[ntri, r2], writes=[PD])
                        for g in range(2):
                            k.op("pe", lambda e, g=g: e.matmul(PCB[:, g * 128:(g + 1) * 128], BCT[:, g, tsl], BCT[:, 2 + g, tsl], start=True, stop=True),
                                 reads=[BCT], writes=[PCB])
                        k.op("pe", lambda e: e.matmul(PCB[:, 256:264], tri[:, d, :], a_ap, start=True, stop=True), reads=[tri, av], writes=[PCB])
                        k.op("pe", lambda e: e.matmul(PCB[:, 272:280], ones_f[:], a_ap, start=True, stop=True), reads=[ones_f, av], writes=[PCB])
                        return (d, tt, tsl, lend, a_ap)

                    def ssd_a(d, tt, tsl, lend, a_ap):
                        cbm = CBm.next()
                        k.op("dve", lambda e: e.tensor_tensor(cbm[:], PCB.ap[:, 0:256].rearrange("p (g l) -> p g l", g=2),
                                                              tri[:, d, :].unsqueeze(1).to_broadcast([128, 2, 128]), ALU.mult), reads=[PCB, tri], writes=[cbm])
                        lx = Lx.next()
                        k.op("dve", lambda e: e.tensor_tensor(lx[:], PD.ap.rearrange("p (h l) -> p h l", h=8), tri[:, d, :].unsqueeze(1).to_broadcast([128, 8, 128]), ALU.mult),
                             reads=[PD, tri], writes=[lx])
                        k.op("act", lambda e: e.activation(lx[:], lx[:], AF.Exp), reads=[lx], writes=[lx])
                        ec = ecs.next(); db = decb.next()
                        k.op("act", lambda e: e.activation(ec[:], PCB[:, 256:264], AF.Exp), reads=[PCB], writes=[ec])
                        k.op("act", lambda e: e.activation(db[:], PCB[:, 272:280], AF.Exp), reads=[PCB], writes=[db])
                        mt = Mt.next()
                        for g in range(2):
                            k.op("dve", lambda e, g=g: e.tensor_tensor(
                                mt[:, g * 4:(g + 1) * 4, :], lx[:, g * 4:(g + 1) * 4, :], cbm[:, g, :].unsqueeze(1).to_broadcast([128, 4, 128]), ALU.mult),
                                reads=[lx, cbm], writes=[mt])
                        xd = xdt.next()
                        k.op("dve", lambda e: e.tensor_tensor(
                            xd.ap.rearrange("p (h q) -> p h q", h=8), Xtm[:, tt, :].rearrange("p (h q) -> p h q", h=8),
                            dtv[:, tt, d * 8:(d + 1) * 8].unsqueeze(2).to_broadcast([128, 8, 64]), ALU.mult), reads=[Xtm, dtv], writes=[xd])
                        w_ = wx.next()
                        k.op("dve", lambda e: e.tensor_tensor(
                            w_.ap.rearrange("p (h q) -> p h q", h=8), xd.ap.rearrange("p (h q) -> p h q", h=8),
                            lx[:, :, lend:lend + 1].to_broadcast([128, 8, 64]), ALU.mult), reads=[xd, lx], writes=[w_])
                        return (d, tt, tsl, ec, db, mt, xd, w_)

                    def ssd_a2(d, tt, tsl, ec, db, mt, xd, w_):
                        PY = PYr.next(); PS = PSr.next()
                        for h in range(8):
                            k.op("pe", lambda e, h=h: e.matmul(PY[:, h * 64:(h + 1) * 64], mt[:, h, :], xd[:, h * 64:(h + 1) * 64], start=True, stop=True),
                                 reads=[mt, xd], writes=[PY])
                        for g in range(2):
                            k.op("pe", lambda e, g=g: e.matmul(PS[:, g * 256:(g + 1) * 256], Btm[:, tt, g * 128:(g + 1) * 128],
                                                               w_[:, g * 256:(g + 1) * 256], start=True, stop=True), reads=[Btm, w_], writes=[PS])
                        return (d, tt, tsl, ec, db, PY, PS)

                    def ssd_b(d, tt, tsl, ec, db, PY, PS):
                        for g in range(2):
                            k.op("pe", lambda e, g=g: e.matmul(PYo[:, g * 256:(g + 1) * 256], BCT[:, 2 + g, tsl], Sb[:, g * 256:(g + 1) * 256],
                                                               start=True, stop=True), reads=[BCT, Sb], writes=[PYo])
                        y_ = yo.next()
                        k.op("dve", lambda e: e.tensor_tensor(y_.ap.rearrange("p (h q) -> p h q", h=8), PYo.ap.rearrange("p (h q) -> p h q", h=8),
                                                              ec[:].unsqueeze(2).to_broadcast([128, 8, 64]), ALU.mult), reads=[PYo, ec], writes=[y_])
                        if first_y[tt]:
                            first_y[tt] = False
                            k.op("dve", lambda e: e.tensor_tensor(Yacc[:, tt, :], PY[:], y_[:], ALU.add), reads=[PY, y_], writes=[Yacc])
                        else:
                            k.op("pool", lambda e: e.tensor_tensor(Yacc[:, tt, :], Yacc[:, tt, :], y_[:], ALU.add), reads=[Yacc, y_], writes=[Yacc])
                            k.op("dve", lambda e: e.tensor_tensor(Yacc[:, tt, :], Yacc[:, tt, :], PY[:], ALU.add), reads=[Yacc, PY], writes=[Yacc])
                        k.op("dve", lambda e: e.tensor_tensor(S.ap.rearrange("p (h q) -> p h q", h=8), S.ap.rearrange("p (h q) -> p h q", h=8),
                                                              db[:].unsqueeze(2).to_broadcast([128, 8, 64]), ALU.mult), reads=[S, db], writes=[S])
                        k.op("dve", lambda e: e.tensor_tensor(S[:], S[:], PS[:], ALU.add), reads=[S, PS], writes=[S])
                        k.op("pool", lambda e: e.tensor_copy(Sb[:], S[:]), reads=[S], writes=[Sb])

                    for d in range(2):
                        order = list(range(NT)) if d == 0 else [1, 0] + list(range(NT - 1, 1, -1))
                        k.op("dve", lambda e: e.memset(S[:], 0.0), writes=[S])
                        k.op("pool", lambda e: e.memset(Sb[:], 0.0), writes=[Sb])
                        n_o = len(order)
                        q0 = []; qa = []; qb = []
                        for step in range(n_o + 3):
                            s0e = ssd_s0(d, order[step]) if step < n_o else None
                            if 1 <= step <= n_o:
                                qa.append(ssd_a(*q0.pop(0)))
                            if s0e is not None:
                                q0.append(ssd_s0m(*s0e))
                            if 2 <= step <= n_o + 1:
                                qb.append(ssd_a2(*qa.pop(0)))
                            if 3 <= step <= n_o + 2:
                                ssd_b(*qb.pop(0))
                    k.barrier()
                with ExitStack() as es2:
                    zt = Rot([sbt(nc, es2, f"zt{i}", [128, 512], BF16) for i in range(3)])
                    sz = Rot([sbt(nc, es2, f"sz{i}", [128, 512], F32) for i in range(3)])
                    tq = Rot([sbt(nc, es2, f"tq{i}", [128, 512], F32) for i in range(3)])
                    ssq = Rot([sbt(nc, es2, f"ssq{i}", [128, 2], F32) for i in range(2)])
                    ynb = Rot([sbt(nc, es2, f"ynb{i}", [128, 512], BF16) for i in range(2)])
                    yct = Rot([sbt(nc, es2, f"yct{i}", [128, 512], BF16) for i in range(3)])
                    ptc = Rot([pst(nc, es2, f"ptc{i}", [128, 512], BF16) for i in range(2)])
                    def p3_a(tt):
                        z_ = zt.next(); s_ = sz.next(); t_ = tq.next()
                        k.dma("sp", z_[:], PZ[tt * 128:(tt + 1) * 128, :], reads=[PZ], writes=[z_])
                        k.op("act", lambda e: e.activation(s_[:], z_[:], AF.Silu), reads=[z_], writes=[s_])
                        k.op("dve", lambda e: e.tensor_tensor(t_.ap.rearrange("p (h q) -> p h q", h=8), Xtm[:, tt, :].rearrange("p (h q) -> p h q", h=8),
                                                              dsk[:].unsqueeze(2).to_broadcast([128, 8, 64]), ALU.mult), reads=[Xtm, dsk], writes=[t_])
                        k.op("pool", lambda e: e.tensor_tensor(t_[:], t_[:], Yacc[:, tt, :], ALU.add), reads=[t_, Yacc], writes=[t_])
                        k.op("dve", lambda e: e.tensor_tensor(t_[:], t_[:], s_[:], ALU.mult), reads=[t_, s_], writes=[t_])
                        return (tt, s_, t_)

                    def p3_b(tt, s_, t_):
                        q_ = ssq.next(); yn = ynb.next()
                        for g in range(2):
                            k.op("act", lambda e, g=g: e.activation(s_[:, g * 256:(g + 1) * 256], t_[:, g * 256:(g + 1) * 256], AF.Square,
                                                                    accum_out=q_[:, g:g + 1]), reads=[t_], writes=[s_, q_])
                        k.op("dve", lambda e: e.tensor_scalar(q_[:], q_[:], 1.0 / 256, RMS_EPS, ALU.mult, ALU.add), reads=[q_], writes=[q_])
                        k.op("act", lambda e: e.activation(q_[:], q_[:], AF.Sqrt), reads=[q_], writes=[q_])
                        k.op("dve", lambda e: e.reciprocal(q_[:], q_[:]), reads=[q_], writes=[q_])
                        for g in range(2):
                            k.op("dve", lambda e, g=g: e.scalar_tensor_tensor(
                                yn[:, g * 256:(g + 1) * 256], t_[:, g * 256:(g + 1) * 256], q_[:, g:g + 1], gn[:, g * 256:(g + 1) * 256], ALU.mult, ALU.mult),
                                reads=[t_, q_, gn], writes=[yn])
                        pc = ptc.next()
                        for c4 in range(4):
                            k.op("pe", lambda e, c4=c4: e.transpose(pc[:, c4 * 128:(c4 + 1) * 128], yn[:, c4 * 128:(c4 + 1) * 128], idb[:]),
                                 reads=[yn, idb], writes=[pc])
                        yc_ = yct.next()
                        k.op("act", lambda e: e.copy(yc_[:], pc[:]), reads=[pc], writes=[yc_])
                        k.dma("pool", YT.ap[1024:1536, tt * 128:(tt + 1) * 128].rearrange("(c p) t -> p c t", p=128),
                              yc_.ap.rearrange("p (c t) -> p c t", c=4), reads=[yc_], writes=[YT])

                    pend3 = p3_a(0)
                    for tt in range(NT):
                        nxt3 = p3_a(tt + 1) if tt + 1 < NT else None
                        p3_b(*pend3)
                        pend3 = nxt3
                    k.barrier()
            if stop_after == "ssd":
                break

            def rowbc(es_, dst, c0, j, ones_f_, pbc_):
                dg = sbt(nc, es_, "dg", [128, 4, 128], F32)
                for hf in range(2):
                    for c in range(4):
                        k.op("dve", lambda e, c=c, hf=hf: e.tensor_scalar_mul(dg[:, c, :], ident[:], mod[:, c0 + hf * 4 + c, j:j + 1]),
                             reads=[ident, mod], writes=[dg])
                    k.op("pe", lambda e: e.matmul(pbc_[:], ones_f_[:], dg.ap.rearrange("p c q -> p (c q)"), start=True, stop=True),
                         reads=[ones_f_, dg], writes=[pbc_])
                    k.op("act", lambda e, hf=hf: e.copy(dst[:, hf * 512:(hf + 1) * 512], pbc_[:]), reads=[pbc_], writes=[dst])

            def ln_tile(v, junk, st4, gbc, bbc):
                k.op("dve", lambda e: e.reduce_sum(st4[:, 0:1], v[:], AX.X), reads=[v], writes=[st4])
                k.op("dve", lambda e: e.tensor_scalar_mul(st4[:, 1:2], st4[:, 0:1], -1.0 / D), reads=[st4], writes=[st4])
                k.op("act", lambda e: e.activation(junk[:], v[:], AF.Square, bias=st4[:, 1:2], accum_out=st4[:, 2:3]), reads=[v, st4], writes=[junk, st4])
                k.op("dve", lambda e: e.tensor_scalar(st4[:, 3:4], st4[:, 2:3], 1.0 / D, LN_EPS, ALU.mult, ALU.add), reads=[st4], writes=[st4])
                k.op("act", lambda e: e.activation(st4[:, 3:4], st4[:, 3:4], AF.Sqrt), reads=[st4], writes=[st4])
                k.op("dve", lambda e: e.reciprocal(st4[:, 3:4], st4[:, 3:4]), reads=[st4], writes=[st4])
                k.op("dve", lambda e: e.tensor_scalar(v[:], v[:], st4[:, 1:2], st4[:, 3:4], ALU.add, ALU.mult), reads=[v, st4], writes=[v])
                k.op("pool", lambda e: e.tensor_tensor(v[:], v[:], gbc[:], ALU.mult), reads=[v, gbc], writes=[v])
                k.op("pool", lambda e: e.tensor_tensor(v[:], v[:], bbc[:], ALU.add), reads=[v, bbc], writes=[v])

            last = (l == DEPTH - 1)
            if stages is None or "merge" in stages:
              with ExitStack() as es:
                ones_f = sbt(nc, es, "ones_f3", [128, 128], F32)
                k.op("dve", lambda e: e.memset(ones_f[:], 1.0), writes=[ones_f])
                wbr = sbt(nc, es, "wbr", [128, 12, D], BF16)
                wo = sbt(nc, es, "wo", [128, KC, D], BF16)
                g1bc = [sbt(nc, es, f"g1bc{j}", [128, D], F32) for j in range(2)]
                lng = sbt(nc, es, "lng", [128, D], F32)
                lnb = sbt(nc, es, "lnb", [128, D], F32)
                k.dma("sp", lng[:], ln_gb[l, 0], writes=[lng])
                k.dma("sp", lnb[:], ln_gb[l, 1], writes=[lnb])
                with ExitStack() as es1:
                    stg32 = Rot([sbt(nc, es1, f"wstg{i}", [128, 4, D], F32) for i in range(2)])
                    pbc = pst(nc, es1, "pbc", [128, 512])
                    for i in range(3):
                        a = stg32.next()
                        k.dma("sp", a[:], w_br[l, :, i * 4:(i + 1) * 4, :], writes=[a])
                        k.op("dve" if i % 2 == 0 else "pool", lambda e, a=a, i=i: e.tensor_copy(wbr[:, i * 4:(i + 1) * 4, :], a[:]), reads=[a], writes=[wbr])
                    for i in range(2):
                        a = stg32.next()
                        k.dma("sp", a[:], w_o[l, :, i * 4:(i + 1) * 4, :], writes=[a])
                        k.op("pool" if i % 2 == 0 else "dve", lambda e, a=a, i=i: e.tensor_copy(wo[:, i * 4:(i + 1) * 4, :], a[:]), reads=[a], writes=[wo])
                    for j in range(2):
                        rowbc(es1, g1bc[j], 16, j, ones_f, pbc)
                    k.barrier()
                ysb = Rot([sbt(nc, es, f"ysb{i}", [128, 12, 512], BF16) for i in range(2)])
                gsb = Rot([sbt(nc, es, f"gsb{i}", [128, 24, 512], BF16) for i in range(2)])
                mrg = Rot([sbt(nc, es, f"mrg{i}", [128, KC, 512], BF16) for i in range(2)])
                mm = Rot([sbt(nc, es, f"mm{i}", [128, 512], F32) for i in range(6)])
                xv = Rot([sbt(nc, es, f"xv{i}", [128, D], F32) for i in range(3)])
                tv = Rot([sbt(nc, es, f"tv{i}", [128, D], F32) for i in range(3)])
                junk = sbt(nc, es, "junk", [128, D], F32)
                st4 = Rot([sbt(nc, es, f"st4{i}", [128, 8], F32) for i in range(3)])
                pbr = Rot([pst(nc, es, f"pbr{i}", [128, 512]) for i in range(4)])
                pmx = Rot([pst(nc, es, f"pmx{i}", [128, 512]) for i in range(4)])
                def mg_a(t0, n):
                    y_ = ysb.next(); g_ = gsb.next(); mg = mrg.next()
                    k.dma("sp", y_[:, :, :n], YT.ap[:, t0:t0 + n].rearrange("(c p) t -> p c t", p=128), reads=[YT], writes=[y_])
                    k.dma("sp", g_[:, :, :n], PF.ap[3072:6144, t0:t0 + n].rearrange("(c p) t -> p c t", p=128), reads=[PF], writes=[g_])
                    k.op("act", lambda e: e.activation(g_[:, :, :n], g_[:, :, :n], AF.Sigmoid), reads=[g_], writes=[g_])
                    for dt_ in range(8):
                        ps3 = []
                        for br in range(3):
                            pb = pbr.next()
                            for kc in range(4):
                                k.op("pe", lambda e, pb=pb, br=br, kc=kc, dt_=dt_: e.matmul(
                                    pb[:, :n], wbr[:, br * 4 + kc, dt_ * 128:(dt_ + 1) * 128], y_[:, br * 4 + kc, :n],
                                    start=(kc == 0), stop=(kc == 3)), reads=[wbr, y_], writes=[pb])
                            ps3.append(pb)
                        m3 = [mm.next() for _ in range(3)]
                        for br in range(3):
                            k.op("dve", lambda e, br=br, m3=m3, ps3=ps3, dt_=dt_: e.tensor_tensor(
                                m3[br][:, :n], ps3[br][:, :n], g_[:, br * 8 + dt_, :n], ALU.mult), reads=[ps3[br], g_], writes=[m3[br]])
                        k.op("pool", lambda e, m3=m3: e.tensor_tensor(m3[0][:, :n], m3[0][:, :n], m3[1][:, :n], ALU.add), reads=[m3[0], m3[1]], writes=[m3[0]])
                        k.op("dve", lambda e, m3=m3, dt_=dt_: e.tensor_tensor(mg[:, dt_, :n], m3[0][:, :n], m3[2][:, :n], ALU.add),
                             reads=[m3[0], m3[2]], writes=[mg])
                    return (t0, n, mg)

                def mg_b(t0, n, mg):
                    j = 1 if t0 == 0 else 0
                    for tl in range(n // 128):
                        tt = t0 // 128 + tl
                        x_ = xv.next(); v_ = tv.next(); s4 = st4.next()
                        if l == 0:
                            k.dma("sp", x_[:], xin[tt * 128:(tt + 1) * 128, :], reads=[xin], writes=[x_])
                        else:
                            k.dma("sp", x_[:], xs_t[tt][:], reads=[xs_t[tt]], writes=[x_])
                        for eh in range(2):
                            pm_ = pmx.next()
                            for kc in range(KC):
                                k.op("pe", lambda e, pm_=pm_, kc=kc, eh=eh: e.matmul(
                                    pm_[:], mg[:, kc, tl * 128:(tl + 1) * 128], wo[:, kc, eh * 512:(eh + 1) * 512],
                                    start=(kc == 0), stop=(kc == KC - 1)), reads=[mg, wo], writes=[pm_])
                            k.op("dve", lambda e, pm_=pm_, eh=eh: e.tensor_tensor(v_[:, eh * 512:(eh + 1) * 512], pm_[:], g1bc[j][:, eh * 512:(eh + 1) * 512], ALU.mult),
                                 reads=[pm_, g1bc[j]], writes=[v_])
                        k.op("dve", lambda e: e.scalar_tensor_tensor(v_[:], x_[:], DN_ALPHA, v_[:], ALU.mult, ALU.add), reads=[x_, v_], writes=[v_])
                        k.op("dve", lambda e: e.reduce_sum(s4[:, 0:1], v_[:], AX.X), reads=[v_], writes=[s4])
                        k.op("dve", lambda e: e.tensor_scalar_mul(s4[:, 1:2], s4[:, 0:1], -1.0 / D), reads=[s4], writes=[s4])
                        k.op("act", lambda e: e.activation(junk[:], v_[:], AF.Square, bias=s4[:, 1:2], accum_out=s4[:, 2:3]), reads=[v_, s4], writes=[junk, s4])
                        k.op("dve", lambda e: e.tensor_scalar(s4[:, 3:4], s4[:, 2:3], 1.0 / D, LN_EPS, ALU.mult, ALU.add), reads=[s4], writes=[s4])
                        k.op("act", lambda e: e.activation(s4[:, 3:4], s4[:, 3:4], AF.Sqrt), reads=[s4], writes=[s4])
                        k.op("dve", lambda e: e.reciprocal(s4[:, 3:4], s4[:, 3:4]), reads=[s4], writes=[s4])
                        k.op("dve", lambda e: e.tensor_tensor(s4[:, 4:5], s4[:, 1:2], s4[:, 3:4], ALU.mult), reads=[s4], writes=[s4])
                        k.op("act", lambda e: e.activation(v_[:], v_[:], AF.Identity, bias=s4[:, 4:5], scale=s4[:, 3:4]), reads=[v_, s4], writes=[v_])
                        k.op("dve", lambda e: e.tensor_tensor(v_[:], v_[:], lng[:], ALU.mult), reads=[v_, lng], writes=[v_])
                        k.op("pool", lambda e: e.tensor_tensor(v_[:], v_[:], lnb[:], ALU.add), reads=[v_, lnb], writes=[v_])
                        k.dma("pool", xs_t[tt][:], v_[:], reads=[v_], writes=[xs_t[tt]])

                mchs = [c for c in CHUNKS if not (last and c[0] == 0)]
                pendm = mg_a(*mchs[0])
                for im in range(len(mchs)):
                    nxtm = mg_a(*mchs[im + 1]) if im + 1 < len(mchs) else None
                    mg_b(*pendm)
                    pendm = nxtm
                k.barrier()
            if stop_after == "merge":
                break

            if stages is None or "moe" in stages:
              tts = list(range(NT))
              with ExitStack() as es:
                ones_f = sbt(nc, es, "ones_f4", [128, 128], F32)
                k.op("dve", lambda e: e.memset(ones_f[:], 1.0), writes=[ones_f])
                wr = sbt(nc, es, "wr", [128, KC, E], F32)
                rb = sbt(nc, es, "rb", [128, E], F32)
                iot = sbt(nc, es, "iot", [128, 64], F32)
                iop = sbt(nc, es, "iop", [128, 1], F32)
                thr = sbt(nc, es, "thr", [128, NBR], F32)
                ust = sbt(nc, es, "ust", [128, 128], F32)
                EM = sbt(nc, es, "EM", [128, NT, 64], F32)
                WW = sbt(nc, es, "WW", [128, NT, 64], F32)
                IX = sbt(nc, es, "IX", [128, NT, 8], F32)
                sc2 = [sbt(nc, es, f"sc2bc{j}", [128, D], F32) for j in range(2)]
                sh2 = [sbt(nc, es, f"sh2bc{j}", [128, D], F32) for j in range(2)]
                k.dma("sp", wr[:], w_rt[l], writes=[wr])
                k.dma("sp", rb[:], r_bias[l], writes=[rb])
                k.dma("sp", iot[:], iota64[:], writes=[iot])
                k.dma("sp", iop[:], iotap[:], writes=[iop])
                k.dma("sp", thr[:], thr_in[:], writes=[thr])
                k.dma("sp", ust[:], ustrict[:], writes=[ust])
                pbc = pst(nc, es, "pbc3", [128, 512])
                with ExitStack() as es1:
                    for j in range(2):
                        rowbc(es1, sc2[j], 32, j, ones_f, pbc)
                        rowbc(es1, sh2[j], 24, j, ones_f, pbc)
                    for j in range(2):
                        k.op("dve", lambda e, j=j: e.tensor_scalar_add(sc2[j][:], sc2[j][:], 1.0), reads=[sc2[j]], writes=[sc2[j]])
                    k.barrier()
                zt_ = sbt(nc, es, "zt_", [128, NROWS // 128], U32)
                k.op("dve", lambda e: e.memset(zt_[:], 0), writes=[zt_])
                ROWTOK.multi = False
                k.dma("sp", ROWTOK.ap[0:NROWS, :].rearrange("(p a) o -> p (a o)", p=128), zt_[:], reads=[zt_], writes=[ROWTOK])
                rsh = sbt(nc, es, "rsh", [128, NT], U32)
                k.dma("sp", rsh[:], rowtok_sh[:], writes=[rsh])
                k.dma("sp", ROWTOK.ap[NBR * 256:NROWS, :].rearrange("(p a) o -> p (a o)", p=128), rsh[:], reads=[rsh], writes=[ROWTOK])
                k._need("pool", [ROWTOK.w])
                ROWTOK.multi = True
                ROWTOK.wset = {id(ROWTOK.w[0]): ROWTOK.w}
                ROWTOK.w = None
                xt = Rot([sbt(nc, es, f"ext{i}", [128, D], F32) for i in range(4)])
                h2f = Rot([sbt(nc, es, f"h2f{i}", [128, KC, 128], F32) for i in range(3)])
                h2t = Rot([sbt(nc, es, f"h2t{i}", [128, D], F32) for i in range(2)])
                h2b = Rot([sbt(nc, es, f"h2b{i}", [128, D], BF16) for i in range(2)])
                ptr = Rot([pst(nc, es, f"eptr{i}", [128, 512]) for i in range(3)])
                prt = Rot([pst(nc, es, f"prt{i}", [128, 512]) for i in range(2)])
                pcnt = pst(nc, es, "pcnt", [128, 512])
                rw = Rot([sbt(nc, es, f"rw{i}", [128, 64], F32) for i in range(10)])
                r8 = Rot([sbt(nc, es, f"r8{i}", [128, 8], F32) for i in range(9)])
                i8 = Rot([sbt(nc, es, f"i8{i}", [128, 8], U32) for i in range(2)])
                v3 = lambda t_: t_.ap.rearrange("p (g i) -> p g i", g=8)
                for tt in tts:
                    j = 1 if tt < 2 else 0
                    x_ = xt.next(); hf_ = h2f.next(); ht_ = h2t.next(); hb_ = h2b.next()
                    k.dma("sp", x_[:], xs_t[tt][:], reads=[xs_t[tt]], writes=[x_])
                    k.op("pool", lambda e, x_=x_, ht_=ht_, j=j: e.tensor_tensor(ht_[:], x_[:], sc2[j][:], ALU.mult), reads=[x_, sc2[j]], writes=[ht_])
                    k.op("pool", lambda e, hb_=hb_, ht_=ht_, j=j: e.tensor_tensor(hb_[:], ht_[:], sh2[j][:], ALU.add), reads=[ht_, sh2[j]], writes=[hb_])
                    k.dma("pool", H2TM[tt * 128:(tt + 1) * 128, :], hb_[:], reads=[hb_], writes=[H2TM])
                    for half in range(2):
                        p_t = ptr.next()
                        for q4 in range(4):
                            kc = half * 4 + q4
                            k.op("pe", lambda e, p_t=p_t, x_=x_, kc=kc, q4=q4: e.transpose(
                                p_t[:, q4 * 128:(q4 + 1) * 128], x_[:, kc * 128:(kc + 1) * 128], ident[:]), reads=[x_, ident], writes=[p_t])
                        for q4 in range(4):
                            kc = half * 4 + q4
                            if q4 % 2 == 0:
                                k.op("act", lambda e, p_t=p_t, kc=kc, q4=q4, j=j, hf_=hf_: e.activation(
                                    hf_[:, kc, :], p_t[:, q4 * 128:(q4 + 1) * 128], AF.Identity,
                                    bias=mod[:, 24 + kc, j:j + 1], scale=mod1[:, 32 + kc, j:j + 1]), reads=[p_t, mod, mod1], writes=[hf_])
                            else:
                                k.op("dve", lambda e, p_t=p_t, kc=kc, q4=q4, j=j, hf_=hf_: e.tensor_scalar(
                                    hf_[:, kc, :], p_t[:, q4 * 128:(q4 + 1) * 128], mod1[:, 32 + kc, j:j + 1], mod[:, 24 + kc, j:j + 1],
                                    ALU.mult, ALU.add), reads=[p_t, mod, mod1], writes=[hf_])
                    pr = prt.next()
                    for kc in range(KC):
                        k.op("pe", lambda e, pr=pr, hf_=hf_, kc=kc: e.matmul(pr[:, :E], hf_[:, kc, :], wr[:, kc, :], start=(kc == 0), stop=(kc == KC - 1)),
                             reads=[hf_, wr], writes=[pr])
                    sc = rw.next(); sel = rw.next(); eq = rw.next(); sel2 = rw.next(); selm = rw.next()
                    mx1 = r8.next(); mx2 = r8.next(); gs = r8.next(); srt = r8.next(); gm = r8.next(); t8 = r8.next(); sm = r8.next()
                    ix = i8.next()
                    k.op("act", lambda e, sc=sc, pr=pr: e.activation(sc[:], pr[:, :E], AF.Sigmoid), reads=[pr], writes=[sc])
                    k.op("dve", lambda e, sel=sel, sc=sc: e.tensor_tensor(sel[:], sc[:], rb[:], ALU.add), reads=[sc, rb], writes=[sel])
                    k.op("dve", lambda e, mx1=mx1, sel=sel: e.reduce_max(mx1[:], v3(sel), AX.X), reads=[sel], writes=[mx1])
                    k.op("dve", lambda e, eq=eq, sel=sel, mx1=mx1: e.tensor_tensor(v3(eq), v3(sel), mx1[:].unsqueeze(2).to_broadcast([128, 8, 8]), ALU.is_equal),
                         reads=[sel, mx1], writes=[eq])
                    k.op("dve", lambda e, sel2=sel2, eq=eq, sel=sel: e.scalar_tensor_tensor(sel2[:], eq[:], -1e9, sel[:], ALU.mult, ALU.add),
                         reads=[eq, sel], writes=[sel2])
                    k.op("dve", lambda e, mx2=mx2, sel2=sel2: e.reduce_max(mx2[:], v3(sel2), AX.X), reads=[sel2], writes=[mx2])
                    k.op("dve", lambda e, gs=gs, mx1=mx1, mx2=mx2: e.tensor_tensor(gs[:], mx1[:], mx2[:], ALU.add), reads=[mx1, mx2], writes=[gs])
                    k.op("dve", lambda e, srt=srt, gs=gs: e.max(srt[:], gs[:]), reads=[gs], writes=[srt])
                    k.op("dve", lambda e, gm=gm, gs=gs, srt=srt: e.tensor_scalar(gm[:], gs[:], srt[:, 3:4], None, ALU.is_ge), reads=[gs, srt], writes=[gm])
                    k.op("dve", lambda e, selm=selm, sel=sel, gm=gm: e.scalar_tensor_tensor(v3(selm), v3(sel), 10.0, gm[:].unsqueeze(2).to_broadcast([128, 8, 8]),
                                                                                           ALU.add, ALU.mult), reads=[sel, gm], writes=[selm])
                    k.op("dve", lambda e, t8=t8, selm=selm: e.max(t8[:], selm[:]), reads=[selm], writes=[t8])
                    k.op("dve", lambda e, ix=ix, t8=t8, selm=selm: e.max_index(ix[:], t8[:], selm[:]), reads=[selm, t8], writes=[ix])
                    k.op("dve", lambda e, ix=ix, tt=tt: e.tensor_copy(IX[:, tt, :], ix[:]), reads=[ix], writes=[IX])
                    k.op("dve", lambda e, selm=selm, t8=t8, tt=tt: e.tensor_scalar(EM[:, tt, :], selm[:], t8[:, 7:8], None, ALU.is_ge), reads=[selm, t8], writes=[EM])
                    k.op("dve", lambda e, sc=sc, tt=tt: e.tensor_tensor(WW[:, tt, :], sc[:], EM[:, tt, :], ALU.mult), reads=[sc, EM], writes=[WW])
                    k.op("dve", lambda e, sm=sm, tt=tt: e.reduce_sum(sm[:, 0:1], WW[:, tt, :], AX.X), reads=[WW], writes=[sm])
                    k.op("dve", lambda e, sm=sm: e.reciprocal(sm[:, 1:2], sm[:, 0:1]), reads=[sm], writes=[sm])
                    k.op("dve", lambda e, sm=sm, tt=tt: e.tensor_scalar(WW[:, tt, :], WW[:, tt, :], sm[:, 1:2], 2.5, ALU.mult, ALU.mult), reads=[WW, sm], writes=[WW])
                    k.op("pe", lambda e, tt=tt: e.matmul(pcnt[:, 0:64], ones_f[:], EM[:, tt, :], start=(tt == tts[0]), stop=(tt == tts[-1])),
                         reads=[ones_f, EM], writes=[pcnt])
                cnt = sbt(nc, es, "cnt", [128, 64], F32)
                pad = sbt(nc, es, "pad", [128, 64], F32)
                pend = sbt(nc, es, "pend", [128, 64], F32)
                carry = sbt(nc, es, "carry", [128, 64], F32)
                one64 = sbt(nc, es, "one64", [128, 64], F32)
                k.op("dve", lambda e: e.memset(one64[:], 1.0), writes=[one64])
                cmp2 = sbt(nc, es, "cmp2", [128, 64, 18], F32)
                k.op("dve", lambda e: e.tensor_copy(cnt[:], pcnt[:, 0:64]), reads=[pcnt], writes=[cnt])
                k.op("dve", lambda e: e.tensor_tensor(cmp2[:], cnt[:].unsqueeze(2).to_broadcast([128, 64, 18]),
                                                      thr[:, 0:18].unsqueeze(1).to_broadcast([128, 64, 18]), ALU.is_gt), reads=[cnt, thr], writes=[cmp2])
                k.op("dve", lambda e: e.reduce_sum(pad[:], cmp2[:], AX.X), reads=[cmp2], writes=[pad])
                k.op("dve", lambda e: e.tensor_scalar_mul(pad[:], pad[:], 256.0), reads=[pad], writes=[pad])
                k.op("dve", lambda e: e.tensor_tensor_scan(pend[:], one64[:], pad[:], 0.0, ALU.mult, ALU.add), reads=[one64, pad], writes=[pend])
                k.op("dve", lambda e: e.tensor_tensor(carry[:], pend[:], pad[:], ALU.subtract), reads=[pend, pad], writes=[carry])
                BCH = 50
                cmp_ = sbt(nc, es, "cmp_", [128, BCH, 64], F32)
                bke = sbt(nc, es, "bke", [128, NBLK], F32)
                for c in range(NBR // BCH):
                    k.op("dve", lambda e, c=c: e.tensor_tensor(cmp_[:], pend[:].unsqueeze(1).to_broadcast([128, BCH, 64]),
                                                               thr[:, c * BCH:(c + 1) * BCH].unsqueeze(2).to_broadcast([128, BCH, 64]), ALU.is_le),
                         reads=[pend, thr], writes=[cmp_])
                    k.op("dve", lambda e, c=c: e.reduce_sum(bke[:, c * BCH:(c + 1) * BCH], cmp_[:], AX.X), reads=[cmp_], writes=[bke])
                k.op("dve", lambda e: e.memset(bke[:, NBR:NBLK], 64.0), writes=[bke])
                k.op("dve", lambda e: e.tensor_scalar(bke[:, 0:NBR], bke[:, 0:NBR], 63.0, None, ALU.min), reads=[bke], writes=[bke])
                k.op("dve", lambda e: e.tensor_scalar(bke[:], bke[:], 128.0, iop[:, 0:1], ALU.mult, ALU.add), reads=[bke, iop], writes=[bke])
                k.op("dve", lambda e: e.tensor_copy(IDXW[:], bke[:]), reads=[bke], writes=[IDXW])
                tokid = sbt(nc, es, "tokid", [128, 1], U32)
                tokf = sbt(nc, es, "tokf", [128, 1], F32)
                dfu = Rot([sbt(nc, es, f"dfu{i}", [128, 64], F32) for i in range(2)])
                junk64 = sbt(nc, es, "junk64", [128, 64], F32)
                d8f = Rot([sbt(nc, es, f"d8f{i}", [128, 8], F32) for i in range(2)])
                ppf = prt
                for tt in tts:
                    pp_ = ppf.next(); df = dfu.next(); d8 = d8f.next()
                    k.op("pe", lambda e, pp_=pp_, tt=tt: e.matmul(pp_[:, 0:64], ust[:], EM[:, tt, :], start=True, stop=True), reads=[ust, EM], writes=[pp_])
                    k.op("pe", lambda e, pp_=pp_, tt=tt: e.matmul(pp_[:, 64:128], ones_f[:], EM[:, tt, :], start=True, stop=True), reads=[ones_f, EM], writes=[pp_])
                    k.op("dve", lambda e, pp_=pp_, df=df: e.tensor_tensor(df[:], pp_[:, 0:64], carry[:], ALU.add), reads=[pp_, carry], writes=[df])
                    k.op("dve", lambda e, pp_=pp_: e.tensor_tensor(carry[:], carry[:], pp_[:, 64:128], ALU.add), reads=[pp_, carry], writes=[carry])
                    k.op("dve", lambda e, d8=d8: e.memset(d8[:], 0.0), writes=[d8])
                    k.op("dve", lambda e, tt=tt: e.memset(W8[:, tt, :], 0.0), writes=[W8])
                    for kk in range(8):
                        k.op("dve", lambda e, df=df, d8=d8, tt=tt, kk=kk: e.scalar_tensor_tensor(
                            junk64[:], iot[:], IX[:, tt, kk:kk + 1], df[:], ALU.is_equal, ALU.mult, accum_out=d8[:, kk:kk + 1]),
                            reads=[iot, IX, df], writes=[junk64, d8])
                        k.op("dve", lambda e, tt=tt, kk=kk: e.scalar_tensor_tensor(
                            junk64[:], iot[:], IX[:, tt, kk:kk + 1], WW[:, tt, :], ALU.is_equal, ALU.mult, accum_out=W8[:, tt, kk:kk + 1]),
                            reads=[iot, IX, WW], writes=[junk64, W8])
                    k.op("dve", lambda e, d8=d8, tt=tt: e.tensor_copy(D8[:, tt, :], d8[:]), reads=[d8], writes=[D8])
                    k.op("dve", lambda e, tt=tt: e.tensor_scalar_add(tokf[:], iop[:], float(tt * 128)), reads=[iop], writes=[tokf])
                    k.op("dve", lambda e: e.tensor_copy(tokid[:], tokf[:]), reads=[tokf], writes=[tokid])
                    for kk in range(8):
                        k.dma_fn("pool", lambda e, tt=tt, kk=kk: e.indirect_dma_start(
                            out=ROWTOK[:], out_offset=bass.IndirectOffsetOnAxis(ap=D8[:, tt, kk:kk + 1], axis=0), in_=tokid[:], in_offset=None),
                            reads=[tokid, D8], writes=[ROWTOK])
                k.barrier()
              if stop_after == "route":
                break
              with ExitStack() as es:
                rtok = sbt(nc, es, "rtok", [128, 2 * NBLK], U32)
                rtc = [TT(rtok.ap[:, rj:rj + 1], f"rtc{rj}") for rj in range(2 * NBLK)]
                for rj in range(2 * NBLK):
                    k.dma("sp", rtc[rj][:], ROWTOK[rj * 128:(rj + 1) * 128, :], reads=[ROWTOK], writes=[rtc[rj]])
                wub = Rot([sbt(nc, es, f"wub{i}", [128, KC, 512], BF16) for i in range(4)])
                wdb = Rot([sbt(nc, es, f"wdb{i}", [128, 2, D], BF16) for i in range(4)])
                hg = Rot([sbt(nc, es, f"hg{i}", [128, D], BF16) for i in range(4)])
                hgT = Rot([sbt(nc, es, f"hgT{i}", [128, KC, 128], BF16) for i in range(3)])
                sg = Rot([sbt(nc, es, f"sg{i}", [128, 256], F32) for i in range(2)])
                hid = Rot([sbt(nc, es, f"hid{i}", [128, 256], BF16) for i in range(4)])
                hidT = Rot([sbt(nc, es, f"hidT{i}", [128, 2, 128], BF16) for i in range(3)])
                osb = Rot([sbt(nc, es, f"osb{i}", [128, D], BF16) for i in range(3)])
                ptg = Rot([pst(nc, es, f"ptg{i}", [128, 1024], BF16) for i in range(2)])
                pup = Rot([pst(nc, es, f"pup{i}", [128, 512]) for i in range(2)])
                pht = pst(nc, es, "pht", [128, 1024], BF16)
                pdn = [pst(nc, es, f"pdn{i}", [128, 512]) for i in range(2)]
                def ph_t8(rj):
                    g_ = hg.next()
                    k.dma_fn("pool", lambda e: e.indirect_dma_start(
                        out=g_[:], out_offset=None, in_=H2TM[:], in_offset=bass.IndirectOffsetOnAxis(ap=rtok[:, rj:rj + 1], axis=0)),
                        reads=[H2TM, rtc[rj]], writes=[g_])
                    pt_ = ptg.next(); gT = hgT.next()
                    for kc in range(KC):
                        k.op("pe", lambda e, kc=kc: e.transpose(pt_[:, kc * 128:(kc + 1) * 128], g_[:, kc * 128:(kc + 1) * 128], idb_g[:]),
                             reads=[g_, idb_g], writes=[pt_])
                    k.op("act", lambda e: e.copy(gT[:, 0:4, :], pt_.ap[:, 0:512].rearrange("p (a c) -> p a c", a=4)), reads=[pt_], writes=[gT])
                    k.op("dve", lambda e: e.tensor_copy(gT[:, 4:8, :], pt_.ap[:, 512:1024].rearrange("p (a c) -> p a c", a=4)), reads=[pt_], writes=[gT])
                    return gT

                def ph_up(gT, wu_):
                    pu_ = pup.next()
                    for kc in range(KC):
                        k.op("pe", lambda e, kc=kc: e.matmul(pu_[:], gT[:, kc, :], wu_[:, kc, :], start=(kc == 0), stop=(kc == KC - 1)),
                             reads=[gT, wu_], writes=[pu_])
                    s_ = sg.next(); h_ = hid.next()
                    k.op("act", lambda e: e.activation(s_[:], pu_[:, 0:256], AF.Silu), reads=[pu_], writes=[s_])
                    k.op("dve", lambda e: e.tensor_tensor(h_[:], s_[:], pu_[:, 256:512], ALU.mult), reads=[s_, pu_], writes=[h_])
                    return h_

                def ph_t2(h_):
                    hT = hidT.next()
                    for fh in range(2):
                        k.op("pe", lambda e, fh=fh: e.transpose(pht[:, fh * 128:(fh + 1) * 128], h_[:, fh * 128:(fh + 1) * 128], idb_g[:]),
                             reads=[h_, idb_g], writes=[pht])
                    k.op("act", lambda e: e.copy(hT[:], pht.ap[:, 0:256].rearrange("p (a c) -> p a c", a=2)), reads=[pht], writes=[hT])
                    return hT

                def ph_dn(rj, hT, wd_):
                    for dh in range(2):
                        for fh in range(2):
                            k.op("pe", lambda e, dh=dh, fh=fh: e.matmul(pdn[dh][:], hT[:, fh, :], wd_[:, fh, dh * 512:(dh + 1) * 512],
                                                                        start=(fh == 0), stop=(fh == 1)), reads=[hT, wd_], writes=[pdn[dh]])
                    o_ = osb.next()
                    k.op("act", lambda e: e.copy(o_[:, 0:512], pdn[0][:]), reads=[pdn[0]], writes=[o_])
                    k.op("dve", lambda e: e.tensor_copy(o_[:, 512:1024], pdn[1][:]), reads=[pdn[1]], writes=[o_])
                    k.dma("sp", OUTB[rj * 128:(rj + 1) * 128, :], o_[:], reads=[o_], writes=[OUTB])

                NSUB = 2 * NBLK
                wts = {}
                st_gT = {}; st_h = {}; st_hT = {}
                for step in range(NSUB + 3):
                    j = step
                    if j < NSUB:
                        if j % 2 == 0:
                            b = j // 2
                            wu_ = wub.next(); wd_ = wdb.next()
                            k.dma_fn("pool", lambda e, wu_=wu_, b=b: e.indirect_dma_start(
                                out=wu_.ap.rearrange("p a c -> p (a c)"), out_offset=None, in_=WUPB[:],
                                in_offset=bass.IndirectOffsetOnAxis(ap=IDXW[:, b:b + 1], axis=0)), reads=[WUPB, IDXW], writes=[wu_])
                            k.dma_fn("pool", lambda e, wd_=wd_, b=b: e.indirect_dma_start(
                                out=wd_.ap.rearrange("p a c -> p (a c)"), out_offset=None, in_=WDNB[:],
                                in_offset=bass.IndirectOffsetOnAxis(ap=IDXW[:, b:b + 1], axis=0)), reads=[WDNB, IDXW], writes=[wd_])
                            wts[b] = (wu_, wd_)
                        st_gT[j] = ph_t8(j)
                    j1 = step - 1
                    if 0 <= j1 < NSUB:
                        st_h[j1] = ph_up(st_gT.pop(j1), wts[j1 // 2][0])
                    j2 = step - 2
                    if 0 <= j2 < NSUB:
                        st_hT[j2] = ph_t2(st_h.pop(j2))
                    j3 = step - 3
                    if 0 <= j3 < NSUB:
                        ph_dn(j3, st_hT.pop(j3), wts[j3 // 2][1])
                        if j3 % 2 == 1:
                            wts.pop(j3 // 2)
                k.barrier()
              last_ = last
              with ExitStack() as es:
                ones_f = sbt(nc, es, "ones_f5", [128, 128], F32)
                k.op("dve", lambda e: e.memset(ones_f[:], 1.0), writes=[ones_f])
                g2bc = [sbt(nc, es, f"g2bc{j}", [128, D], F32) for j in range(2)]
                lng = sbt(nc, es, "lng2", [128, D], F32)
                lnb = sbt(nc, es, "lnb2", [128, D], F32)
                k.dma("sp", lng[:], ln_gb[l, 2], writes=[lng])
                k.dma("sp", lnb[:], ln_gb[l, 3], writes=[lnb])
                pbc = pst(nc, es, "pbc2", [128, 512])
                with ExitStack() as es1:
                    for j in range(2):
                        rowbc(es1, g2bc[j], 40, j, ones_f, pbc)
                    k.barrier()
                fp_ = Rot([sbt(nc, es, f"fp{i}", [128, D], BF16) for i in range(36)])
                fa = Rot([sbt(nc, es, f"fa{i}", [128, D], F32) for i in range(4)])
                xv = Rot([sbt(nc, es, f"xv2{i}", [128, D], F32) for i in range(4)])
                junk = sbt(nc, es, "junk2", [128, D], F32)
                st4 = Rot([sbt(nc, es, f"st42{i}", [128, 4], F32) for i in range(2)])
                def ln_tile_nopool(v, s4):
                    k.op("dve", lambda e: e.reduce_sum(s4[:, 0:1], v[:], AX.X), reads=[v], writes=[s4])
                    k.op("dve", lambda e: e.tensor_scalar_mul(s4[:, 1:2], s4[:, 0:1], -1.0 / D), reads=[s4], writes=[s4])
                    k.op("act", lambda e: e.activation(junk[:], v[:], AF.Square, bias=s4[:, 1:2], accum_out=s4[:, 2:3]), reads=[v, s4], writes=[junk, s4])
                    k.op("dve", lambda e: e.tensor_scalar(s4[:, 3:4], s4[:, 2:3], 1.0 / D, LN_EPS, ALU.mult, ALU.add), reads=[s4], writes=[s4])
                    k.op("act", lambda e: e.activation(s4[:, 3:4], s4[:, 3:4], AF.Sqrt), reads=[s4], writes=[s4])
                    k.op("dve", lambda e: e.reciprocal(s4[:, 3:4], s4[:, 3:4]), reads=[s4], writes=[s4])
                    k.op("dve", lambda e: e.tensor_tensor(s4[:, 4:5], s4[:, 1:2], s4[:, 3:4], ALU.mult), reads=[s4], writes=[s4])
                    k.op("act", lambda e: e.activation(v[:], v[:], AF.Identity, bias=s4[:, 4:5], scale=s4[:, 3:4]), reads=[v, s4], writes=[v])
                    k.op("dve", lambda e: e.tensor_tensor(v[:], v[:], lng[:], ALU.mult), reads=[v, lng], writes=[v])
                    k.op("dve", lambda e: e.tensor_tensor(v[:], v[:], lnb[:], ALU.add), reads=[v, lnb], writes=[v])

                st5 = Rot([sbt(nc, es, f"st5{i}", [128, 8], F32) for i in range(3)])
                pacc = Rot([pst(nc, es, f"pacc{i}", [128, 512]) for i in range(4)])
                dgs = Rot([sbt(nc, es, f"dgs{i}", [128, 128], BF16) for i in range(6)])

                def e3_a0(tt):
                    acc_ = fa.next(); x_ = xv.next()
                    k.dma("sp", x_[:], xs_t[tt][:], reads=[xs_t[tt]], writes=[x_])
                    psh = fp_.next()
                    k.dma("sp", psh[:], OUTB[NBR * 256 + tt * 128:NBR * 256 + (tt + 1) * 128, :], reads=[OUTB], writes=[psh])
                    ps_ = []
                    for kk in range(8):
                        p_ = fp_.next()
                        k.dma_fn("pool", lambda e, p_=p_, kk=kk: e.indirect_dma_start(
                            out=p_[:], out_offset=None, in_=OUTB[:], in_offset=bass.IndirectOffsetOnAxis(ap=D8[:, tt, kk:kk + 1], axis=0)),
                            reads=[OUTB, D8], writes=[p_])
                        ps_.append(p_)
                    return (tt, acc_, x_, psh, ps_)

                def e3_a1(tt, acc_, x_, psh, ps_):
                    j = 1 if tt < 2 else 0
                    pa = [pacc.next(), pacc.next()]
                    for kk in range(8):
                        dg_ = dgs.next()
                        p_ = ps_[kk]
                        k.op("dve", lambda e, dg_=dg_, kk=kk: e.tensor_scalar_mul(dg_[:], idb_g[:], W8[:, tt, kk:kk + 1]), reads=[idb_g, W8], writes=[dg_])
                        for hf in range(2):
                            k.op("pe", lambda e, dg_=dg_, p_=p_, hf=hf, kk=kk: e.matmul(pa[hf][:], dg_[:], p_[:, hf * 512:(hf + 1) * 512],
                                                                                       start=(kk == 0), stop=False), reads=[dg_, p_], writes=[pa[hf]])
                    for hf in range(2):
                        k.op("pe", lambda e, hf=hf: e.matmul(pa[hf][:], idb_g[:], psh[:, hf * 512:(hf + 1) * 512], start=False, stop=True),
                             reads=[idb_g, psh], writes=[pa[hf]])
                        k.op("dve", lambda e, hf=hf: e.tensor_tensor(acc_[:, hf * 512:(hf + 1) * 512], pa[hf][:], g2bc[j][:, hf * 512:(hf + 1) * 512], ALU.mult),
                             reads=[pa[hf], g2bc[j]], writes=[acc_])
                    k.op("dve", lambda e: e.scalar_tensor_tensor(acc_[:], x_[:], DN_ALPHA, acc_[:], ALU.mult, ALU.add), reads=[x_, acc_], writes=[acc_])
                    return (tt, acc_)

                def e3_b(tt, acc_):
                    s4 = st5.next()
                    ln_tile_nopool(acc_, s4)
                    if last_:
                        k.dma("sp", out[(tt - 2) * 128:(tt - 1) * 128, :], acc_[:], reads=[acc_], writes=[out])
                    else:
                        k.dma("sp", xs_t[tt][:], acc_[:], reads=[acc_], writes=[xs_t[tt]])

                e3_tiles = list(range(2, NT)) if last_ else tts
                n3 = len(e3_tiles)
                q0_ = [e3_a0(e3_tiles[0]), e3_a0(e3_tiles[1])]
                q1_ = [e3_a1(*q0_.pop(0))]
                for i3 in range(n3):
                    if i3 + 2 < n3:
                        q0_.append(e3_a0(e3_tiles[i3 + 2]))
                    if q0_:
                        q1_.append(e3_a1(*q0_.pop(0)))
                    e3_b(*q1_.pop(0))
                k.barrier()
            if stop_after == "moe":
                break
        k.barrier()
        k.close()
    return P


def _rope_tables():
    rows = LAT // 64
    row = np.repeat(np.arange(rows, dtype=np.float32), 64)
    col = (np.arange(LAT) % 64).astype(np.float32)
    inv = (10000.0 ** (-np.arange(16, dtype=np.float32) / 16)).astype(np.float32)
    ang = np.concatenate([row[:, None] * inv, col[:, None] * inv], axis=-1)
    cos = np.cos(ang).astype(np.float32)
    sin = np.sin(ang).astype(np.float32)
    C = np.ones((128, T), np.float32)
    S = np.zeros((128, T), np.float32)
    for r in range(128):
        j = r % 64
        C[r, CTX:] = cos[:, j % 32]
        S[r, CTX:] = -sin[:, j] if j < 32 else sin[:, j - 32]
    return C, S


def _swap_cols(w):
    idx = np.arange(512)
    blk = idx // 64
    j = idx % 64
    return w[:, blk * 64 + (j + 32) % 64]


def _blk(w):
    return np.ascontiguousarray(w.reshape(KC, 128, w.shape[1]).transpose(1, 0, 2))


def prep_shared(inputs):
    w_in = inputs["w_in"]
    sh = {}
    fm = np.empty((DEPTH, 14, 128, KC, 512), np.float32)
    tm = np.empty((DEPTH, 2, 128, KC, 512), np.float32)
    wdt = np.empty((DEPTH, 128, KC, 16), np.float32)
    for l in range(DEPTH):
        w = w_in[l]
        sl = lambda nm: w[:, _W[nm][0]:_W[nm][0] + _W[nm][1]]
        blocks = [sl("q"), _swap_cols(sl("q")), sl("k"), _swap_cols(sl("k")), sl("lx"), sl("lg"),
                  sl("xbc")[:, :512], sl("xbc")[:, 512:]] + [sl("gates")[:, i * 512:(i + 1) * 512] for i in range(6)]
        for i, b in enumerate(blocks):
            fm[l, i] = _blk(b)
        tm[l, 0] = _blk(sl("v"))
        tm[l, 1] = _blk(sl("z"))
        wdt[l] = _blk(sl("dt"))
    sh["w_fm"] = fm
    sh["w_tm"] = tm
    sh["w_dt"] = wdt
    sh["w_mod"] = np.ascontiguousarray(inputs["w_mod"])
    sh["b_modT"] = np.ascontiguousarray(inputs["b_mod"].reshape(DEPTH, 48, 128).transpose(0, 2, 1))
    lqk = np.concatenate([inputs["lam_q"].reshape(DEPTH, 1, 128), inputs["lam_k"].reshape(DEPTH, 1, 128)], axis=2)
    sh["lamqk"] = np.ascontiguousarray(np.broadcast_to(lqk, (DEPTH, 128, 256))).astype(np.float32)
    sh["attn_gT"] = np.ascontiguousarray(inputs["attn_norm_g"].transpose(0, 2, 1))
    L_ = DEPTH
    sh["lru_cw"] = np.ascontiguousarray(inputs["lru_conv_w"].reshape(L_, 4, 4, 128).transpose(0, 3, 2, 1))
    sh["lru_cb"] = np.ascontiguousarray(inputs["lru_conv_b"].reshape(L_, 4, 128).transpose(0, 2, 1))
    wbd = np.zeros((L_, 128, 16, 128), np.float32)
    for l in range(L_):
        for d in range(2):
            for ai, nm in enumerate(("lru_wa", "lru_wi")):
                for ct in range(4):
                    idx = (d * 2 + ai) * 4 + ct
                    for bb in range(2):
                        wbd[l, bb * 64:(bb + 1) * 64, idx, bb * 64:(bb + 1) * 64] = inputs[nm][l, d, 2 * ct + bb]
    sh["lru_wbd"] = wbd
    lb = np.stack([inputs["lru_ba"], inputs["lru_bi"]], axis=2)
    sh["lru_bias"] = np.ascontiguousarray(lb.reshape(L_, 2, 2, 4, 128).transpose(0, 4, 1, 2, 3).reshape(L_, 128, 16))
    sh["lru_lam"] = np.ascontiguousarray(inputs["lru_lambda"].reshape(L_, 2, 4, 128).transpose(0, 3, 1, 2).reshape(L_, 128, 8))
    sh["ssd_cw"] = np.ascontiguousarray(inputs["ssd_conv_w"].reshape(L_, 4, 8, 128).transpose(0, 3, 2, 1))
    sh["ssd_cb"] = np.ascontiguousarray(inputs["ssd_conv_b"].reshape(L_, 8, 128).transpose(0, 2, 1))
    sh["ssd_dtb"] = np.ascontiguousarray(np.broadcast_to(inputs["ssd_dt_bias"].reshape(L_, 1, 1, 16), (L_, 128, NT, 16)).reshape(L_, 128, NT * 16))
    sh["ssd_alog"] = np.ascontiguousarray(np.broadcast_to(inputs["ssd_a_log"].reshape(L_, 1, 1, 16), (L_, 128, NT, 16)).reshape(L_, 128, NT * 16))
    sh["ssd_dsk"] = np.ascontiguousarray(np.broadcast_to(inputs["ssd_d"].reshape(L_, 1, 8), (L_, 128, 8)))
    sh["ssd_gn"] = np.ascontiguousarray(np.broadcast_to(inputs["ssd_norm_g"].reshape(L_, 1, 512), (L_, 128, 512)))
    kk_, ll_ = np.meshgrid(np.arange(128), np.arange(128), indexing="ij")
    tri = np.stack([(kk_ <= ll_), (kk_ >= ll_)], axis=1).astype(np.float32)
    sh["tri"] = np.ascontiguousarray(tri)
    sh["maskf"] = np.ascontiguousarray(np.broadcast_to(tri[:, :, None, :], (128, 2, 8, 128)))
    sh["identb"] = np.eye(128, dtype=np.float32)
    sh["w_br"] = np.ascontiguousarray(inputs["w_branch"].reshape(L_, 12, 128, D).transpose(0, 2, 1, 3))
    sh["w_o"] = np.ascontiguousarray(inputs["w_out"].reshape(L_, KC, 128, D).transpose(0, 2, 1, 3))
    lngb = np.stack([inputs["ln1_g"], inputs["ln1_b"], inputs["ln2_g"], inputs["ln2_b"]], axis=1)
    sh["ln_gb"] = np.ascontiguousarray(np.broadcast_to(lngb[:, :, None, :], (L_, 4, 128, D)))
    sh["w_rt"] = np.ascontiguousarray(inputs["w_router"].reshape(L_, KC, 128, E).transpose(0, 2, 1, 3))
    sh["r_bias"] = np.ascontiguousarray(np.broadcast_to(inputs["router_bias"][:, None, :], (L_, 128, E)))
    wu = np.concatenate([inputs["w_up"], inputs["ws_up"][:, None]], axis=1)
    sh["w_upx"] = np.ascontiguousarray(wu.reshape(L_, E + 1, KC, 128, 512).transpose(0, 1, 3, 2, 4))
    wd = np.concatenate([inputs["w_down"], inputs["ws_down"][:, None]], axis=1)
    sh["w_dnx"] = np.ascontiguousarray(wd.reshape(L_, E + 1, 2, 128, D).transpose(0, 1, 3, 2, 4))
    sh["iota64"] = np.ascontiguousarray(np.broadcast_to(np.arange(64, dtype=np.float32)[None, :], (128, 64)))
    sh["iotap"] = np.arange(128, dtype=np.float32).reshape(128, 1)
    sh["thr_in"] = np.ascontiguousarray(np.broadcast_to((256.0 * np.arange(200, dtype=np.float32))[None, :], (128, 200)))
    kk2, tt2 = np.meshgrid(np.arange(128), np.arange(128), indexing="ij")
    sh["ustrict"] = (kk2 < tt2).astype(np.float32)
    sh["rowtok_sh"] = np.ascontiguousarray((np.arange(34, dtype=np.uint32)[None, :] + 34 * np.arange(128, dtype=np.uint32)[:, None]).astype(np.uint32))
    C, S = _rope_tables()
    sh["ropec"] = C
    sh["ropes"] = S
    sh["ident"] = np.eye(128, dtype=np.float32)
    return sh


def prep_core(inputs, b):
    d = {}
    d["xin"] = np.ascontiguousarray(np.concatenate([inputs["ctx"][b], inputs["x"][b]], axis=0))
    c2 = np.stack([inputs["c"][b].reshape(KC, 128).T, inputs["c_ctx"].reshape(KC, 128).T], axis=-1)
    d["c2"] = np.ascontiguousarray(c2.astype(np.float32))
    return d


_CACHE = {}


def kernel(**inputs):
    inputs = {k_: np.asarray(v) for k_, v in inputs.items()}
    if "prog" not in _CACHE:
        _CACHE["prog"] = build_program()
    P = _CACHE["prog"]
    sh = prep_shared(inputs)
    in_maps = []
    for b in range(8):
        m = dict(sh)
        m.update(prep_core(inputs, b))
        in_maps.append(m)
    res = run_bass_kernel_spmd(P.nc, in_maps, core_ids=list(range(8)))
    return np.stack([r["out"] for r in res.results], axis=0)
```

```python
import math
from contextlib import ExitStack
import numpy as np
import concourse.bass as bass
import concourse.mybir as mybir
from concourse.bass_utils import run_bass_kernel_spmd

F32 = mybir.dt.float32
U32 = mybir.dt.uint32
BF16 = mybir.dt.bfloat16
AF = mybir.ActivationFunctionType
ALU = mybir.AluOpType
AX = mybir.AxisListType

D = 1024
LAT = 4096
CTX = 256
T = LAT + CTX
NT = T // 128
DEPTH = 2
KC = D // 128
N_MOD = 6 * D
E = 64
EF = 256
DN_ALPHA = (2 * DEPTH) ** 0.25
LN_EPS = 1e-5
RMS_EPS = 1e-6
CHUNKS = [(0, 256)] + [(256 + 512 * i, 512) for i in range(8)]

_W = dict(q=(0, 512), k=(512, 512), v=(1024, 512), lx=(1536, 512), lg=(2048, 512), z=(2560, 512),
          xbc=(3072, 1024), dt=(4096, 16), gates=(4112, 3072))
FM_ROWS = dict(q=0, k=512, lx=1024, lg=1536, xbc=2048, gates=3072)
N_FM = 6144


class TT:
    def __init__(self, ap, name="", multi=False):
        self.ap = ap
        self.name = name
        self.w = None
        self.r = []
        self.multi = multi
        self.wset = {}

    def __getitem__(self, idx):
        return self.ap[idx]


class KB:
    def __init__(self, nc, n_dma_sems=8):
        self.nc = nc
        self.eng = {"pe": nc.tensor, "act": nc.scalar, "dve": nc.vector, "pool": nc.gpsimd, "sp": nc.sync}
        self.sem = {}
        self.cnt = {}
        self.waited = {e: {} for e in self.eng}
        self._ctx = []
        self.allsems = {}
        for e in ("pe", "act", "dve", "pool"):
            cm = nc.semaphore("s_" + e)
            s = cm.__enter__()
            self._ctx.append(cm)
            self.sem[e] = s
            self.cnt[e] = 0
        self.dsem = {}
        for q in ("sp", "act", "pool"):
            lst = []
            for i in range(n_dma_sems):
                cm = nc.semaphore(f"d_{q}{i}")
                s = cm.__enter__()
                self._ctx.append(cm)
                lst.append([s, 0])
            self.dsem[q] = [lst, 0]
        self.n_inst = 0

    def close(self):
        for cm in reversed(self._ctx):
            cm.__exit__(None, None, None)

    def _need(self, e, toks):
        eng = self.eng[e]
        wd = self.waited[e]
        best = {}
        for tok in toks:
            if tok is None:
                continue
            s, v = tok
            key = id(s)
            if wd.get(key, 0) >= v:
                continue
            if key not in best or best[key][1] < v:
                best[key] = (s, v)
        for key, (s, v) in best.items():
            eng.wait_ge(s, v)
            wd[key] = v
            self.n_inst += 1

    def _deps(self, e, reads, writes):
        toks = []
        for t in reads:
            if t.w is None and not t.wset and not getattr(t, "ext", False):
                raise RuntimeError(f"read of {t.name} before any tracked write")
            toks.append(t.w)
            if t.wset:
                toks.extend(t.wset.values())
        for t in writes:
            toks.append(t.w)
            toks.extend(t.r)
            if t.wset and not t.multi:
                toks.extend(t.wset.values())
        if e == "pe":
            own = id(self.sem["pe"])
            toks = [t for t in toks if t is not None and id(t[0]) != own]
        self._need(e, toks)

    def _mark(self, tok, reads, writes):
        for t in reads:
            t.r.append(tok)
            if len(t.r) > 16:
                m = {}
                for s, v in t.r:
                    k = id(s)
                    if k not in m or m[k][1] < v:
                        m[k] = (s, v)
                t.r = list(m.values())
        for t in writes:
            if t.multi:
                kk_ = id(tok[0])
                if kk_ not in t.wset or t.wset[kk_][1] < tok[1]:
                    t.wset[kk_] = tok
            else:
                t.w = tok
                t.wset = {}
            t.r = []

    def op(self, e, fn, reads=(), writes=()):
        self._deps(e, reads, writes)
        ins = fn(self.eng[e])
        self.cnt[e] += 1
        ins.then_inc(self.sem[e], 1)
        tok = (self.sem[e], self.cnt[e])
        self._mark(tok, reads, writes)
        self.n_inst += 1
        return ins

    def dma(self, q, out, in_, reads=(), writes=(), **kw):
        lst = self.dsem[q][0]
        i = self.dsem[q][1]
        self.dsem[q][1] = (i + 1) % len(lst)
        ent = lst[i]
        if ent[1] > 0:
            self._need(q, [(ent[0], ent[1])])
        self._deps(q, reads, writes)
        ins = self.eng[q].dma_start(out=out, in_=in_, **kw)
        ent[1] += 16
        ins.then_inc(ent[0], 16)
        tok = (ent[0], ent[1])
        self._mark(tok, reads, writes)
        self.n_inst += 1
        return ins

    def dma_fn(self, q, fn, reads=(), writes=()):
        lst = self.dsem[q][0]
        i = self.dsem[q][1]
        self.dsem[q][1] = (i + 1) % len(lst)
        ent = lst[i]
        if ent[1] > 0:
            self._need(q, [(ent[0], ent[1])])
        self._deps(q, reads, writes)
        ins = fn(self.eng[q])
        ent[1] += 16
        ins.then_inc(ent[0], 16)
        tok = (ent[0], ent[1])
        self._mark(tok, reads, writes)
        self.n_inst += 1
        return ins

    def barrier(self):
        toks = [(self.sem[e], self.cnt[e]) for e in self.sem if self.cnt[e] > 0]
        for q in self.dsem:
            for s, v in self.dsem[q][0]:
                if v > 0:
                    toks.append((s, v))
        for e in self.eng:
            self._need(e, toks)


class Rot:
    def __init__(self, tiles):
        self.tiles = tiles
        self.i = 0

    def next(self):
        t = self.tiles[self.i]
        self.i = (self.i + 1) % len(self.tiles)
        return t


class Prog:
    def __init__(self, debug=False):
        self.debug = debug
        self.nc = bass.Bass("TRN2", target_bir_lowering=False)
        self.k = None
        self.ins = {}
        self.dbg = {}

    def inp(self, name, shape, dt=F32):
        ap = self.nc.dram_tensor(name, list(shape), dt, kind="ExternalInput").ap()
        self.ins[name] = TT(ap, name)
        self.ins[name].ext = True
        return self.ins[name]

    def scratch(self, name, shape, dt, dump=False, multi=False):
        if dump and self.debug:
            ap = self.nc.dram_tensor(name, list(shape), dt, kind="ExternalOutput").ap()
            self.dbg[name] = True
        else:
            ap = self.nc.dram_tensor(name, list(shape), dt).ap()
        return TT(ap, name, multi=multi)


_UID = [0]


def sbt(nc, es, name, shape, dt):
    _UID[0] += 1
    name = f"{name}_s{_UID[0]}"
    return TT(es.enter_context(nc.sbuf_tensor(name, list(shape), dt)), name)


def pst(nc, es, name, shape, dt=F32):
    _UID[0] += 1
    name = f"{name}_p{_UID[0]}"
    return TT(es.enter_context(nc.psum_tensor(name, list(shape), dt)), name)


def build_program(debug=False, n_layers=DEPTH, stop_after=None, stages=None):
    P = Prog(debug)
    nc = P.nc
    xin = P.inp("xin", [T, D])
    c2 = P.inp("c2", [128, KC, 2])
    ident_in = P.inp("ident", [128, 128])
    w_mod = P.inp("w_mod", [DEPTH, D, N_MOD])
    b_modT = P.inp("b_modT", [DEPTH, 128, 48])
    w_fm = P.inp("w_fm", [DEPTH, 14, 128, KC, 512])
    w_tm = P.inp("w_tm", [DEPTH, 2, 128, KC, 512])
    w_dt = P.inp("w_dt", [DEPTH, 128, KC, 16])
    ropec = P.inp("ropec", [128, T])
    ropes = P.inp("ropes", [128, T])
    lamqk = P.inp("lamqk", [DEPTH, 128, 256])
    attn_gT = P.inp("attn_gT", [DEPTH, 128, 4])
    lru_cw = P.inp("lru_cw", [DEPTH, 128, 4, 4])
    lru_cb = P.inp("lru_cb", [DEPTH, 128, 4])
    lru_wbd = P.inp("lru_wbd", [DEPTH, 128, 16, 128])
    lru_bias = P.inp("lru_bias", [DEPTH, 128, 16])
    lru_lam = P.inp("lru_lam", [DEPTH, 128, 8])
    ssd_cw = P.inp("ssd_cw", [DEPTH, 128, 8, 4])
    ssd_cb = P.inp("ssd_cb", [DEPTH, 128, 8])
    ssd_dtb = P.inp("ssd_dtb", [DEPTH, 128, NT * 16])
    ssd_alog = P.inp("ssd_alog", [DEPTH, 128, NT * 16])
    ssd_dsk = P.inp("ssd_dsk", [DEPTH, 128, 8])
    ssd_gn = P.inp("ssd_gn", [DEPTH, 128, 512])
    tri_in = P.inp("tri", [128, 2, 128])
    maskf_in = P.inp("maskf", [128, 2, 8, 128])
    identb_in = P.inp("identb", [128, 128])
    w_br = P.inp("w_br", [DEPTH, 128, 12, D])
    w_o = P.inp("w_o", [DEPTH, 128, KC, D])
    ln_gb = P.inp("ln_gb", [DEPTH, 4, 128, D])
    w_rt = P.inp("w_rt", [DEPTH, 128, KC, E])
    r_bias = P.inp("r_bias", [DEPTH, 128, E])
    w_upx = P.inp("w_upx", [DEPTH, E + 1, 128, KC, 512])
    w_dnx = P.inp("w_dnx", [DEPTH, E + 1, 128, 2, D])
    iota64 = P.inp("iota64", [128, 64])
    iotap = P.inp("iotap", [128, 1])
    thr_in = P.inp("thr_in", [128, 200])
    ustrict = P.inp("ustrict", [128, 128])
    rowtok_sh = P.inp("rowtok_sh", [128, 34], U32)
    out = TT(nc.dram_tensor("out", [LAT, D], F32, kind="ExternalOutput").ap(), "out")
    xs_full = P.scratch("xs", [T, D], F32, dump=True)
    xs_t = [TT(xs_full.ap[tt * 128:(tt + 1) * 128, :], f"xs{tt}") for tt in range(NT)]
    NBR = 200
    NBS = T // 256
    NBLK = NBR + NBS
    NROWS = NBLK * 256
    H2TM = P.scratch("H2TM", [T, D], BF16, multi=True)
    WUPB = P.scratch("WUPB", [(E + 1) * 128, KC * 512], BF16, multi=True)
    WDNB = P.scratch("WDNB", [(E + 1) * 128, 2 * D], BF16, multi=True)
    ROWTOK = P.scratch("ROWTOK", [NROWS, 1], U32, dump=True)
    OUTB = P.scratch("OUTB", [NROWS, D], BF16, multi=True)
    PF = P.scratch("PF", [N_FM, T], BF16, dump=True, multi=True)
    PV = P.scratch("PVt", [T, 512], BF16, dump=True, multi=True)
    PZ = P.scratch("PZt", [T, 512], BF16, dump=True, multi=True)
    PDT = P.scratch("PDT", [T, 16], F32, dump=True)
    YT = P.scratch("YT", [1536, T], BF16, dump=True, multi=True)

    k = KB(nc)
    P.k = k
    with ExitStack() as es0:
        ident = sbt(nc, es0, "ident_sb", [128, 128], F32)
        mod = sbt(nc, es0, "mod", [128, 48, 2], F32)
        mod1 = sbt(nc, es0, "mod1", [128, 48, 2], F32)
        s2 = sbt(nc, es0, "s2", [128, KC, 2], F32)
        D8 = sbt(nc, es0, "D8", [128, NT, 8], U32)
        W8 = sbt(nc, es0, "W8", [128, NT, 8], F32)
        IDXW = sbt(nc, es0, "IDXW", [128, NBLK], U32)
        idb_g = sbt(nc, es0, "idb0", [128, 128], BF16)
        k.dma("sp", ident[:], ident_in[:], writes=[ident])
        k.op("dve", lambda e: e.tensor_copy(idb_g[:], ident[:]), reads=[ident], writes=[idb_g])
        k.dma("sp", s2[:], c2[:], writes=[s2])
        k.op("act", lambda e: e.activation(s2[:], s2[:], AF.Silu), reads=[s2], writes=[s2])

        for l in range(n_layers):
            with ExitStack() as es:
                wm = Rot([sbt(nc, es, f"wm{i}", [128, N_MOD], F32) for i in range(2)])
                bm = sbt(nc, es, "bm", [128, 48], F32)
                pm = pst(nc, es, "pm", [128, 96])
                k.dma("sp", bm[:], b_modT[l], writes=[bm])
                for kc in range(KC):
                    w = wm.next()
                    k.dma("sp" if kc % 2 == 0 else "pool", w[:], w_mod[l, kc * 128:(kc + 1) * 128, :], writes=[w])
                    for cb in range(48):
                        k.op("pe", lambda e, w=w, cb=cb, kc=kc: e.matmul(
                            pm[:, 2 * cb:2 * cb + 2], w[:, cb * 128:(cb + 1) * 128], s2[:, kc, :],
                            start=(kc == 0 and cb == 0), stop=(kc == KC - 1 and cb == 47), skip_group_check=True),
                            reads=[w, s2], writes=[pm])
                pm3 = pm.ap.rearrange("p (c j) -> p c j", j=2)
                for j in range(2):
                    k.op("dve", lambda e, j=j: e.tensor_tensor(mod[:, :, j], pm3[:, :, j], bm[:], ALU.add),
                         reads=[pm, bm], writes=[mod])
                k.op("dve", lambda e: e.tensor_scalar_add(mod1[:], mod[:], 1.0), reads=[mod], writes=[mod1])
                k.barrier()
            if stop_after == "mod":
                break

            with ExitStack() as es:
                hT = sbt(nc, es, "hT", [128, KC, T], BF16)
                with ExitStack() as es1:
                    xt = Rot([sbt(nc, es1, f"xt{i}", [128, D], F32) for i in range(3)])
                    ptr = Rot([pst(nc, es1, f"ptr{i}", [128, 512]) for i in range(4)])
                    for tt in range(NT):
                        x_t = xt.next()
                        if l == 0:
                            k.dma("sp" if tt % 2 == 0 else "pool", x_t[:], xin[tt * 128:(tt + 1) * 128, :], reads=[xin], writes=[x_t])
                        else:
                            k.dma("sp" if tt % 2 == 0 else "pool", x_t[:], xs_t[tt][:], reads=[xs_t[tt]], writes=[x_t])
                        j = 1 if tt < 2 else 0
                        for half in range(2):
                            p_t = ptr.next()
                            for q4 in range(4):
                                kc = half * 4 + q4
                                k.op("pe", lambda e, p_t=p_t, x_t=x_t, kc=kc, q4=q4: e.transpose(
                                    p_t[:, q4 * 128:(q4 + 1) * 128], x_t[:, kc * 128:(kc + 1) * 128], ident[:]),
                                    reads=[x_t, ident], writes=[p_t])
                            for q4 in range(4):
                                kc = half * 4 + q4
                                eng = "act" if q4 % 2 == 0 else "dve"
                                if eng == "act":
                                    k.op("act", lambda e, p_t=p_t, kc=kc, q4=q4, tt=tt, j=j: e.activation(
                                        hT[:, kc, tt * 128:(tt + 1) * 128], p_t[:, q4 * 128:(q4 + 1) * 128], AF.Identity,
                                        bias=mod[:, kc, j:j + 1], scale=mod1[:, 8 + kc, j:j + 1]),
                                        reads=[p_t, mod, mod1], writes=[hT])
                                else:
                                    k.op("dve", lambda e, p_t=p_t, kc=kc, q4=q4, tt=tt, j=j: e.tensor_scalar(
                                        hT[:, kc, tt * 128:(tt + 1) * 128], p_t[:, q4 * 128:(q4 + 1) * 128],
                                        mod1[:, 8 + kc, j:j + 1], mod[:, kc, j:j + 1], ALU.mult, ALU.add),
                                        reads=[p_t, mod, mod1], writes=[hT])
                    k.barrier()
                with ExitStack() as es2:
                    wf32 = Rot([sbt(nc, es2, f"wf32_{i}", [128, KC, 512], F32) for i in range(2)])
                    wbf = Rot([sbt(nc, es2, f"wbf_{i}", [128, KC, 512], BF16) for i in range(3)])
                    stg = Rot([sbt(nc, es2, f"stg{i}", [128, T], BF16) for i in range(3)])
                    pp = Rot([pst(nc, es2, f"pp{i}", [128, 512]) for i in range(6)])
                    rc = sbt(nc, es2, "rc", [128, T], F32)
                    rs = sbt(nc, es2, "rs", [128, T], F32)
                    tmp = Rot([sbt(nc, es2, f"rtmp{i}", [128, 512], F32) for i in range(4)])
                    k.dma("sp", rc[:], ropec[:], writes=[rc])
                    k.dma("pool", rs[:], ropes[:], writes=[rs])
                    cast_i = [0]

                    def load_w(src_ap, src_t):
                        a = wf32.next()
                        k.dma("sp", a[:], src_ap, reads=[src_t], writes=[a])
                        b = wbf.next()
                        eng = ("pool", "dve")[cast_i[0] % 2]
                        cast_i[0] += 1
                        for h2 in range(2):
                            k.op(eng, lambda e, a=a, b=b, h2=h2: e.tensor_copy(b[:, h2 * 4:(h2 + 1) * 4, :], a[:, h2 * 4:(h2 + 1) * 4, :]),
                                 reads=[a], writes=[b])
                        return b

                    evac_i = [0]

                    def fm_block(wb, row0):
                        for m in range(4):
                            st = stg.next()
                            for (t0, n) in CHUNKS:
                                p_t = pp.next()
                                for kc in range(KC):
                                    k.op("pe", lambda e, p_t=p_t, wb=wb, kc=kc, m=m, t0=t0, n=n: e.matmul(
                                        p_t[:, :n], wb[:, kc, m * 128:(m + 1) * 128], hT[:, kc, t0:t0 + n],
                                        start=(kc == 0), stop=(kc == KC - 1)), reads=[wb, hT], writes=[p_t])
                                eng = ("act", "dve")[evac_i[0] % 2]
                                evac_i[0] += 1
                                if eng == "act":
                                    k.op("act", lambda e, p_t=p_t, st=st, t0=t0, n=n: e.copy(st[:, t0:t0 + n], p_t[:, :n]),
                                         reads=[p_t], writes=[st])
                                else:
                                    k.op("dve", lambda e, p_t=p_t, st=st, t0=t0, n=n: e.tensor_copy(st[:, t0:t0 + n], p_t[:, :n]),
                                         reads=[p_t], writes=[st])
                            k.dma("pool", PF[row0 + m * 128:row0 + (m + 1) * 128, :], st[:], reads=[st], writes=[PF])

                    def rope_block(wb, wbs, row0):
                        for m in range(4):
                            st = stg.next()
                            for (t0, n) in CHUNKS:
                                pa = pp.next()
                                pb = pp.next()
                                for kc in range(KC):
                                    k.op("pe", lambda e, pa=pa, wb=wb, kc=kc, m=m, t0=t0, n=n: e.matmul(
                                        pa[:, :n], wb[:, kc, m * 128:(m + 1) * 128], hT[:, kc, t0:t0 + n],
                                        start=(kc == 0), stop=(kc == KC - 1)), reads=[wb, hT], writes=[pa])
                                for kc in range(KC):
                                    k.op("pe", lambda e, pb=pb, wbs=wbs, kc=kc, m=m, t0=t0, n=n: e.matmul(
                                        pb[:, :n], wbs[:, kc, m * 128:(m + 1) * 128], hT[:, kc, t0:t0 + n],
                                        start=(kc == 0), stop=(kc == KC - 1)), reads=[wbs, hT], writes=[pb])
                                t1 = tmp.next()
                                t2 = tmp.next()
                                k.op("dve", lambda e, pa=pa, t1=t1, t0=t0, n=n: e.tensor_tensor(t1[:, :n], pa[:, :n], rc[:, t0:t0 + n], ALU.mult),
                                     reads=[pa, rc], writes=[t1])
                                k.op("dve", lambda e, pb=pb, t2=t2, t0=t0, n=n: e.tensor_tensor(t2[:, :n], pb[:, :n], rs[:, t0:t0 + n], ALU.mult),
                                     reads=[pb, rs], writes=[t2])
                                k.op("pool", lambda e, t1=t1, t2=t2, st=st, t0=t0, n=n: e.tensor_tensor(st[:, t0:t0 + n], t1[:, :n], t2[:, :n], ALU.add),
                                     reads=[t1, t2], writes=[st])
                            k.dma("pool", PF[row0 + m * 128:row0 + (m + 1) * 128, :], st[:], reads=[st], writes=[PF])

                    for qi, name in enumerate(("q", "k")):
                        wb = load_w(w_fm[l, 2 * qi], w_fm)
                        wbs = load_w(w_fm[l, 2 * qi + 1], w_fm)
                        rope_block(wb, wbs, FM_ROWS[name])
                    for bi in range(4, 14):
                        wb = load_w(w_fm[l, bi], w_fm)
                        fm_block(wb, 1024 + (bi - 4) * 512)
                    stt = Rot([sbt(nc, es2, f"stt{i}", [128, 512], BF16) for i in range(3)])
                    for vi, dst in enumerate((PV, PZ)):
                        wb = load_w(w_tm[l, vi], w_tm)
                        for tt in range(NT):
                            p_t = pp.next()
                            for kc in range(KC):
                                k.op("pe", lambda e, p_t=p_t, wb=wb, kc=kc, tt=tt: e.matmul(
                                    p_t[:], hT[:, kc, tt * 128:(tt + 1) * 128], wb[:, kc, :],
                                    start=(kc == 0), stop=(kc == KC - 1)), reads=[wb, hT], writes=[p_t])
                            st = stt.next()
                            eng = ("act", "dve")[tt % 2]
                            if eng == "act":
                                k.op("act", lambda e, p_t=p_t, st=st: e.copy(st[:], p_t[:]), reads=[p_t], writes=[st])
                            else:
                                k.op("dve", lambda e, p_t=p_t, st=st: e.tensor_copy(st[:], p_t[:]), reads=[p_t], writes=[st])
                            k.dma("pool", dst[tt * 128:(tt + 1) * 128, :], st[:], reads=[st], writes=[dst])
                    wd32 = sbt(nc, es2, "wd32", [128, KC, 16], F32)
                    wdb = sbt(nc, es2, "wdb", [128, KC, 16], BF16)
                    dts = sbt(nc, es2, "dts", [128, NT, 16], F32)
                    k.dma("sp", wd32[:], w_dt[l], writes=[wd32])
                    k.op("dve", lambda e: e.tensor_copy(wdb[:], wd32[:]), reads=[wd32], writes=[wdb])
                    for tt in range(NT):
                        p_t = pp.next()
                        for kc in range(KC):
                            k.op("pe", lambda e, p_t=p_t, kc=kc, tt=tt: e.matmul(
                                p_t[:, :16], hT[:, kc, tt * 128:(tt + 1) * 128], wdb[:, kc, :],
                                start=(kc == 0), stop=(kc == KC - 1)), reads=[wdb, hT], writes=[p_t])
                        k.op("act", lambda e, p_t=p_t, tt=tt: e.copy(dts[:, tt, :], p_t[:, :16]), reads=[p_t], writes=[dts])
                    k.dma("sp", PDT.ap.rearrange("(n p) c -> p n c", p=128), dts[:], reads=[dts], writes=[PDT])
                    k.barrier()
            if stop_after == "proj":
                break


            if stages is None or "attn" in stages:
              with ExitStack() as es:
                lam_init = 0.8 - 0.6 * math.exp(-0.3 * l)
                QT = sbt(nc, es, "QT", [128, 4, T], BF16)
                KT = sbt(nc, es, "KT", [128, 4, T], BF16)
                V = sbt(nc, es, "V", [128, NT, 512], BF16)
                ones_b = sbt(nc, es, "ones_b", [128, 128], BF16)
                ones_f = sbt(nc, es, "ones_f", [128, 128], F32)
                lq = sbt(nc, es, "lq", [128, 256], F32)
                lpr = sbt(nc, es, "lpr", [128, 128], F32)
                lsum = sbt(nc, es, "lsum", [128, 2], F32)
                nlam = sbt(nc, es, "nlam", [128, 1], F32)
                gsc = sbt(nc, es, "gsc", [128, 4], F32)
                for h in range(4):
                    k.dma("sp", QT[:, h, :], PF[h * 128:(h + 1) * 128, :], reads=[PF], writes=[QT])
                    k.dma("pool", KT[:, h, :], PF[512 + h * 128:512 + (h + 1) * 128, :], reads=[PF], writes=[KT])
                pv3 = PV.ap.rearrange("(n p) c -> p n c", p=128)
                for hh in range(2):
                    k.dma("sp", V[:, hh * 17:(hh + 1) * 17, :], pv3[:, hh * 17:(hh + 1) * 17, :], reads=[PV], writes=[V])
                k.op("dve", lambda e: e.memset(ones_b[:], 1.0), writes=[ones_b])
                k.op("dve", lambda e: e.memset(ones_f[:], 1.0), writes=[ones_f])
                k.dma("sp", lq[:], lamqk[l], writes=[lq])
                k.dma("sp", gsc[:], attn_gT[l], writes=[gsc])
                k.op("dve", lambda e: e.tensor_tensor(lpr[:], lq[:, 0:128], lq[:, 128:256], ALU.mult), reads=[lq], writes=[lpr])
                k.op("dve", lambda e: e.reduce_sum(lsum[:], lpr.ap.rearrange("p (a b) -> p a b", a=2), AX.X), reads=[lpr], writes=[lsum])
                k.op("act", lambda e: e.activation(lsum[:], lsum[:], AF.Exp), reads=[lsum], writes=[lsum])
                k.op("dve", lambda e: e.scalar_tensor_tensor(nlam[:], lsum[:, 1:2], -lam_init, lsum[:, 0:1], ALU.add, ALU.subtract),
                     reads=[lsum], writes=[nlam])
                k.op("dve", lambda e: e.tensor_scalar_mul(gsc[:], gsc[:], 1.0 - lam_init), reads=[gsc], writes=[gsc])
                sps = Rot([pst(nc, es, f"sps{i}", [128, 512]) for i in range(4)])
                acc = [pst(nc, es, f"acc{i}", [128, 512]) for i in range(4)]
                pts = Rot([sbt(nc, es, f"pts{i}", [128, 512], BF16) for i in range(4)])
                wk = Rot([sbt(nc, es, f"awk{i}", [128, 512], F32) for i in range(9)])
                yst = Rot([sbt(nc, es, f"yst{i}", [128, 512], BF16) for i in range(2)])
                su = Rot([sbt(nc, es, f"su{i}", [128, KC, 512], F32) for i in range(2)])
                sd = Rot([sbt(nc, es, f"sd{i}", [128, 2, D], F32) for i in range(2)])
                bu = Rot([sbt(nc, es, f"bu{i}", [128, KC * 512], BF16) for i in range(2)])
                bd = Rot([sbt(nc, es, f"bd{i}", [128, 2 * D], BF16) for i in range(2)])

                def precast_gen():
                    for ex in range(E + 1):
                        a_ = su.next(); b_ = sd.next(); c_ = bu.next(); d_ = bd.next()
                        k.dma("sp", a_[:], w_upx[l, ex], writes=[a_])
                        k.dma("sp", b_[:], w_dnx[l, ex], writes=[b_])
                        yield
                        k.op("pool", lambda e, a_=a_, c_=c_: e.tensor_copy(c_[:], a_.ap.rearrange("p a b -> p (a b)")), reads=[a_], writes=[c_])
                        k.op("dve", lambda e, b_=b_, d_=d_: e.tensor_copy(d_[:], b_.ap.rearrange("p a b -> p (a b)")), reads=[b_], writes=[d_])
                        yield
                        k.dma("pool", WUPB[ex * 128:(ex + 1) * 128, :], c_[:], reads=[c_], writes=[WUPB])
                        k.dma("pool", WDNB[ex * 128:(ex + 1) * 128, :], d_[:], reads=[d_], writes=[WDNB])
                        yield

                pcg = precast_gen()
                pc_ctr = [0]

                def precast_tick():
                    pc_ctr[0] += 1
                    if pc_ctr[0] % 5 == 0:
                        next(pcg, None)

                pend_epi = []
                for h in range(4):
                    for (q0, n) in CHUNKS:
                        kts = [0, 1] if q0 == 0 else list(range(NT))

                        def s_mm(kt, h=h, q0=q0, n=n):
                            res = []
                            for m in range(2):
                                sp_ = sps.next()
                                k.op("pe", lambda e, sp_=sp_, m=m, kt=kt: e.matmul(
                                    sp_[:, :n], KT[m * 64:(m + 1) * 64, h, kt * 128:(kt + 1) * 128],
                                    QT[m * 64:(m + 1) * 64, h, q0:q0 + n], start=True, stop=True),
                                    reads=[KT, QT], writes=[sp_])
                                res.append(sp_)
                            return res

                        cur = s_mm(kts[0])
                        for i, kt in enumerate(kts):
                            precast_tick()
                            if (i == 3 or (len(kts) < 4 and i == len(kts) - 1)) and pend_epi:
                                pend_epi.pop(0)()
                            nxt = s_mm(kts[i + 1]) if i + 1 < len(kts) else None
                            for m in range(2):
                                pt = pts.next()
                                sp_ = cur[m]
                                k.op("act", lambda e, pt=pt, sp_=sp_: e.activation(pt[:, :n], sp_[:, :n], AF.Exp, scale=0.125),
                                     reads=[sp_], writes=[pt])
                                k.op("pe", lambda e, pt=pt, m=m, kt=kt, i=i: e.matmul(
                                    acc[2 * m][:, :n], V[:, kt, h * 128:(h + 1) * 128], pt[:, :n],
                                    start=(i == 0), stop=(i == len(kts) - 1)), reads=[V, pt], writes=[acc[2 * m]])
                                k.op("pe", lambda e, pt=pt, m=m, i=i: e.matmul(
                                    acc[2 * m + 1][:, :n], ones_b[:], pt[:, :n],
                                    start=(i == 0), stop=(i == len(kts) - 1)), reads=[ones_b, pt], writes=[acc[2 * m + 1]])
                            cur = nxt
                        rd1, o1s, rd2, o2s, o, sq = [wk.next() for _ in range(6)]
                        k.op("act", lambda e, o1s=o1s: e.copy(o1s[:, :n], acc[0][:, :n]), reads=[acc[0]], writes=[o1s])
                        k.op("dve", lambda e, rd1=rd1: e.reciprocal(rd1[:, :n], acc[1][:, :n]), reads=[acc[1]], writes=[rd1])
                        k.op("act", lambda e, o2s=o2s: e.copy(o2s[:, :n], acc[2][:, :n]), reads=[acc[2]], writes=[o2s])
                        k.op("dve", lambda e, rd2=rd2: e.reciprocal(rd2[:, :n], acc[3][:, :n]), reads=[acc[3]], writes=[rd2])
                        k.op("dve", lambda e, rd1=rd1, o1s=o1s: e.tensor_tensor(o1s[:, :n], o1s[:, :n], rd1[:, :n], ALU.mult),
                             reads=[o1s, rd1], writes=[o1s])
                        k.op("pool", lambda e, rd2=rd2, o2s=o2s: e.tensor_tensor(o2s[:, :n], o2s[:, :n], rd2[:, :n], ALU.mult),
                             reads=[o2s, rd2], writes=[o2s])
                        k.op("dve", lambda e, o=o, o1s=o1s, o2s=o2s: e.scalar_tensor_tensor(
                            o[:, :n], o2s[:, :n], nlam[:, 0:1], o1s[:, :n], ALU.mult, ALU.add), reads=[o1s, o2s, nlam], writes=[o])
                        k.op("pool", lambda e, o=o, sq=sq: e.tensor_tensor(sq[:, :n], o[:, :n], o[:, :n], ALU.mult), reads=[o], writes=[sq])
                        def epi_b(o=o, sq=sq, h=h, q0=q0, n=n):
                            ssp = sps.tiles[sps.i]
                            k.op("pe", lambda e: e.matmul(ssp[:, :n], ones_f[:], sq[:, :n], start=True, stop=True),
                                 reads=[ones_f, sq], writes=[ssp])
                            rs_ = wk.next()
                            k.op("dve", lambda e: e.tensor_scalar(rs_[:, :n], ssp[:, :n], 1.0 / 128, RMS_EPS, ALU.mult, ALU.add),
                                 reads=[ssp], writes=[rs_])
                            k.op("act", lambda e: e.activation(rs_[:, :n], rs_[:, :n], AF.Sqrt), reads=[rs_], writes=[rs_])
                            k.op("dve", lambda e: e.reciprocal(rs_[:, :n], rs_[:, :n]), reads=[rs_], writes=[rs_])
                            ys = yst.next()
                            k.op("dve", lambda e: e.scalar_tensor_tensor(
                                ys[:, :n], o[:, :n], gsc[:, h:h + 1], rs_[:, :n], ALU.mult, ALU.mult), reads=[o, rs_, gsc], writes=[ys])
                            k.dma("sp", YT[h * 128:(h + 1) * 128, q0:q0 + n], ys[:, :n], reads=[ys], writes=[YT])
                        pend_epi.append(epi_b)
                while pend_epi:
                    pend_epi.pop(0)()
                for _ in pcg:
                    pass
                k.barrier()
            if stop_after == "attn":
                break

            if stages is None or "lru" in stages:
              with ExitStack() as es:
                cw = sbt(nc, es, "cw", [128, 4, 4], F32)
                cb = sbt(nc, es, "cb", [128, 4], F32)
                wbd32 = sbt(nc, es, "wbd32", [128, 16, 128], F32)
                wbd = sbt(nc, es, "wbd", [128, 16, 128], BF16)
                lbias = sbt(nc, es, "lbias", [128, 16], F32)
                cch = sbt(nc, es, "cch", [128, 8], F32)
                cch2 = sbt(nc, es, "cch2", [128, 8], F32)
                k.dma("sp", cw[:], lru_cw[l], writes=[cw])
                k.dma("sp", cb[:], lru_cb[l], writes=[cb])
                k.dma("sp", wbd32[:], lru_wbd[l], writes=[wbd32])
                k.dma("sp", lbias[:], lru_bias[l], writes=[lbias])
                k.dma("sp", cch[:], lru_lam[l], writes=[cch])
                k.op("dve", lambda e: e.tensor_copy(wbd[:], wbd32[:]), reads=[wbd32], writes=[wbd])
                k.op("act", lambda e: e.activation(cch[:], cch[:], AF.Exp, scale=-1.0), reads=[cch], writes=[cch])
                k.op("act", lambda e: e.activation(cch[:], cch[:], AF.Ln, bias=1.0), reads=[cch], writes=[cch])
                k.op("dve", lambda e: e.tensor_scalar_mul(cch2[:], cch[:], -16.0), reads=[cch], writes=[cch2])
                k.op("dve", lambda e: e.tensor_scalar_mul(cch[:], cch[:], -8.0), reads=[cch], writes=[cch])
                lxb = sbt(nc, es, "lxb", [128, T], BF16)
                u = sbt(nc, es, "u", [128, T], F32)
                ub = sbt(nc, es, "ub", [128, T], BF16)
                rr2 = [sbt(nc, es, f"rr{d}", [128, T], F32) for d in range(2)]
                ig2 = [sbt(nc, es, f"ig{d}", [128, T], F32) for d in range(2)]
                aa2 = [sbt(nc, es, f"aa{d}", [128, T], F32) for d in range(2)]
                tmp2 = [sbt(nc, es, f"tmpb{d}", [128, T], F32) for d in range(2)]
                yy = sbt(nc, es, "yy", [128, T], F32)
                lgb = ub
                ybo = lxb
                pg = Rot([pst(nc, es, f"pg{i}", [128, 512]) for i in range(6)])
                SEGS = [(0, CTX), (CTX, T)]
                for ct in range(4):
                    k.dma("sp", lxb[:], PF[1024 + ct * 128:1024 + (ct + 1) * 128, :], reads=[PF], writes=[lxb])
                    k.op("dve", lambda e, ct=ct: e.tensor_scalar(u[:], lxb[:], cw[:, ct, 2:3], cb[:, ct:ct + 1], ALU.mult, ALU.add),
                         reads=[lxb, cw, cb], writes=[u])
                    for j in (0, 1, 3):
                        o = j - 2
                        for (s0, s1) in SEGS:
                            a_, b_ = (s0 - o, s1) if o < 0 else (s0, s1 - o)
                            k.op("dve", lambda e, ct=ct, j=j, a_=a_, b_=b_, o=o: e.scalar_tensor_tensor(
                                u[:, a_:b_], lxb[:, a_ + o:b_ + o], cw[:, ct, j:j + 1], u[:, a_:b_], ALU.mult, ALU.add),
                                reads=[lxb, cw, u], writes=[u])
                    k.op("pool", lambda e: e.tensor_copy(ub[:], u[:]), reads=[u], writes=[ub])
                    for d in range(2):
                        rr, ig = rr2[d], ig2[d]
                        ia = (d * 2 + 0) * 4 + ct
                        ii = (d * 2 + 1) * 4 + ct
                        for (t0, n) in CHUNKS:
                            pa = pg.next()
                            pi = pg.next()
                            k.op("pe", lambda e, pa=pa, ia=ia, t0=t0, n=n: e.matmul(pa[:, :n], wbd[:, ia, :], ub[:, t0:t0 + n], start=True, stop=True),
                                 reads=[wbd, ub], writes=[pa])
                            k.op("pe", lambda e, pi=pi, ii=ii, t0=t0, n=n: e.matmul(pi[:, :n], wbd[:, ii, :], ub[:, t0:t0 + n], start=True, stop=True),
                                 reads=[wbd, ub], writes=[pi])
                            k.op("act", lambda e, pa=pa, ia=ia, t0=t0, n=n, rr=rr: e.activation(rr[:, t0:t0 + n], pa[:, :n], AF.Sigmoid, bias=lbias[:, ia:ia + 1]),
                                 reads=[pa, lbias], writes=[rr])
                            k.op("act", lambda e, pi=pi, ii=ii, t0=t0, n=n, ig=ig: e.activation(ig[:, t0:t0 + n], pi[:, :n], AF.Sigmoid, bias=lbias[:, ii:ii + 1]),
                                 reads=[pi, lbias], writes=[ig])
                    k.dma("sp", lgb[:], PF[1536 + ct * 128:1536 + (ct + 1) * 128, :], reads=[PF], writes=[lgb])
                    for d in range(2):
                        rr, ig, aa, tmpb = rr2[d], ig2[d], aa2[d], tmp2[d]
                        dc = d * 4 + ct
                        k.op("act", lambda e, dc=dc, rr=rr, aa=aa: e.activation(aa[:], rr[:], AF.Exp, scale=cch[:, dc:dc + 1]), reads=[rr, cch], writes=[aa])
                        k.op("act", lambda e, dc=dc, rr=rr, tmpb=tmpb: e.activation(tmpb[:], rr[:], AF.Exp, scale=cch2[:, dc:dc + 1]), reads=[rr, cch2], writes=[tmpb])
                    for d in range(2):
                        tmpb = tmp2[d]
                        k.op("act", lambda e, tmpb=tmpb: e.activation(tmpb[:], tmpb[:], AF.Sqrt, bias=1.0, scale=-1.0), reads=[tmpb], writes=[tmpb])
                    for d in range(2):
                        rr, ig, aa, tmpb = rr2[d], ig2[d], aa2[d], tmp2[d]
                        k.op("dve", lambda e, ig=ig: e.tensor_tensor(ig[:], ig[:], u[:], ALU.mult), reads=[ig, u], writes=[ig])
                        k.op("pool", lambda e, ig=ig, tmpb=tmpb: e.tensor_tensor(ig[:], ig[:], tmpb[:], ALU.mult), reads=[ig, tmpb], writes=[ig])
                        dst = yy if d == 0 else rr
                        if d == 0:
                            k.op("dve", lambda e, dst=dst, aa=aa, ig=ig: e.tensor_tensor_scan(dst[:], aa[:], ig[:], 0.0, ALU.mult, ALU.add),
                                 reads=[aa, ig], writes=[dst])
                        else:
                            k.op("dve", lambda e, dst=dst, aa=aa, ig=ig: e.tensor_tensor_scan(dst[:, 0:CTX][:, ::-1], aa[:, 0:CTX][:, ::-1], ig[:, 0:CTX][:, ::-1],
                                                                                             0.0, ALU.mult, ALU.add), reads=[aa, ig], writes=[dst])
                            k.op("dve", lambda e, dst=dst, aa=aa, ig=ig: e.tensor_tensor_scan(dst[:, CTX:T][:, ::-1], aa[:, CTX:T][:, ::-1], ig[:, CTX:T][:, ::-1],
                                                                                             dst[:, 0:1], ALU.mult, ALU.add), reads=[aa, ig, dst], writes=[dst])
                            k.op("pool", lambda e, dst=dst: e.tensor_tensor(yy[:], yy[:], dst[:], ALU.add), reads=[yy, dst], writes=[yy])
                    tmpb = tmp2[0]
                    k.op("act", lambda e: e.activation(tmpb[:], lgb[:], AF.Gelu), reads=[lgb], writes=[tmpb])
                    k.op("dve", lambda e: e.tensor_tensor(ybo[:], yy[:], tmpb[:], ALU.mult), reads=[yy, tmpb], writes=[ybo])
                    k.dma("sp", YT[512 + ct * 128:512 + (ct + 1) * 128, :], ybo[:], reads=[ybo], writes=[YT])
                k.barrier()
            if stop_after == "lru":
                break

            if stages is None or "ssd" in stages:
              with ExitStack() as es:
                tri = sbt(nc, es, "tri", [128, 2, 128], F32)
                ntri = sbt(nc, es, "ntri", [128, 2, 128], F32)
                ones_f = sbt(nc, es, "ones_f2", [128, 128], F32)
                idb32 = sbt(nc, es, "idb32", [128, 128], F32)
                idb = sbt(nc, es, "idb", [128, 128], BF16)
                scw = sbt(nc, es, "scw", [128, 8, 4], F32)
                scb = sbt(nc, es, "scb", [128, 8], F32)
                dtv = sbt(nc, es, "dtv", [128, NT, 16], F32)
                av = sbt(nc, es, "av", [128, NT, 16], F32)
                eal = sbt(nc, es, "eal", [128, NT * 16], F32)
                dsk = sbt(nc, es, "dsk", [128, 8], F32)
                gn = sbt(nc, es, "gn", [128, 512], F32)
                BCT = sbt(nc, es, "BCT", [128, 4, T], BF16)
                Xtm = sbt(nc, es, "Xtm", [128, NT, 512], BF16)
                Btm = sbt(nc, es, "Btm", [128, NT, 256], BF16)
                k.dma("sp", tri[:], tri_in[:], writes=[tri])
                k.dma("sp", idb32[:], identb_in[:], writes=[idb32])
                k.dma("sp", scw[:], ssd_cw[l], writes=[scw])
                k.dma("sp", scb[:], ssd_cb[l], writes=[scb])
                k.dma("sp", dsk[:], ssd_dsk[l], writes=[dsk])
                k.dma("sp", gn[:], ssd_gn[l], writes=[gn])
                k.dma("sp", eal[:], ssd_alog[l], writes=[eal])
                k.dma("sp", av.ap.rearrange("p n c -> p (n c)"), ssd_dtb[l], writes=[av])
                k.dma("sp", dtv[:], PDT.ap.rearrange("(n p) c -> p n c", p=128), reads=[PDT], writes=[dtv])
                k.op("dve", lambda e: e.tensor_scalar_mul(ntri[:], tri[:], -1.0), reads=[tri], writes=[ntri])
                k.op("dve", lambda e: e.memset(ones_f[:], 1.0), writes=[ones_f])
                k.op("dve", lambda e: e.tensor_copy(idb[:], idb32[:]), reads=[idb32], writes=[idb])
                k.op("dve", lambda e: e.tensor_tensor(dtv[:], dtv[:], av[:], ALU.add), reads=[dtv, av], writes=[dtv])
                k.op("act", lambda e: e.activation(dtv[:], dtv[:], AF.Exp), reads=[dtv], writes=[dtv])
                k.op("act", lambda e: e.activation(dtv[:], dtv[:], AF.Ln, bias=1.0), reads=[dtv], writes=[dtv])
                k.op("act", lambda e: e.activation(eal[:], eal[:], AF.Exp), reads=[eal], writes=[eal])
                k.op("dve", lambda e: e.scalar_tensor_tensor(av.ap.rearrange("p n c -> p (n c)"), dtv.ap.rearrange("p n c -> p (n c)"), -1.0,
                                                            eal[:], ALU.mult, ALU.mult), reads=[dtv, eal], writes=[av])
                SEGS = [(0, CTX), (CTX, T)]
                with ExitStack() as es1:
                    XT = sbt(nc, es1, "XT", [128, 4, T], BF16)
                    inb = Rot([sbt(nc, es1, f"cinb{i}", [128, T], BF16) for i in range(2)])
                    uu = Rot([sbt(nc, es1, f"cu{i}", [128, T], F32) for i in range(2)])
                    ptb = Rot([pst(nc, es1, f"ptb{i}", [128, 512], BF16) for i in range(3)])
                    for ct in range(8):
                        ib = inb.next()
                        u = uu.next()
                        k.dma("sp" if ct % 2 == 0 else "pool", ib[:], PF[2048 + ct * 128:2048 + (ct + 1) * 128, :], reads=[PF], writes=[ib])
                        k.op("dve", lambda e, ct=ct, ib=ib, u=u: e.tensor_scalar(u[:], ib[:], scw[:, ct, 2:3], scb[:, ct:ct + 1], ALU.mult, ALU.add),
                             reads=[ib, scw, scb], writes=[u])
                        for j in (0, 1, 3):
                            o = j - 2
                            for (s0, s1) in SEGS:
                                a_, b_ = (s0 - o, s1) if o < 0 else (s0, s1 - o)
                                k.op("dve", lambda e, ct=ct, j=j, a_=a_, b_=b_, o=o, ib=ib, u=u: e.scalar_tensor_tensor(
                                    u[:, a_:b_], ib[:, a_ + o:b_ + o], scw[:, ct, j:j + 1], u[:, a_:b_], ALU.mult, ALU.add),
                                    reads=[ib, scw, u], writes=[u])
                        dst = XT[:, ct, :] if ct < 4 else BCT[:, ct - 4, :]
                        dstt = XT if ct < 4 else BCT
                        k.op("act", lambda e, u=u, dst=dst: e.activation(dst, u[:], AF.Silu), reads=[u], writes=[dstt])
                    for tt in range(NT):
                        p1 = ptb.next()
                        for c4 in range(4):
                            k.op("pe", lambda e, p1=p1, c4=c4, tt=tt: e.transpose(p1[:, c4 * 128:(c4 + 1) * 128], XT[:, c4, tt * 128:(tt + 1) * 128], idb[:]),
                                 reads=[XT, idb], writes=[p1])
                        k.op("dve" if tt % 2 == 0 else "act", (lambda e, p1=p1, tt=tt: e.tensor_copy(Xtm[:, tt, :], p1[:])) if tt % 2 == 0 else
                             (lambda e, p1=p1, tt=tt: e.copy(Xtm[:, tt, :], p1[:])), reads=[p1], writes=[Xtm])
                        p2 = ptb.next()
                        for g in range(2):
                            k.op("pe", lambda e, p2=p2, g=g, tt=tt: e.transpose(p2[:, g * 128:(g + 1) * 128], BCT[:, g, tt * 128:(tt + 1) * 128], idb[:]),
                                 reads=[BCT, idb], writes=[p2])
                        k.op("act" if tt % 2 == 0 else "dve", (lambda e, p2=p2, tt=tt: e.copy(Btm[:, tt, :], p2[:, 0:256])) if tt % 2 == 0 else
                             (lambda e, p2=p2, tt=tt: e.tensor_copy(Btm[:, tt, :], p2[:, 0:256])), reads=[p2], writes=[Btm])
                    k.barrier()
                Yacc = sbt(nc, es, "Yacc", [128, NT, 512], BF16)
                with ExitStack() as es2:
                    S = sbt(nc, es2, "S", [128, 512], F32)
                    Sb = sbt(nc, es2, "Sb", [128, 512], BF16)
                    Rr = Rot([sbt(nc, es2, f"Rr{i}", [128, 8, 128], F32) for i in range(3)])
                    R2 = Rot([sbt(nc, es2, f"R2{i}", [128, 8, 128], F32) for i in range(2)])
                    Lx = Rot([sbt(nc, es2, f"Lx{i}", [128, 8, 128], F32) for i in range(3)])
                    Mt = Rot([sbt(nc, es2, f"Mt{i}", [128, 8, 128], BF16) for i in range(3)])
                    CBm = Rot([sbt(nc, es2, f"CBm{i}", [128, 2, 128], F32) for i in range(3)])
                    xdt = Rot([sbt(nc, es2, f"xdt{i}", [128, 512], BF16) for i in range(3)])
                    wx = Rot([sbt(nc, es2, f"wx{i}", [128, 512], BF16) for i in range(3)])
                    ecs = Rot([sbt(nc, es2, f"ecs{i}", [128, 8], F32) for i in range(4)])
                    decb = Rot([sbt(nc, es2, f"decb{i}", [128, 8], F32) for i in range(4)])
                    yo = Rot([sbt(nc, es2, f"yo{i}", [128, 512], F32) for i in range(2)])
                    PD = pst(nc, es2, "PD", [128, 1024])
                    PCB = pst(nc, es2, "PCB", [128, 512])
                    PYr = Rot([pst(nc, es2, f"PY{i}", [128, 512]) for i in range(2)])
                    PSr = Rot([pst(nc, es2, f"PS{i}", [128, 512]) for i in range(2)])
                    PYo = pst(nc, es2, "PYo", [128, 512])
                    first_y = [True] * NT

                    def ssd_s0(d, tt):
                        lend = 127 if d == 0 else 0
                        a_ap = av[:, tt, d * 8:(d + 1) * 8]
                        tsl = slice(tt * 128, (tt + 1) * 128)
                        r1 = Rr.next(); r2 = R2.next()
                        k.op("dve", lambda e: e.tensor_tensor(r1[:], tri[:, d, :].unsqueeze(1).to_broadcast([128, 8, 128]),
                                                              a_ap.unsqueeze(2).to_broadcast([128, 8, 128]), ALU.mult), reads=[tri, av], writes=[r1])
                        k.op("pool", lambda e: e.tensor_copy(r2[:], a_ap.unsqueeze(2).to_broadcast([128, 8, 128])), reads=[av], writes=[r2])
                        return (d, tt, tsl, lend, a_ap, r1, r2)

                    def ssd_s0m(d, tt, tsl, lend, a_ap, r1, r2):
                        for hf in range(2):
                            k.op("pe", lambda e, hf=hf: e.matmul(PD[:, hf * 512:(hf + 1) * 512], ones_f[:], r1[:, hf * 4:(hf + 1) * 4, :], start=True, stop=False),
                                 reads=[ones_f, r1], writes=[PD])
                            k.op("pe", lambda e, hf=hf: e.matmul(PD[:, hf * 512:(hf + 1) * 512], ntri[:, d, :], r2[:, hf * 4:(hf + 1) * 4, :], start=False, stop=True),
                                 reads=[ntri, r2], writes=[PD])
                        for g in range(2):
                            k.op("pe", lambda e, g=g: e.matmul(PCB[:, g * 128:(g + 1) * 128], BCT[:, g, tsl], BCT[:, 2 + g, tsl], start=True, stop=True),
                                 reads=[BCT], writes=[PCB])
                        k.op("pe", lambda e: e.matmul(PCB[:, 256:264], tri[:, d, :], a_ap, start=True, stop=True), reads=[tri, av], writes=[PCB])
                        k.op("pe", lambda e: e.matmul(PCB[:, 272:280], ones_f[:], a_ap, start=True, stop=True), reads=[ones_f, av], writes=[PCB])
                        return (d, tt, tsl, lend, a_ap)

                    def ssd_a(d, tt, tsl, lend, a_ap):
                        cbm = CBm.next()
                        k.op("dve", lambda e: e.tensor_tensor(cbm[:], PCB.ap[:, 0:256].rearrange("p (g l) -> p g l", g=2),
                                                              tri[:, d, :].unsqueeze(1).to_broadcast([128, 2, 128]), ALU.mult), reads=[PCB, tri], writes=[cbm])
                        lx = Lx.next()
                        k.op("dve", lambda e: e.tensor_tensor(lx[:], PD.ap.rearrange("p (h l) -> p h l", h=8), tri[:, d, :].unsqueeze(1).to_broadcast([128, 8, 128]), ALU.mult),
                             reads=[PD, tri], writes=[lx])
                        k.op("act", lambda e: e.activation(lx[:], lx[:], AF.Exp), reads=[lx], writes=[lx])
                        ec = ecs.next(); db = decb.next()
                        k.op("act", lambda e: e.activation(ec[:], PCB[:, 256:264], AF.Exp), reads=[PCB], writes=[ec])
                        k.op("act", lambda e: e.activation(db[:], PCB[:, 272:280], AF.Exp), reads=[PCB], writes=[db])
                        mt = Mt.next()
                        for g in range(2):
                            k.op("dve", lambda e, g=g: e.tensor_tensor(
                                mt[:, g * 4:(g + 1) * 4, :], lx[:, g * 4:(g + 1) * 4, :], cbm[:, g, :].unsqueeze(1).to_broadcast([128, 4, 128]), ALU.mult),
                                reads=[lx, cbm], writes=[mt])
                        xd = xdt.next()
                        k.op("dve", lambda e: e.tensor_tensor(
                            xd.ap.rearrange("p (h q) -> p h q", h=8), Xtm[:, tt, :].rearrange("p (h q) -> p h q", h=8),
                            dtv[:, tt, d * 8:(d + 1) * 8].unsqueeze(2).to_broadcast([128, 8, 64]), ALU.mult), reads=[Xtm, dtv], writes=[xd])
                        w_ = wx.next()
                        k.op("dve", lambda e: e.tensor_tensor(
                            w_.ap.rearrange("p (h q) -> p h q", h=8), xd.ap.rearrange("p (h q) -> p h q", h=8),
                            lx[:, :, lend:lend + 1].to_broadcast([128, 8, 64]), ALU.mult), reads=[xd, lx], writes=[w_])
                        return (d, tt, tsl, ec, db, mt, xd, w_)

                    def ssd_a2(d, tt, tsl, ec, db, mt, xd, w_):
                        PY = PYr.next(); PS = PSr.next()
                        for h in range(8):
                            k.op("pe", lambda e, h=h: e.matmul(PY[:, h * 64:(h + 1) * 64], mt[:, h, :], xd[:, h * 64:(h + 1) * 64], start=True, stop=True),
                                 reads=[mt, xd], writes=[PY])
                        for g in range(2):
                            k.op("pe", lambda e, g=g: e.matmul(PS[:, g * 256:(g + 1) * 256], Btm[:, tt, g * 128:(g + 1) * 128],
                                                               w_[:, g * 256:(g + 1) * 256], start=True, stop=True), reads=[Btm, w_], writes=[PS])
                        return (d, tt, tsl, ec, db, PY, PS)

                    def ssd_b(d, tt, tsl, ec, db, PY, PS):
                        for g in range(2):
                            k.op("pe", lambda e, g=g: e.matmul(PYo[:, g * 256:(g + 1) * 256], BCT[:, 2 + g, tsl], Sb[:, g * 256:(g + 1) * 256],
                                                               start=True, stop=True), reads=[BCT, Sb], writes=[PYo])
                        y_ = yo.next()
                        k.op("dve", lambda e: e.tensor_tensor(y_.ap.rearrange("p (h q) -> p h q", h=8), PYo.ap.rearrange("p (h q) -> p h q", h=8),
                                                              ec[:].unsqueeze(2).to_broadcast([128, 8, 64]), ALU.mult), reads=[PYo, ec], writes=[y_])
                        if first_y[tt]:
                            first_y[tt] = False
                            k.op("dve", lambda e: e.tensor_tensor(Yacc[:, tt, :], PY[:], y_[:], ALU.add), reads=[PY, y_], writes=[Yacc])
                        else:
                            k.op("pool", lambda e: e.tensor_tensor(Yacc[:, tt, :], Yacc[:, tt, :], y_[:], ALU.add), reads=[Yacc, y_], writes=[Yacc])
                            k.op("dve", lambda e: e.tensor_tensor(Yacc[:, tt, :], Yacc[:, tt, :], PY[:], ALU.add), reads=[Yacc, PY], writes=[Yacc])
                        k.op("dve", lambda e: e.tensor_tensor(S.ap.rearrange("p (h q) -> p h q", h=8), S.ap.rearrange("p (h q) -> p h q", h=8),
                                                              db[:].unsqueeze(2).to_broadcast([128, 8, 64]), ALU.mult), reads=[S, db], writes=[S])
                        k.op("dve", lambda e: e.tensor_tensor(S[:], S[:], PS[:], ALU.add), reads=[S, PS], writes=[S])
                        k.op("pool", lambda e: e.tensor_copy(Sb[:], S[:]), reads=[S], writes=[Sb])

                    for d in range(2):
                        order = list(range(NT)) if d == 0 else [1, 0] + list(range(NT - 1, 1, -1))
                        k.op("dve", lambda e: e.memset(S[:], 0.0), writes=[S])
                        k.op("pool", lambda e: e.memset(Sb[:], 0.0), writes=[Sb])
                        n_o = len(order)
                        q0 = []; qa = []; qb = []
                        for step in range(n_o + 3):
                            s0e = ssd_s0(d, order[step]) if step < n_o else None
                            if 1 <= step <= n_o:
                                qa.append(ssd_a(*q0.pop(0)))
                            if s0e is not None:
                                q0.append(ssd_s0m(*s0e))
                            if 2 <= step <= n_o + 1:
                                qb.append(ssd_a2(*qa.pop(0)))
                            if 3 <= step <= n_o + 2:
                                ssd_b(*qb.pop(0))
                    k.barrier()
                with ExitStack() as es2:
                    zt = Rot([sbt(nc, es2, f"zt{i}", [128, 512], BF16) for i in range(2)])
                    sz = Rot([sbt(nc, es2, f"sz{i}", [128, 512], F32) for i in range(2)])
                    tq = Rot([sbt(nc, es2, f"tq{i}", [128, 512], F32) for i in range(2)])
                    ssq = Rot([sbt(nc, es2, f"ssq{i}", [128, 2], F32) for i in range(2)])
                    ynb = Rot([sbt(nc, es2, f"ynb{i}", [128, 512], BF16) for i in range(2)])
                    yct = Rot([sbt(nc, es2, f"yct{i}", [128, 512], BF16) for i in range(3)])
                    ptc = Rot([pst(nc, es2, f"ptc{i}", [128, 512], BF16) for i in range(2)])
                    for tt in range(NT):
                        z_ = zt.next(); s_ = sz.next(); t_ = tq.next(); q_ = ssq.next(); yn = ynb.next()
                        k.dma("sp", z_[:], PZ[tt * 128:(tt + 1) * 128, :], reads=[PZ], writes=[z_])
                        k.op("act", lambda e, z_=z_, s_=s_: e.activation(s_[:], z_[:], AF.Silu), reads=[z_], writes=[s_])
                        k.op("dve", lambda e, t_=t_, tt=tt: e.tensor_tensor(t_.ap.rearrange("p (h q) -> p h q", h=8), Xtm[:, tt, :].rearrange("p (h q) -> p h q", h=8),
                                                                            dsk[:].unsqueeze(2).to_broadcast([128, 8, 64]), ALU.mult), reads=[Xtm, dsk], writes=[t_])
                        k.op("pool", lambda e, t_=t_, tt=tt: e.tensor_tensor(t_[:], t_[:], Yacc[:, tt, :], ALU.add), reads=[t_, Yacc], writes=[t_])
                        k.op("dve", lambda e, t_=t_, s_=s_: e.tensor_tensor(t_[:], t_[:], s_[:], ALU.mult), reads=[t_, s_], writes=[t_])
                        for g in range(2):
                            k.op("act", lambda e, t_=t_, s_=s_, q_=q_, g=g: e.activation(s_[:, g * 256:(g + 1) * 256], t_[:, g * 256:(g + 1) * 256], AF.Square,
                                                                                         accum_out=q_[:, g:g + 1]), reads=[t_], writes=[s_, q_])
                        k.op("dve", lambda e, q_=q_: e.tensor_scalar(q_[:], q_[:], 1.0 / 256, RMS_EPS, ALU.mult, ALU.add), reads=[q_], writes=[q_])
                        k.op("act", lambda e, q_=q_: e.activation(q_[:], q_[:], AF.Sqrt), reads=[q_], writes=[q_])
                        k.op("dve", lambda e, q_=q_: e.reciprocal(q_[:], q_[:]), reads=[q_], writes=[q_])
                        for g in range(2):
                            k.op("dve", lambda e, t_=t_, q_=q_, yn=yn, g=g: e.scalar_tensor_tensor(
                                yn[:, g * 256:(g + 1) * 256], t_[:, g * 256:(g + 1) * 256], q_[:, g:g + 1], gn[:, g * 256:(g + 1) * 256], ALU.mult, ALU.mult),
                                reads=[t_, q_, gn], writes=[yn])
                        pc = ptc.next()
                        for c4 in range(4):
                            k.op("pe", lambda e, pc=pc, yn=yn, c4=c4: e.transpose(pc[:, c4 * 128:(c4 + 1) * 128], yn[:, c4 * 128:(c4 + 1) * 128], idb[:]),
                                 reads=[yn, idb], writes=[pc])
                        yc_ = yct.next()
                        k.op("act", lambda e, pc=pc, yc_=yc_: e.copy(yc_[:], pc[:]), reads=[pc], writes=[yc_])
                        k.dma("pool", YT.ap[1024:1536, tt * 128:(tt + 1) * 128].rearrange("(c p) t -> p c t", p=128),
                              yc_.ap.rearrange("p (c t) -> p c t", c=4), reads=[yc_], writes=[YT])
                    k.barrier()
            if stop_after == "ssd":
                break

            def rowbc(es_, dst, c0, j, ones_f_, pbc_):
                dg = sbt(nc, es_, "dg", [128, 4, 128], F32)
                for hf in range(2):
                    for c in range(4):
                        k.op("dve", lambda e, c=c, hf=hf: e.tensor_scalar_mul(dg[:, c, :], ident[:], mod[:, c0 + hf * 4 + c, j:j + 1]),
                             reads=[ident, mod], writes=[dg])
                    k.op("pe", lambda e: e.matmul(pbc_[:], ones_f_[:], dg.ap.rearrange("p c q -> p (c q)"), start=True, stop=True),
                         reads=[ones_f_, dg], writes=[pbc_])
                    k.op("act", lambda e, hf=hf: e.copy(dst[:, hf * 512:(hf + 1) * 512], pbc_[:]), reads=[pbc_], writes=[dst])

            def ln_tile(v, junk, st4, gbc, bbc):
                k.op("dve", lambda e: e.reduce_sum(st4[:, 0:1], v[:], AX.X), reads=[v], writes=[st4])
                k.op("dve", lambda e: e.tensor_scalar_mul(st4[:, 1:2], st4[:, 0:1], -1.0 / D), reads=[st4], writes=[st4])
                k.op("act", lambda e: e.activation(junk[:], v[:], AF.Square, bias=st4[:, 1:2], accum_out=st4[:, 2:3]), reads=[v, st4], writes=[junk, st4])
                k.op("dve", lambda e: e.tensor_scalar(st4[:, 3:4], st4[:, 2:3], 1.0 / D, LN_EPS, ALU.mult, ALU.add), reads=[st4], writes=[st4])
                k.op("act", lambda e: e.activation(st4[:, 3:4], st4[:, 3:4], AF.Sqrt), reads=[st4], writes=[st4])
                k.op("dve", lambda e: e.reciprocal(st4[:, 3:4], st4[:, 3:4]), reads=[st4], writes=[st4])
                k.op("dve", lambda e: e.tensor_scalar(v[:], v[:], st4[:, 1:2], st4[:, 3:4], ALU.add, ALU.mult), reads=[v, st4], writes=[v])
                k.op("pool", lambda e: e.tensor_tensor(v[:], v[:], gbc[:], ALU.mult), reads=[v, gbc], writes=[v])
                k.op("pool", lambda e: e.tensor_tensor(v[:], v[:], bbc[:], ALU.add), reads=[v, bbc], writes=[v])

            last = (l == DEPTH - 1)
            if stages is None or "merge" in stages:
              with ExitStack() as es:
                ones_f = sbt(nc, es, "ones_f3", [128, 128], F32)
                k.op("dve", lambda e: e.memset(ones_f[:], 1.0), writes=[ones_f])
                wbr = sbt(nc, es, "wbr", [128, 12, D], BF16)
                wo = sbt(nc, es, "wo", [128, KC, D], BF16)
                g1bc = [sbt(nc, es, f"g1bc{j}", [128, D], F32) for j in range(2)]
                lng = sbt(nc, es, "lng", [128, D], F32)
                lnb = sbt(nc, es, "lnb", [128, D], F32)
                k.dma("sp", lng[:], ln_gb[l, 0], writes=[lng])
                k.dma("sp", lnb[:], ln_gb[l, 1], writes=[lnb])
                with ExitStack() as es1:
                    stg32 = Rot([sbt(nc, es1, f"wstg{i}", [128, 4, D], F32) for i in range(2)])
                    pbc = pst(nc, es1, "pbc", [128, 512])
                    for i in range(3):
                        a = stg32.next()
                        k.dma("sp", a[:], w_br[l, :, i * 4:(i + 1) * 4, :], writes=[a])
                        k.op("dve" if i % 2 == 0 else "pool", lambda e, a=a, i=i: e.tensor_copy(wbr[:, i * 4:(i + 1) * 4, :], a[:]), reads=[a], writes=[wbr])
                    for i in range(2):
                        a = stg32.next()
                        k.dma("sp", a[:], w_o[l, :, i * 4:(i + 1) * 4, :], writes=[a])
                        k.op("pool" if i % 2 == 0 else "dve", lambda e, a=a, i=i: e.tensor_copy(wo[:, i * 4:(i + 1) * 4, :], a[:]), reads=[a], writes=[wo])
                    for j in range(2):
                        rowbc(es1, g1bc[j], 16, j, ones_f, pbc)
                    k.barrier()
                ysb = Rot([sbt(nc, es, f"ysb{i}", [128, 12, 512], BF16) for i in range(2)])
                gsb = Rot([sbt(nc, es, f"gsb{i}", [128, 24, 512], BF16) for i in range(2)])
                mrg = Rot([sbt(nc, es, f"mrg{i}", [128, KC, 512], BF16) for i in range(2)])
                mm = Rot([sbt(nc, es, f"mm{i}", [128, 512], F32) for i in range(6)])
                xv = Rot([sbt(nc, es, f"xv{i}", [128, D], F32) for i in range(3)])
                tv = Rot([sbt(nc, es, f"tv{i}", [128, D], F32) for i in range(3)])
                junk = sbt(nc, es, "junk", [128, D], F32)
                st4 = Rot([sbt(nc, es, f"st4{i}", [128, 8], F32) for i in range(3)])
                pbr = Rot([pst(nc, es, f"pbr{i}", [128, 512]) for i in range(5)])
                pmx = Rot([pst(nc, es, f"pmx{i}", [128, 512]) for i in range(3)])
                def mg_a(t0, n):
                    y_ = ysb.next(); g_ = gsb.next(); mg = mrg.next()
                    k.dma("sp", y_[:, :, :n], YT.ap[:, t0:t0 + n].rearrange("(c p) t -> p c t", p=128), reads=[YT], writes=[y_])
                    k.dma("sp", g_[:, :, :n], PF.ap[3072:6144, t0:t0 + n].rearrange("(c p) t -> p c t", p=128), reads=[PF], writes=[g_])
                    k.op("act", lambda e: e.activation(g_[:, :, :n], g_[:, :, :n], AF.Sigmoid), reads=[g_], writes=[g_])
                    for dt_ in range(8):
                        ps3 = []
                        for br in range(3):
                            pb = pbr.next()
                            for kc in range(4):
                                k.op("pe", lambda e, pb=pb, br=br, kc=kc, dt_=dt_: e.matmul(
                                    pb[:, :n], wbr[:, br * 4 + kc, dt_ * 128:(dt_ + 1) * 128], y_[:, br * 4 + kc, :n],
                                    start=(kc == 0), stop=(kc == 3)), reads=[wbr, y_], writes=[pb])
                            ps3.append(pb)
                        m3 = [mm.next() for _ in range(3)]
                        for br in range(3):
                            k.op("dve", lambda e, br=br, m3=m3, ps3=ps3, dt_=dt_: e.tensor_tensor(
                                m3[br][:, :n], ps3[br][:, :n], g_[:, br * 8 + dt_, :n], ALU.mult), reads=[ps3[br], g_], writes=[m3[br]])
                        k.op("pool", lambda e, m3=m3: e.tensor_tensor(m3[0][:, :n], m3[0][:, :n], m3[1][:, :n], ALU.add), reads=[m3[0], m3[1]], writes=[m3[0]])
                        k.op("dve", lambda e, m3=m3, dt_=dt_: e.tensor_tensor(mg[:, dt_, :n], m3[0][:, :n], m3[2][:, :n], ALU.add),
                             reads=[m3[0], m3[2]], writes=[mg])
                    return (t0, n, mg)

                def mg_b(t0, n, mg):
                    j = 1 if t0 == 0 else 0
                    for tl in range(n // 128):
                        tt = t0 // 128 + tl
                        x_ = xv.next(); v_ = tv.next(); s4 = st4.next()
                        if l == 0:
                            k.dma("sp", x_[:], xin[tt * 128:(tt + 1) * 128, :], reads=[xin], writes=[x_])
                        else:
                            k.dma("sp", x_[:], xs_t[tt][:], reads=[xs_t[tt]], writes=[x_])
                        for eh in range(2):
                            pm_ = pmx.next()
                            for kc in range(KC):
                                k.op("pe", lambda e, pm_=pm_, kc=kc, eh=eh: e.matmul(
                                    pm_[:], mg[:, kc, tl * 128:(tl + 1) * 128], wo[:, kc, eh * 512:(eh + 1) * 512],
                                    start=(kc == 0), stop=(kc == KC - 1)), reads=[mg, wo], writes=[pm_])
                            k.op("dve", lambda e, pm_=pm_, eh=eh: e.tensor_tensor(v_[:, eh * 512:(eh + 1) * 512], pm_[:], g1bc[j][:, eh * 512:(eh + 1) * 512], ALU.mult),
                                 reads=[pm_, g1bc[j]], writes=[v_])
                        k.op("dve", lambda e: e.scalar_tensor_tensor(v_[:], x_[:], DN_ALPHA, v_[:], ALU.mult, ALU.add), reads=[x_, v_], writes=[v_])
                        k.op("dve", lambda e: e.reduce_sum(s4[:, 0:1], v_[:], AX.X), reads=[v_], writes=[s4])
                        k.op("dve", lambda e: e.tensor_scalar_mul(s4[:, 1:2], s4[:, 0:1], -1.0 / D), reads=[s4], writes=[s4])
                        k.op("act", lambda e: e.activation(junk[:], v_[:], AF.Square, bias=s4[:, 1:2], accum_out=s4[:, 2:3]), reads=[v_, s4], writes=[junk, s4])
                        k.op("dve", lambda e: e.tensor_scalar(s4[:, 3:4], s4[:, 2:3], 1.0 / D, LN_EPS, ALU.mult, ALU.add), reads=[s4], writes=[s4])
                        k.op("act", lambda e: e.activation(s4[:, 3:4], s4[:, 3:4], AF.Sqrt), reads=[s4], writes=[s4])
                        k.op("dve", lambda e: e.reciprocal(s4[:, 3:4], s4[:, 3:4]), reads=[s4], writes=[s4])
                        k.op("dve", lambda e: e.tensor_tensor(s4[:, 4:5], s4[:, 1:2], s4[:, 3:4], ALU.mult), reads=[s4], writes=[s4])
                        k.op("act", lambda e: e.activation(v_[:], v_[:], AF.Identity, bias=s4[:, 4:5], scale=s4[:, 3:4]), reads=[v_, s4], writes=[v_])
                        k.op("dve", lambda e: e.tensor_tensor(v_[:], v_[:], lng[:], ALU.mult), reads=[v_, lng], writes=[v_])
                        k.op("pool", lambda e: e.tensor_tensor(v_[:], v_[:], lnb[:], ALU.add), reads=[v_, lnb], writes=[v_])
                        k.dma("pool", xs_t[tt][:], v_[:], reads=[v_], writes=[xs_t[tt]])

                mchs = [c for c in CHUNKS if not (last and c[0] == 0)]
                pendm = mg_a(*mchs[0])
                for im in range(len(mchs)):
                    nxtm = mg_a(*mchs[im + 1]) if im + 1 < len(mchs) else None
                    mg_b(*pendm)
                    pendm = nxtm
                k.barrier()
            if stop_after == "merge":
                break

            if stages is None or "moe" in stages:
              tts = list(range(NT))
              with ExitStack() as es:
                ones_f = sbt(nc, es, "ones_f4", [128, 128], F32)
                k.op("dve", lambda e: e.memset(ones_f[:], 1.0), writes=[ones_f])
                wr = sbt(nc, es, "wr", [128, KC, E], F32)
                rb = sbt(nc, es, "rb", [128, E], F32)
                iot = sbt(nc, es, "iot", [128, 64], F32)
                iop = sbt(nc, es, "iop", [128, 1], F32)
                thr = sbt(nc, es, "thr", [128, NBR], F32)
                ust = sbt(nc, es, "ust", [128, 128], F32)
                EM = sbt(nc, es, "EM", [128, NT, 64], F32)
                WW = sbt(nc, es, "WW", [128, NT, 64], F32)
                IX = sbt(nc, es, "IX", [128, NT, 8], F32)
                sc2 = [sbt(nc, es, f"sc2bc{j}", [128, D], F32) for j in range(2)]
                sh2 = [sbt(nc, es, f"sh2bc{j}", [128, D], F32) for j in range(2)]
                k.dma("sp", wr[:], w_rt[l], writes=[wr])
                k.dma("sp", rb[:], r_bias[l], writes=[rb])
                k.dma("sp", iot[:], iota64[:], writes=[iot])
                k.dma("sp", iop[:], iotap[:], writes=[iop])
                k.dma("sp", thr[:], thr_in[:], writes=[thr])
                k.dma("sp", ust[:], ustrict[:], writes=[ust])
                pbc = pst(nc, es, "pbc3", [128, 512])
                with ExitStack() as es1:
                    for j in range(2):
                        rowbc(es1, sc2[j], 32, j, ones_f, pbc)
                        rowbc(es1, sh2[j], 24, j, ones_f, pbc)
                    for j in range(2):
                        k.op("dve", lambda e, j=j: e.tensor_scalar_add(sc2[j][:], sc2[j][:], 1.0), reads=[sc2[j]], writes=[sc2[j]])
                    k.barrier()
                zt_ = sbt(nc, es, "zt_", [128, NROWS // 128], U32)
                k.op("dve", lambda e: e.memset(zt_[:], 0), writes=[zt_])
                ROWTOK.multi = False
                k.dma("sp", ROWTOK.ap[0:NROWS, :].rearrange("(p a) o -> p (a o)", p=128), zt_[:], reads=[zt_], writes=[ROWTOK])
                rsh = sbt(nc, es, "rsh", [128, NT], U32)
                k.dma("sp", rsh[:], rowtok_sh[:], writes=[rsh])
                k.dma("sp", ROWTOK.ap[NBR * 256:NROWS, :].rearrange("(p a) o -> p (a o)", p=128), rsh[:], reads=[rsh], writes=[ROWTOK])
                k._need("pool", [ROWTOK.w])
                ROWTOK.multi = True
                ROWTOK.wset = {id(ROWTOK.w[0]): ROWTOK.w}
                ROWTOK.w = None
                xt = Rot([sbt(nc, es, f"ext{i}", [128, D], F32) for i in range(4)])
                h2f = Rot([sbt(nc, es, f"h2f{i}", [128, KC, 128], F32) for i in range(3)])
                h2t = Rot([sbt(nc, es, f"h2t{i}", [128, D], F32) for i in range(2)])
                h2b = Rot([sbt(nc, es, f"h2b{i}", [128, D], BF16) for i in range(2)])
                ptr = Rot([pst(nc, es, f"eptr{i}", [128, 512]) for i in range(3)])
                prt = Rot([pst(nc, es, f"prt{i}", [128, 512]) for i in range(2)])
                pcnt = pst(nc, es, "pcnt", [128, 512])
                rw = Rot([sbt(nc, es, f"rw{i}", [128, 64], F32) for i in range(10)])
                r8 = Rot([sbt(nc, es, f"r8{i}", [128, 8], F32) for i in range(9)])
                i8 = Rot([sbt(nc, es, f"i8{i}", [128, 8], U32) for i in range(2)])
                v3 = lambda t_: t_.ap.rearrange("p (g i) -> p g i", g=8)
                for tt in tts:
                    j = 1 if tt < 2 else 0
                    x_ = xt.next(); hf_ = h2f.next(); ht_ = h2t.next(); hb_ = h2b.next()
                    k.dma("sp", x_[:], xs_t[tt][:], reads=[xs_t[tt]], writes=[x_])
                    k.op("pool", lambda e, x_=x_, ht_=ht_, j=j: e.tensor_tensor(ht_[:], x_[:], sc2[j][:], ALU.mult), reads=[x_, sc2[j]], writes=[ht_])
                    k.op("pool", lambda e, hb_=hb_, ht_=ht_, j=j: e.tensor_tensor(hb_[:], ht_[:], sh2[j][:], ALU.add), reads=[ht_, sh2[j]], writes=[hb_])
                    k.dma("pool", H2TM[tt * 128:(tt + 1) * 128, :], hb_[:], reads=[hb_], writes=[H2TM])
                    for half in range(2):
                        p_t = ptr.next()
                        for q4 in range(4):
                            kc = half * 4 + q4
                            k.op("pe", lambda e, p_t=p_t, x_=x_, kc=kc, q4=q4: e.transpose(
                                p_t[:, q4 * 128:(q4 + 1) * 128], x_[:, kc * 128:(kc + 1) * 128], ident[:]), reads=[x_, ident], writes=[p_t])
                        for q4 in range(4):
                            kc = half * 4 + q4
                            if q4 % 2 == 0:
                                k.op("act", lambda e, p_t=p_t, kc=kc, q4=q4, j=j, hf_=hf_: e.activation(
                                    hf_[:, kc, :], p_t[:, q4 * 128:(q4 + 1) * 128], AF.Identity,
                                    bias=mod[:, 24 + kc, j:j + 1], scale=mod1[:, 32 + kc, j:j + 1]), reads=[p_t, mod, mod1], writes=[hf_])
                            else:
                                k.op("dve", lambda e, p_t=p_t, kc=kc, q4=q4, j=j, hf_=hf_: e.tensor_scalar(
                                    hf_[:, kc, :], p_t[:, q4 * 128:(q4 + 1) * 128], mod1[:, 32 + kc, j:j + 1], mod[:, 24 + kc, j:j + 1],
                                    ALU.mult, ALU.add), reads=[p_t, mod, mod1], writes=[hf_])
                    pr = prt.next()
                    for kc in range(KC):
                        k.op("pe", lambda e, pr=pr, hf_=hf_, kc=kc: e.matmul(pr[:, :E], hf_[:, kc, :], wr[:, kc, :], start=(kc == 0), stop=(kc == KC - 1)),
                             reads=[hf_, wr], writes=[pr])
                    sc = rw.next(); sel = rw.next(); eq = rw.next(); sel2 = rw.next(); selm = rw.next()
                    mx1 = r8.next(); mx2 = r8.next(); gs = r8.next(); srt = r8.next(); gm = r8.next(); t8 = r8.next(); sm = r8.next()
                    ix = i8.next()
                    k.op("act", lambda e, sc=sc, pr=pr: e.activation(sc[:], pr[:, :E], AF.Sigmoid), reads=[pr], writes=[sc])
                    k.op("dve", lambda e, sel=sel, sc=sc: e.tensor_tensor(sel[:], sc[:], rb[:], ALU.add), reads=[sc, rb], writes=[sel])
                    k.op("dve", lambda e, mx1=mx1, sel=sel: e.reduce_max(mx1[:], v3(sel), AX.X), reads=[sel], writes=[mx1])
                    k.op("dve", lambda e, eq=eq, sel=sel, mx1=mx1: e.tensor_tensor(v3(eq), v3(sel), mx1[:].unsqueeze(2).to_broadcast([128, 8, 8]), ALU.is_equal),
                         reads=[sel, mx1], writes=[eq])
                    k.op("dve", lambda e, sel2=sel2, eq=eq, sel=sel: e.scalar_tensor_tensor(sel2[:], eq[:], -1e9, sel[:], ALU.mult, ALU.add),
                         reads=[eq, sel], writes=[sel2])
                    k.op("dve", lambda e, mx2=mx2, sel2=sel2: e.reduce_max(mx2[:], v3(sel2), AX.X), reads=[sel2], writes=[mx2])
                    k.op("dve", lambda e, gs=gs, mx1=mx1, mx2=mx2: e.tensor_tensor(gs[:], mx1[:], mx2[:], ALU.add), reads=[mx1, mx2], writes=[gs])
                    k.op("dve", lambda e, srt=srt, gs=gs: e.max(srt[:], gs[:]), reads=[gs], writes=[srt])
                    k.op("dve", lambda e, gm=gm, gs=gs, srt=srt: e.tensor_scalar(gm[:], gs[:], srt[:, 3:4], None, ALU.is_ge), reads=[gs, srt], writes=[gm])
                    k.op("dve", lambda e, selm=selm, sel=sel, gm=gm: e.scalar_tensor_tensor(v3(selm), v3(sel), 10.0, gm[:].unsqueeze(2).to_broadcast([128, 8, 8]),
                                                                                           ALU.add, ALU.mult), reads=[sel, gm], writes=[selm])
                    k.op("dve", lambda e, t8=t8, selm=selm: e.max(t8[:], selm[:]), reads=[selm], writes=[t8])
                    k.op("dve", lambda e, ix=ix, t8=t8, selm=selm: e.max_index(ix[:], t8[:], selm[:]), reads=[selm, t8], writes=[ix])
                    k.op("dve", lambda e, ix=ix, tt=tt: e.tensor_copy(IX[:, tt, :], ix[:]), reads=[ix], writes=[IX])
                    k.op("dve", lambda e, selm=selm, t8=t8, tt=tt: e.tensor_scalar(EM[:, tt, :], selm[:], t8[:, 7:8], None, ALU.is_ge), reads=[selm, t8], writes=[EM])
                    k.op("dve", lambda e, sc=sc, tt=tt: e.tensor_tensor(WW[:, tt, :], sc[:], EM[:, tt, :], ALU.mult), reads=[sc, EM], writes=[WW])
                    k.op("dve", lambda e, sm=sm, tt=tt: e.reduce_sum(sm[:, 0:1], WW[:, tt, :], AX.X), reads=[WW], writes=[sm])
                    k.op("dve", lambda e, sm=sm: e.reciprocal(sm[:, 1:2], sm[:, 0:1]), reads=[sm], writes=[sm])
                    k.op("dve", lambda e, sm=sm, tt=tt: e.tensor_scalar(WW[:, tt, :], WW[:, tt, :], sm[:, 1:2], 2.5, ALU.mult, ALU.mult), reads=[WW, sm], writes=[WW])
                    k.op("pe", lambda e, tt=tt: e.matmul(pcnt[:, 0:64], ones_f[:], EM[:, tt, :], start=(tt == tts[0]), stop=(tt == tts[-1])),
                         reads=[ones_f, EM], writes=[pcnt])
                cnt = sbt(nc, es, "cnt", [128, 64], F32)
                pad = sbt(nc, es, "pad", [128, 64], F32)
                pend = sbt(nc, es, "pend", [128, 64], F32)
                carry = sbt(nc, es, "carry", [128, 64], F32)
                one64 = sbt(nc, es, "one64", [128, 64], F32)
                k.op("dve", lambda e: e.memset(one64[:], 1.0), writes=[one64])
                cmp2 = sbt(nc, es, "cmp2", [128, 64, 18], F32)
                k.op("dve", lambda e: e.tensor_copy(cnt[:], pcnt[:, 0:64]), reads=[pcnt], writes=[cnt])
                k.op("dve", lambda e: e.tensor_tensor(cmp2[:], cnt[:].unsqueeze(2).to_broadcast([128, 64, 18]),
                                                      thr[:, 0:18].unsqueeze(1).to_broadcast([128, 64, 18]), ALU.is_gt), reads=[cnt, thr], writes=[cmp2])
                k.op("dve", lambda e: e.reduce_sum(pad[:], cmp2[:], AX.X), reads=[cmp2], writes=[pad])
                k.op("dve", lambda e: e.tensor_scalar_mul(pad[:], pad[:], 256.0), reads=[pad], writes=[pad])
                k.op("dve", lambda e: e.tensor_tensor_scan(pend[:], one64[:], pad[:], 0.0, ALU.mult, ALU.add), reads=[one64, pad], writes=[pend])
                k.op("dve", lambda e: e.tensor_tensor(carry[:], pend[:], pad[:], ALU.subtract), reads=[pend, pad], writes=[carry])
                BCH = 50
                cmp_ = sbt(nc, es, "cmp_", [128, BCH, 64], F32)
                bke = sbt(nc, es, "bke", [128, NBLK], F32)
                for c in range(NBR // BCH):
                    k.op("dve", lambda e, c=c: e.tensor_tensor(cmp_[:], pend[:].unsqueeze(1).to_broadcast([128, BCH, 64]),
                                                               thr[:, c * BCH:(c + 1) * BCH].unsqueeze(2).to_broadcast([128, BCH, 64]), ALU.is_le),
                         reads=[pend, thr], writes=[cmp_])
                    k.op("dve", lambda e, c=c: e.reduce_sum(bke[:, c * BCH:(c + 1) * BCH], cmp_[:], AX.X), reads=[cmp_], writes=[bke])
                k.op("dve", lambda e: e.memset(bke[:, NBR:NBLK], 64.0), writes=[bke])
                k.op("dve", lambda e: e.tensor_scalar(bke[:, 0:NBR], bke[:, 0:NBR], 63.0, None, ALU.min), reads=[bke], writes=[bke])
                k.op("dve", lambda e: e.tensor_scalar(bke[:], bke[:], 128.0, iop[:, 0:1], ALU.mult, ALU.add), reads=[bke, iop], writes=[bke])
                k.op("dve", lambda e: e.tensor_copy(IDXW[:], bke[:]), reads=[bke], writes=[IDXW])
                tokid = sbt(nc, es, "tokid", [128, 1], U32)
                tokf = sbt(nc, es, "tokf", [128, 1], F32)
                dfu = Rot([sbt(nc, es, f"dfu{i}", [128, 64], F32) for i in range(2)])
                junk64 = sbt(nc, es, "junk64", [128, 64], F32)
                d8f = Rot([sbt(nc, es, f"d8f{i}", [128, 8], F32) for i in range(2)])
                ppf = prt
                for tt in tts:
                    pp_ = ppf.next(); df = dfu.next(); d8 = d8f.next()
                    k.op("pe", lambda e, pp_=pp_, tt=tt: e.matmul(pp_[:, 0:64], ust[:], EM[:, tt, :], start=True, stop=True), reads=[ust, EM], writes=[pp_])
                    k.op("pe", lambda e, pp_=pp_, tt=tt: e.matmul(pp_[:, 64:128], ones_f[:], EM[:, tt, :], start=True, stop=True), reads=[ones_f, EM], writes=[pp_])
                    k.op("dve", lambda e, pp_=pp_, df=df: e.tensor_tensor(df[:], pp_[:, 0:64], carry[:], ALU.add), reads=[pp_, carry], writes=[df])
                    k.op("dve", lambda e, pp_=pp_: e.tensor_tensor(carry[:], carry[:], pp_[:, 64:128], ALU.add), reads=[pp_, carry], writes=[carry])
                    k.op("dve", lambda e, d8=d8: e.memset(d8[:], 0.0), writes=[d8])
                    k.op("dve", lambda e, tt=tt: e.memset(W8[:, tt, :], 0.0), writes=[W8])
                    for kk in range(8):
                        k.op("dve", lambda e, df=df, d8=d8, tt=tt, kk=kk: e.scalar_tensor_tensor(
                            junk64[:], iot[:], IX[:, tt, kk:kk + 1], df[:], ALU.is_equal, ALU.mult, accum_out=d8[:, kk:kk + 1]),
                            reads=[iot, IX, df], writes=[junk64, d8])
                        k.op("dve", lambda e, tt=tt, kk=kk: e.scalar_tensor_tensor(
                            junk64[:], iot[:], IX[:, tt, kk:kk + 1], WW[:, tt, :], ALU.is_equal, ALU.mult, accum_out=W8[:, tt, kk:kk + 1]),
                            reads=[iot, IX, WW], writes=[junk64, W8])
                    k.op("dve", lambda e, d8=d8, tt=tt: e.tensor_copy(D8[:, tt, :], d8[:]), reads=[d8], writes=[D8])
                    k.op("dve", lambda e, tt=tt: e.tensor_scalar_add(tokf[:], iop[:], float(tt * 128)), reads=[iop], writes=[tokf])
                    k.op("dve", lambda e: e.tensor_copy(tokid[:], tokf[:]), reads=[tokf], writes=[tokid])
                    for kk in range(8):
                        k.dma_fn("pool", lambda e, tt=tt, kk=kk: e.indirect_dma_start(
                            out=ROWTOK[:], out_offset=bass.IndirectOffsetOnAxis(ap=D8[:, tt, kk:kk + 1], axis=0), in_=tokid[:], in_offset=None),
                            reads=[tokid, D8], writes=[ROWTOK])
                k.barrier()
              if stop_after == "route":
                break
              with ExitStack() as es:
                rtok = sbt(nc, es, "rtok", [128, 2 * NBLK], U32)
                rtc = [TT(rtok.ap[:, rj:rj + 1], f"rtc{rj}") for rj in range(2 * NBLK)]
                for rj in range(2 * NBLK):
                    k.dma("sp", rtc[rj][:], ROWTOK[rj * 128:(rj + 1) * 128, :], reads=[ROWTOK], writes=[rtc[rj]])
                wub = Rot([sbt(nc, es, f"wub{i}", [128, KC, 512], BF16) for i in range(4)])
                wdb = Rot([sbt(nc, es, f"wdb{i}", [128, 2, D], BF16) for i in range(4)])
                hg = Rot([sbt(nc, es, f"hg{i}", [128, D], BF16) for i in range(4)])
                hgT = Rot([sbt(nc, es, f"hgT{i}", [128, KC, 128], BF16) for i in range(3)])
                sg = Rot([sbt(nc, es, f"sg{i}", [128, 256], F32) for i in range(2)])
                hid = Rot([sbt(nc, es, f"hid{i}", [128, 256], BF16) for i in range(4)])
                hidT = Rot([sbt(nc, es, f"hidT{i}", [128, 2, 128], BF16) for i in range(3)])
                osb = Rot([sbt(nc, es, f"osb{i}", [128, D], BF16) for i in range(3)])
                ptg = Rot([pst(nc, es, f"ptg{i}", [128, 1024], BF16) for i in range(2)])
                pup = Rot([pst(nc, es, f"pup{i}", [128, 512]) for i in range(2)])
                pht = pst(nc, es, "pht", [128, 1024], BF16)
                pdn = [pst(nc, es, f"pdn{i}", [128, 512]) for i in range(2)]
                def ph_t8(rj):
                    g_ = hg.next()
                    k.dma_fn("pool", lambda e: e.indirect_dma_start(
                        out=g_[:], out_offset=None, in_=H2TM[:], in_offset=bass.IndirectOffsetOnAxis(ap=rtok[:, rj:rj + 1], axis=0)),
                        reads=[H2TM, rtc[rj]], writes=[g_])
                    pt_ = ptg.next(); gT = hgT.next()
                    for kc in range(KC):
                        k.op("pe", lambda e, kc=kc: e.transpose(pt_[:, kc * 128:(kc + 1) * 128], g_[:, kc * 128:(kc + 1) * 128], idb_g[:]),
                             reads=[g_, idb_g], writes=[pt_])
                    k.op("act", lambda e: e.copy(gT[:, 0:4, :], pt_.ap[:, 0:512].rearrange("p (a c) -> p a c", a=4)), reads=[pt_], writes=[gT])
                    k.op("dve", lambda e: e.tensor_copy(gT[:, 4:8, :], pt_.ap[:, 512:1024].rearrange("p (a c) -> p a c", a=4)), reads=[pt_], writes=[gT])
                    return gT

                def ph_up(gT, wu_):
                    pu_ = pup.next()
                    for kc in range(KC):
                        k.op("pe", lambda e, kc=kc: e.matmul(pu_[:], gT[:, kc, :], wu_[:, kc, :], start=(kc == 0), stop=(kc == KC - 1)),
                             reads=[gT, wu_], writes=[pu_])
                    s_ = sg.next(); h_ = hid.next()
                    k.op("act", lambda e: e.activation(s_[:], pu_[:, 0:256], AF.Silu), reads=[pu_], writes=[s_])
                    k.op("dve", lambda e: e.tensor_tensor(h_[:], s_[:], pu_[:, 256:512], ALU.mult), reads=[s_, pu_], writes=[h_])
                    return h_

                def ph_t2(h_):
                    hT = hidT.next()
                    for fh in range(2):
                        k.op("pe", lambda e, fh=fh: e.transpose(pht[:, fh * 128:(fh + 1) * 128], h_[:, fh * 128:(fh + 1) * 128], idb_g[:]),
                             reads=[h_, idb_g], writes=[pht])
                    k.op("act", lambda e: e.copy(hT[:], pht.ap[:, 0:256].rearrange("p (a c) -> p a c", a=2)), reads=[pht], writes=[hT])
                    return hT

                def ph_dn(rj, hT, wd_):
                    for dh in range(2):
                        for fh in range(2):
                            k.op("pe", lambda e, dh=dh, fh=fh: e.matmul(pdn[dh][:], hT[:, fh, :], wd_[:, fh, dh * 512:(dh + 1) * 512],
                                                                        start=(fh == 0), stop=(fh == 1)), reads=[hT, wd_], writes=[pdn[dh]])
                    o_ = osb.next()
                    k.op("act", lambda e: e.copy(o_[:, 0:512], pdn[0][:]), reads=[pdn[0]], writes=[o_])
                    k.op("dve", lambda e: e.tensor_copy(o_[:, 512:1024], pdn[1][:]), reads=[pdn[1]], writes=[o_])
                    k.dma("sp", OUTB[rj * 128:(rj + 1) * 128, :], o_[:], reads=[o_], writes=[OUTB])

                NSUB = 2 * NBLK
                wts = {}
                st_gT = {}; st_h = {}; st_hT = {}
                for step in range(NSUB + 3):
                    j = step
                    if j < NSUB:
                        if j % 2 == 0:
                            b = j // 2
                            wu_ = wub.next(); wd_ = wdb.next()
                            k.dma_fn("pool", lambda e, wu_=wu_, b=b: e.indirect_dma_start(
                                out=wu_.ap.rearrange("p a c -> p (a c)"), out_offset=None, in_=WUPB[:],
                                in_offset=bass.IndirectOffsetOnAxis(ap=IDXW[:, b:b + 1], axis=0)), reads=[WUPB, IDXW], writes=[wu_])
                            k.dma_fn("pool", lambda e, wd_=wd_, b=b: e.indirect_dma_start(
                                out=wd_.ap.rearrange("p a c -> p (a c)"), out_offset=None, in_=WDNB[:],
                                in_offset=bass.IndirectOffsetOnAxis(ap=IDXW[:, b:b + 1], axis=0)), reads=[WDNB, IDXW], writes=[wd_])
                            wts[b] = (wu_, wd_)
                        st_gT[j] = ph_t8(j)
                    j1 = step - 1
                    if 0 <= j1 < NSUB:
                        st_h[j1] = ph_up(st_gT.pop(j1), wts[j1 // 2][0])
                    j2 = step - 2
                    if 0 <= j2 < NSUB:
                        st_hT[j2] = ph_t2(st_h.pop(j2))
                    j3 = step - 3
                    if 0 <= j3 < NSUB:
                        ph_dn(j3, st_hT.pop(j3), wts[j3 // 2][1])
                        if j3 % 2 == 1:
                            wts.pop(j3 // 2)
                k.barrier()
              last_ = last
              with ExitStack() as es:
                ones_f = sbt(nc, es, "ones_f5", [128, 128], F32)
                k.op("dve", lambda e: e.memset(ones_f[:], 1.0), writes=[ones_f])
                g2bc = [sbt(nc, es, f"g2bc{j}", [128, D], F32) for j in range(2)]
                lng = sbt(nc, es, "lng2", [128, D], F32)
                lnb = sbt(nc, es, "lnb2", [128, D], F32)
                k.dma("sp", lng[:], ln_gb[l, 2], writes=[lng])
                k.dma("sp", lnb[:], ln_gb[l, 3], writes=[lnb])
                pbc = pst(nc, es, "pbc2", [128, 512])
                with ExitStack() as es1:
                    for j in range(2):
                        rowbc(es1, g2bc[j], 40, j, ones_f, pbc)
                    k.barrier()
                fp_ = Rot([sbt(nc, es, f"fp{i}", [128, D], BF16) for i in range(36)])
                fa = Rot([sbt(nc, es, f"fa{i}", [128, D], F32) for i in range(4)])
                xv = Rot([sbt(nc, es, f"xv2{i}", [128, D], F32) for i in range(4)])
                junk = sbt(nc, es, "junk2", [128, D], F32)
                st4 = Rot([sbt(nc, es, f"st42{i}", [128, 4], F32) for i in range(2)])
                def ln_tile_nopool(v, s4):
                    k.op("dve", lambda e: e.reduce_sum(s4[:, 0:1], v[:], AX.X), reads=[v], writes=[s4])
                    k.op("dve", lambda e: e.tensor_scalar_mul(s4[:, 1:2], s4[:, 0:1], -1.0 / D), reads=[s4], writes=[s4])
                    k.op("act", lambda e: e.activation(junk[:], v[:], AF.Square, bias=s4[:, 1:2], accum_out=s4[:, 2:3]), reads=[v, s4], writes=[junk, s4])
                    k.op("dve", lambda e: e.tensor_scalar(s4[:, 3:4], s4[:, 2:3], 1.0 / D, LN_EPS, ALU.mult, ALU.add), reads=[s4], writes=[s4])
                    k.op("act", lambda e: e.activation(s4[:, 3:4], s4[:, 3:4], AF.Sqrt), reads=[s4], writes=[s4])
                    k.op("dve", lambda e: e.reciprocal(s4[:, 3:4], s4[:, 3:4]), reads=[s4], writes=[s4])
                    k.op("dve", lambda e: e.tensor_tensor(s4[:, 4:5], s4[:, 1:2], s4[:, 3:4], ALU.mult), reads=[s4], writes=[s4])
                    k.op("act", lambda e: e.activation(v[:], v[:], AF.Identity, bias=s4[:, 4:5], scale=s4[:, 3:4]), reads=[v, s4], writes=[v])
                    k.op("dve", lambda e: e.tensor_tensor(v[:], v[:], lng[:], ALU.mult), reads=[v, lng], writes=[v])
                    k.op("dve", lambda e: e.tensor_tensor(v[:], v[:], lnb[:], ALU.add), reads=[v, lnb], writes=[v])

                st5 = Rot([sbt(nc, es, f"st5{i}", [128, 8], F32) for i in range(3)])
                pacc = Rot([pst(nc, es, f"pacc{i}", [128, 512]) for i in range(4)])
                dgs = Rot([sbt(nc, es, f"dgs{i}", [128, 128], BF16) for i in range(6)])

                def e3_a0(tt):
                    acc_ = fa.next(); x_ = xv.next()
                    k.dma("sp", x_[:], xs_t[tt][:], reads=[xs_t[tt]], writes=[x_])
                    psh = fp_.next()
                    k.dma("sp", psh[:], OUTB[NBR * 256 + tt * 128:NBR * 256 + (tt + 1) * 128, :], reads=[OUTB], writes=[psh])
                    ps_ = []
                    for kk in range(8):
                        p_ = fp_.next()
                        k.dma_fn("pool", lambda e, p_=p_, kk=kk: e.indirect_dma_start(
                            out=p_[:], out_offset=None, in_=OUTB[:], in_offset=bass.IndirectOffsetOnAxis(ap=D8[:, tt, kk:kk + 1], axis=0)),
                            reads=[OUTB, D8], writes=[p_])
                        ps_.append(p_)
                    return (tt, acc_, x_, psh, ps_)

                def e3_a1(tt, acc_, x_, psh, ps_):
                    j = 1 if tt < 2 else 0
                    pa = [pacc.next(), pacc.next()]
                    for kk in range(8):
                        dg_ = dgs.next()
                        p_ = ps_[kk]
                        k.op("dve", lambda e, dg_=dg_, kk=kk: e.tensor_scalar_mul(dg_[:], idb_g[:], W8[:, tt, kk:kk + 1]), reads=[idb_g, W8], writes=[dg_])
                        for hf in range(2):
                            k.op("pe", lambda e, dg_=dg_, p_=p_, hf=hf, kk=kk: e.matmul(pa[hf][:], dg_[:], p_[:, hf * 512:(hf + 1) * 512],
                                                                                       start=(kk == 0), stop=False), reads=[dg_, p_], writes=[pa[hf]])
                    for hf in range(2):
                        k.op("pe", lambda e, hf=hf: e.matmul(pa[hf][:], idb_g[:], psh[:, hf * 512:(hf + 1) * 512], start=False, stop=True),
                             reads=[idb_g, psh], writes=[pa[hf]])
                        k.op("dve", lambda e, hf=hf: e.tensor_tensor(acc_[:, hf * 512:(hf + 1) * 512], pa[hf][:], g2bc[j][:, hf * 512:(hf + 1) * 512], ALU.mult),
                             reads=[pa[hf], g2bc[j]], writes=[acc_])
                    k.op("dve", lambda e: e.scalar_tensor_tensor(acc_[:], x_[:], DN_ALPHA, acc_[:], ALU.mult, ALU.add), reads=[x_, acc_], writes=[acc_])
                    return (tt, acc_)

                def e3_b(tt, acc_):
                    s4 = st5.next()
                    ln_tile_nopool(acc_, s4)
                    if last_:
                        k.dma("sp", out[(tt - 2) * 128:(tt - 1) * 128, :], acc_[:], reads=[acc_], writes=[out])
                    else:
                        k.dma("sp", xs_t[tt][:], acc_[:], reads=[acc_], writes=[xs_t[tt]])

                e3_tiles = list(range(2, NT)) if last_ else tts
                n3 = len(e3_tiles)
                q0_ = [e3_a0(e3_tiles[0]), e3_a0(e3_tiles[1])]
                q1_ = [e3_a1(*q0_.pop(0))]
                for i3 in range(n3):
                    if i3 + 2 < n3:
                        q0_.append(e3_a0(e3_tiles[i3 + 2]))
                    if q0_:
                        q1_.append(e3_a1(*q0_.pop(0)))
                    e3_b(*q1_.pop(0))
                k.barrier()
            if stop_after == "moe":
                break
        k.barrier()
        k.close()
    return P


def _rope_tables():
    rows = LAT // 64
    row = np.repeat(np.arange(rows, dtype=np.float32), 64)
    col = (np.arange(LAT) % 64).astype(np.float32)
    inv = (10000.0 ** (-np.arange(16, dtype=np.float32) / 16)).astype(np.float32)
    ang = np.concatenate([row[:, None] * inv, col[:, None] * inv], axis=-1)
    cos = np.cos(ang).astype(np.float32)
    sin = np.sin(ang).astype(np.float32)
    C = np.ones((128, T), np.float32)
    S = np.zeros((128, T), np.float32)
    for r in range(128):
        j = r % 64
        C[r, CTX:] = cos[:, j % 32]
        S[r, CTX:] = -sin[:, j] if j < 32 else sin[:, j - 32]
    return C, S


def _swap_cols(w):
    idx = np.arange(512)
    blk = idx // 64
    j = idx % 64
    return w[:, blk * 64 + (j + 32) % 64]


def _blk(w):
    return np.ascontiguousarray(w.reshape(KC, 128, w.shape[1]).transpose(1, 0, 2))


def prep_shared(inputs):
    w_in = inputs["w_in"]
    sh = {}
    fm = np.empty((DEPTH, 14, 128, KC, 512), np.float32)
    tm = np.empty((DEPTH, 2, 128, KC, 512), np.float32)
    wdt = np.empty((DEPTH, 128, KC, 16), np.float32)
    for l in range(DEPTH):
        w = w_in[l]
        sl = lambda nm: w[:, _W[nm][0]:_W[nm][0] + _W[nm][1]]
        blocks = [sl("q"), _swap_cols(sl("q")), sl("k"), _swap_cols(sl("k")), sl("lx"), sl("lg"),
                  sl("xbc")[:, :512], sl("xbc")[:, 512:]] + [sl("gates")[:, i * 512:(i + 1) * 512] for i in range(6)]
        for i, b in enumerate(blocks):
            fm[l, i] = _blk(b)
        tm[l, 0] = _blk(sl("v"))
        tm[l, 1] = _blk(sl("z"))
        wdt[l] = _blk(sl("dt"))
    sh["w_fm"] = fm
    sh["w_tm"] = tm
    sh["w_dt"] = wdt
    sh["w_mod"] = np.ascontiguousarray(inputs["w_mod"])
    sh["b_modT"] = np.ascontiguousarray(inputs["b_mod"].reshape(DEPTH, 48, 128).transpose(0, 2, 1))
    lqk = np.concatenate([inputs["lam_q"].reshape(DEPTH, 1, 128), inputs["lam_k"].reshape(DEPTH, 1, 128)], axis=2)
    sh["lamqk"] = np.ascontiguousarray(np.broadcast_to(lqk, (DEPTH, 128, 256))).astype(np.float32)
    sh["attn_gT"] = np.ascontiguousarray(inputs["attn_norm_g"].transpose(0, 2, 1))
    L_ = DEPTH
    sh["lru_cw"] = np.ascontiguousarray(inputs["lru_conv_w"].reshape(L_, 4, 4, 128).transpose(0, 3, 2, 1))
    sh["lru_cb"] = np.ascontiguousarray(inputs["lru_conv_b"].reshape(L_, 4, 128).transpose(0, 2, 1))
    wbd = np.zeros((L_, 128, 16, 128), np.float32)
    for l in range(L_):
        for d in range(2):
            for ai, nm in enumerate(("lru_wa", "lru_wi")):
                for ct in range(4):
                    idx = (d * 2 + ai) * 4 + ct
                    for bb in range(2):
                        wbd[l, bb * 64:(bb + 1) * 64, idx, bb * 64:(bb + 1) * 64] = inputs[nm][l, d, 2 * ct + bb]
    sh["lru_wbd"] = wbd
    lb = np.stack([inputs["lru_ba"], inputs["lru_bi"]], axis=2)
    sh["lru_bias"] = np.ascontiguousarray(lb.reshape(L_, 2, 2, 4, 128).transpose(0, 4, 1, 2, 3).reshape(L_, 128, 16))
    sh["lru_lam"] = np.ascontiguousarray(inputs["lru_lambda"].reshape(L_, 2, 4, 128).transpose(0, 3, 1, 2).reshape(L_, 128, 8))
    sh["ssd_cw"] = np.ascontiguousarray(inputs["ssd_conv_w"].reshape(L_, 4, 8, 128).transpose(0, 3, 2, 1))
    sh["ssd_cb"] = np.ascontiguousarray(inputs["ssd_conv_b"].reshape(L_, 8, 128).transpose(0, 2, 1))
    sh["ssd_dtb"] = np.ascontiguousarray(np.broadcast_to(inputs["ssd_dt_bias"].reshape(L_, 1, 1, 16), (L_, 128, NT, 16)).reshape(L_, 128, NT * 16))
    sh["ssd_alog"] = np.ascontiguousarray(np.broadcast_to(inputs["ssd_a_log"].reshape(L_, 1, 1, 16), (L_, 128, NT, 16)).reshape(L_, 128, NT * 16))
    sh["ssd_dsk"] = np.ascontiguousarray(np.broadcast_to(inputs["ssd_d"].reshape(L_, 1, 8), (L_, 128, 8)))
    sh["ssd_gn"] = np.ascontiguousarray(np.broadcast_to(inputs["ssd_norm_g"].reshape(L_, 1, 512), (L_, 128, 512)))
    kk_, ll_ = np.meshgrid(np.arange(128), np.arange(128), indexing="ij")
    tri = np.stack([(kk_ <= ll_), (kk_ >= ll_)], axis=1).astype(np.float32)
    sh["tri"] = np.ascontiguousarray(tri)
    sh["maskf"] = np.ascontiguousarray(np.broadcast_to(tri[:, :, None, :], (128, 2, 8, 128)))
    sh["identb"] = np.eye(128, dtype=np.float32)
    sh["w_br"] = np.ascontiguousarray(inputs["w_branch"].reshape(L_, 12, 128, D).transpose(0, 2, 1, 3))
    sh["w_o"] = np.ascontiguousarray(inputs["w_out"].reshape(L_, KC, 128, D).transpose(0, 2, 1, 3))
    lngb = np.stack([inputs["ln1_g"], inputs["ln1_b"], inputs["ln2_g"], inputs["ln2_b"]], axis=1)
    sh["ln_gb"] = np.ascontiguousarray(np.broadcast_to(lngb[:, :, None, :], (L_, 4, 128, D)))
    sh["w_rt"] = np.ascontiguousarray(inputs["w_router"].reshape(L_, KC, 128, E).transpose(0, 2, 1, 3))
    sh["r_bias"] = np.ascontiguousarray(np.broadcast_to(inputs["router_bias"][:, None, :], (L_, 128, E)))
    wu = np.concatenate([inputs["w_up"], inputs["ws_up"][:, None]], axis=1)
    sh["w_upx"] = np.ascontiguousarray(wu.reshape(L_, E + 1, KC, 128, 512).transpose(0, 1, 3, 2, 4))
    wd = np.concatenate([inputs["w_down"], inputs["ws_down"][:, None]], axis=1)
    sh["w_dnx"] = np.ascontiguousarray(wd.reshape(L_, E + 1, 2, 128, D).transpose(0, 1, 3, 2, 4))
    sh["iota64"] = np.ascontiguousarray(np.broadcast_to(np.arange(64, dtype=np.float32)[None, :], (128, 64)))
    sh["iotap"] = np.arange(128, dtype=np.float32).reshape(128, 1)
    sh["thr_in"] = np.ascontiguousarray(np.broadcast_to((256.0 * np.arange(200, dtype=np.float32))[None, :], (128, 200)))
    kk2, tt2 = np.meshgrid(np.arange(128), np.arange(128), indexing="ij")
    sh["ustrict"] = (kk2 < tt2).astype(np.float32)
    sh["rowtok_sh"] = np.ascontiguousarray((np.arange(34, dtype=np.uint32)[None, :] + 34 * np.arange(128, dtype=np.uint32)[:, None]).astype(np.uint32))
    C, S = _rope_tables()
    sh["ropec"] = C
    sh["ropes"] = S
    sh["ident"] = np.eye(128, dtype=np.float32)
    return sh


def prep_core(inputs, b):
    d = {}
    d["xin"] = np.ascontiguousarray(np.concatenate([inputs["ctx"][b], inputs["x"][b]], axis=0))
    c2 = np.stack([inputs["c"][b].reshape(KC, 128).T, inputs["c_ctx"].reshape(KC, 128).T], axis=-1)
    d["c2"] = np.ascontiguousarray(c2.astype(np.float32))
    return d


_CACHE = {}


def kernel(**inputs):
    inputs = {k_: np.asarray(v) for k_, v in inputs.items()}
    if "prog" not in _CACHE:
        _CACHE["prog"] = build_program()
    P = _CACHE["prog"]
    sh = prep_shared(inputs)
    in_maps = []
    for b in range(8):
        m = dict(sh)
        m.update(prep_core(inputs, b))
        in_maps.append(m)
    res = run_bass_kernel_spmd(P.nc, in_maps, core_ids=list(range(8)))
    return np.stack([r["out"] for r in res.results], axis=0)
```
